# Optimizing a Trainium2 kernel written in Bass

```python
import jax
import jax.numpy as jnp
from jax import lax
import numpy as np

D_MODEL = 2048
BATCH = 2
SEQ = 8192
DEPTH = 2

GRID_W = 64
CTX_LEN = 256
HEAD_DIM = 128
ROPE_THETA = 10000.0
EPS = 1e-6
NEG = -1e30
N_BRANCH = 4
BRANCH_W = 512
QBLOCK = 128

ATT_HEADS = 4
ATT_KV_HEADS = 2
MLA_HEADS = 4
MLA_Q_LORA = 384
MLA_KV_LORA = 256
MLA_NOPE = 128
MLA_ROPE = 64
MLA_V = 128
WIN_HEADS = 4
WIN_KV_HEADS = 2
WINDOW = 128
NA_HEADS = 4
NA_WIN_H = 8
NA_WIN_W = 16
N_EXPERTS = 16
N_GROUPS = 4
TOPK_GROUPS = 1
TOP_K = 2
D_EXPERT = 512

SPLITS = (ATT_HEADS * HEAD_DIM, ATT_KV_HEADS * HEAD_DIM, ATT_KV_HEADS * HEAD_DIM,
          MLA_Q_LORA, MLA_KV_LORA, MLA_ROPE,
          WIN_HEADS * HEAD_DIM, WIN_KV_HEADS * HEAD_DIM, WIN_KV_HEADS * HEAD_DIM,
          NA_HEADS * HEAD_DIM, NA_HEADS * HEAD_DIM, NA_HEADS * HEAD_DIM)
D_IN = sum(SPLITS)

kernel_name = 'hybrid_gated_mixers_grouped_moe_dit_block'


def rms_norm(x, g):
    xf = x.astype(jnp.float32)
    xf = xf * lax.rsqrt(jnp.mean(xf * xf, axis=-1, keepdims=True) + EPS)
    return (xf * g.astype(jnp.float32)).astype(x.dtype)


def modulate(h, shift, scale):
    return h * (1 + scale) + shift


def heads(t, n_heads):
    return t.reshape(t.shape[:-1] + (n_heads, t.shape[-1] // n_heads))


def rope_1d(x, pos):
    d2 = x.shape[-1] // 2
    freqs = ROPE_THETA ** (-jnp.arange(d2, dtype=jnp.float32) / d2)
    ang = pos.astype(jnp.float32)[:, None] * freqs[None, :]
    cos = jnp.cos(ang)[None, :, None, :].astype(x.dtype)
    sin = jnp.sin(ang)[None, :, None, :].astype(x.dtype)
    x1, x2 = x[..., :d2], x[..., d2:]
    return jnp.concatenate([x1 * cos - x2 * sin, x1 * sin + x2 * cos], axis=-1)


def axial_rope(x, rows, cols):
    h = x.shape[-1] // 2
    return jnp.concatenate([rope_1d(x[..., :h], rows), rope_1d(x[..., h:], cols)], axis=-1)


def mixer_heads(proj, pos, mla_qa_norm, mla_w_uq, mla_kva_norm, mla_w_ukv,
                qn_att, kn_att, qn_mla, kn_mla, qn_win, kn_win, qn_na, kn_na):
    points, acc = [], 0
    for s in SPLITS[:-1]:
        acc += s
        points.append(acc)
    (a_q, a_k, a_v, m_cq, m_ckv, m_kr, w_q, w_k, w_v, n_q, n_k, n_v) = jnp.split(proj, points, axis=-1)
    if pos is None:
        rope = lambda t: t
    else:
        rope = lambda t: axial_rope(t, pos[0], pos[1])
    att = (rope(rms_norm(heads(a_q, ATT_HEADS), qn_att)),
           rope(rms_norm(heads(a_k, ATT_KV_HEADS), kn_att)),
           heads(a_v, ATT_KV_HEADS))
    q = heads(rms_norm(m_cq, mla_qa_norm) @ mla_w_uq, MLA_HEADS)
    kv = heads(rms_norm(m_ckv, mla_kva_norm) @ mla_w_ukv, MLA_HEADS)
    k_nope, v_m = kv[..., :MLA_NOPE], kv[..., MLA_NOPE:]
    k_rope = jnp.broadcast_to(m_kr[..., None, :], k_nope.shape[:-1] + (MLA_ROPE,))
    q = rms_norm(q, qn_mla)
    k = rms_norm(jnp.concatenate([k_nope, k_rope], axis=-1), kn_mla)
    q = jnp.concatenate([q[..., :MLA_NOPE], rope(q[..., MLA_NOPE:])], axis=-1)
    k = jnp.concatenate([k[..., :MLA_NOPE], rope(k[..., MLA_NOPE:])], axis=-1)
    mla = (q, k, v_m)
    win = (rope(rms_norm(heads(w_q, WIN_HEADS), qn_win)),
           rope(rms_norm(heads(w_k, WIN_KV_HEADS), kn_win)),
           heads(w_v, WIN_KV_HEADS))
    na = (rms_norm(heads(n_q, NA_HEADS), qn_na),
          rms_norm(heads(n_k, NA_HEADS), kn_na),
          heads(n_v, NA_HEADS))
    return att, mla, win, na


def dense_gqa(q, k, v, sink=None):
    b, lq, hq, d = q.shape
    hkv = k.shape[2]
    g = hq // hkv
    qg = q.reshape(b, lq, hkv, g, d)
    s = jnp.einsum('bqkgd,bskd->bkgqs', qg, k).astype(jnp.float32) * (d ** -0.5)
    if sink is None:
        p = jax.nn.softmax(s, axis=-1)
    else:
        s_sink = jnp.broadcast_to(sink.astype(jnp.float32).reshape(1, hkv, g, 1, 1), s.shape[:-1] + (1,))
        p = jax.nn.softmax(jnp.concatenate([s, s_sink], axis=-1), axis=-1)[..., :-1]
    o = jnp.einsum('bkgqs,bskd->bqkgd', p.astype(v.dtype), v)
    return o.reshape(b, lq, hq * v.shape[-1])


def blocked_dense_gqa(q, k_all, v_all):
    b, n, hq, d = q.shape
    qb = q.reshape(b, n // QBLOCK, QBLOCK, hq, d).swapaxes(0, 1)
    o = lax.map(lambda qi: dense_gqa(qi, k_all, v_all), qb)
    return o.swapaxes(0, 1).reshape(b, n, -1)


def windowed_gqa(q, k, v, k_ctx, v_ctx, sink):
    b, n, hq, d = q.shape
    hkv = k.shape[2]
    g = hq // hkv
    wb = WINDOW
    nb = n // wb
    n_ctx = k_ctx.shape[1]
    scale = d ** -0.5

    def band(t):
        tp = jnp.pad(t, ((0, 0), (wb, wb), (0, 0), (0, 0))).reshape(b, nb + 2, wb, hkv, t.shape[-1])
        return jnp.concatenate([tp[:, :-2], tp[:, 1:-1], tp[:, 2:]], axis=2)

    k_band, v_band = band(k), band(v)
    qb = q.reshape(b, nb, wb, hkv, g, d)
    s_loc = jnp.einsum('bnqkgd,bnskd->bnkgqs', qb, k_band).astype(jnp.float32) * scale
    blk = jnp.arange(nb)[:, None, None] * wb
    q_pos = blk + jnp.arange(wb)[None, :, None]
    k_pos = blk - wb + jnp.arange(3 * wb)[None, None, :]
    valid = (jnp.abs(q_pos - k_pos) <= WINDOW) & (k_pos >= 0) & (k_pos < n)
    s_loc = jnp.where(valid[None, :, None, None], s_loc, NEG)
    s_ctx = jnp.einsum('bnqkgd,bskd->bnkgqs', qb, k_ctx).astype(jnp.float32) * scale
    s_sink = jnp.broadcast_to(sink.astype(jnp.float32).reshape(1, 1, hkv, g, 1, 1), s_loc.shape[:-1] + (1,))
    p = jax.nn.softmax(jnp.concatenate([s_loc, s_ctx, s_sink], axis=-1), axis=-1).astype(v.dtype)
    p_loc, p_ctx = p[..., :3 * wb], p[..., 3 * wb:3 * wb + n_ctx]
    o = (jnp.einsum('bnkgqs,bnskd->bnqkgd', p_loc, v_band)
         + jnp.einsum('bnkgqs,bskd->bnqkgd', p_ctx, v_ctx))
    return o.reshape(b, n, hq * v.shape[-1])


def neighbourhood_attn(q, k, v, k_ctx, v_ctx, rpb):
    b, n, h, d = q.shape
    n_rows = n // GRID_W
    kh = min(NA_WIN_H, n_rows)
    kw = NA_WIN_W
    qg = q.reshape(b, n_rows, GRID_W, h, d)
    kg = k.reshape(b, n_rows, GRID_W, h, d)
    vg = v.reshape(b, n_rows, GRID_W, h, v.shape[-1])
    cols = jnp.arange(GRID_W)
    col_idx = jnp.clip(cols - kw // 2, 0, GRID_W - kw)[:, None] + jnp.arange(kw)[None, :]
    dx = col_idx - cols[:, None] + (NA_WIN_W - 1)
    scale = d ** -0.5

    def row(r):
        rs = jnp.clip(r - kh // 2, 0, n_rows - kh)
        k_nb = lax.dynamic_slice_in_dim(kg, rs, kh, axis=1)[:, :, col_idx]
        v_nb = lax.dynamic_slice_in_dim(vg, rs, kh, axis=1)[:, :, col_idx]
        q_r = lax.dynamic_index_in_dim(qg, r, axis=1, keepdims=False)
        dy = rs + jnp.arange(kh) - r + (NA_WIN_H - 1)
        bias = rpb[:, dy][:, :, dx].transpose(0, 2, 1, 3)
        s_nb = jnp.einsum('bwhd,bywkhd->bhwyk', q_r, k_nb).astype(jnp.float32) * scale
        s_nb = s_nb + bias[None].astype(jnp.float32)
        s_ctx = jnp.einsum('bwhd,bshd->bhws', q_r, k_ctx).astype(jnp.float32) * scale
        s = jnp.concatenate([s_nb.reshape(b, h, GRID_W, kh * kw), s_ctx], axis=-1)
        p = jax.nn.softmax(s, axis=-1).astype(v.dtype)
        p_nb = p[..., :kh * kw].reshape(b, h, GRID_W, kh, kw)
        p_ctx = p[..., kh * kw:]
        return (jnp.einsum('bhwyk,bywkhd->bwhd', p_nb, v_nb)
                + jnp.einsum('bhws,bshd->bwhd', p_ctx, v_ctx))

    o = lax.map(row, jnp.arange(n_rows))
    return o.swapaxes(0, 1).reshape(b, n, h * v.shape[-1])


def merge_branches(h, outs, w_branch, w_gate, b_gate, w_out):
    acc = None
    for i, o in enumerate(outs):
        y = jax.nn.sigmoid(h @ w_gate[i] + b_gate[i]) * (o @ w_branch[i])
        acc = y if acc is None else acc + y
    return acc @ w_out


def token_mixer(h, hc, need_ctx, w_in, head_params, win_sink, na_rpb, merge_params):
    n = h.shape[1]
    t = jnp.arange(n)
    pos = (t // GRID_W, t % GRID_W)
    att, mla, win, na = mixer_heads(h @ w_in, pos, *head_params)
    att_c, mla_c, win_c, na_c = mixer_heads(hc @ w_in, None, *head_params)
    cat = lambda a, bb: jnp.concatenate([a, bb], axis=1)
    outs = (blocked_dense_gqa(att[0], cat(att_c[1], att[1]), cat(att_c[2], att[2])),
            blocked_dense_gqa(mla[0], cat(mla_c[1], mla[1]), cat(mla_c[2], mla[2])),
            windowed_gqa(win[0], win[1], win[2], win_c[1], win_c[2], win_sink),
            neighbourhood_attn(na[0], na[1], na[2], na_c[1], na_c[2], na_rpb))
    y = merge_branches(h, outs, *merge_params)
    if not need_ctx:
        return y, None
    outs_c = (dense_gqa(*att_c), dense_gqa(*mla_c), dense_gqa(*win_c, sink=win_sink), dense_gqa(*na_c))
    yc = merge_branches(hc, outs_c, *merge_params)
    return y, yc


def moe(h, w_router, b_router, w1, w3, w2):
    shape = h.shape
    t = h.reshape(-1, shape[-1])
    scores = jax.nn.sigmoid((t @ w_router).astype(jnp.float32))
    biased = scores + b_router.astype(jnp.float32)
    grp = biased.reshape(-1, N_GROUPS, N_EXPERTS // N_GROUPS)
    grp_score = lax.top_k(grp, 2)[0].sum(axis=-1)
    _, g_idx = lax.top_k(grp_score, TOPK_GROUPS)
    g_mask = jax.nn.one_hot(g_idx, N_GROUPS, dtype=jnp.float32).sum(axis=-2)
    e_mask = jnp.repeat(g_mask, N_EXPERTS // N_GROUPS, axis=-1)
    _, e_idx = lax.top_k(jnp.where(e_mask > 0, biased, NEG), TOP_K)
    w = jnp.take_along_axis(scores, e_idx, axis=-1)
    w = w / jnp.sum(w, axis=-1, keepdims=True)
    combine = jnp.sum(jax.nn.one_hot(e_idx, N_EXPERTS, dtype=jnp.float32) * w[..., None], axis=-2).astype(t.dtype)
    out = jnp.zeros_like(t)
    for e in range(N_EXPERTS):
        hid = jax.nn.silu(t @ w1[e]) * (t @ w3[e])
        out = out + combine[:, e:e + 1] * (hid @ w2[e])
    return out.reshape(shape)


def setup_inputs(seed: int = 0) -> dict:
    key = jax.random.key(seed)
    keys = jax.random.split(key, 32)

    def nrm(i, shape, scale):
        return jax.random.normal(keys[i], shape, jnp.float32) * scale

    def gain(i, shape):
        return 1.0 + nrm(i, shape, 0.02)

    L, D = DEPTH, D_MODEL
    return {
        'x': nrm(0, (BATCH, SEQ, D), 1.0),
        'c': nrm(1, (BATCH, D), 1.0),
        'ctx': nrm(2, (BATCH, CTX_LEN, D), 1.0),
        'c_ctx': nrm(3, (D,), 1.0),
        'w_mod': nrm(4, (L, D, 6 * D), 0.5 * D ** -0.5),
        'b_mod': nrm(5, (L, 6 * D), 0.02),
        'norm_mix': gain(6, (L, D)),
        'norm_ffn': gain(7, (L, D)),
        'w_in': nrm(8, (L, D, D_IN), D ** -0.5),
        'mla_qa_norm': gain(9, (L, MLA_Q_LORA)),
        'mla_w_uq': nrm(10, (L, MLA_Q_LORA, MLA_HEADS * (MLA_NOPE + MLA_ROPE)), MLA_Q_LORA ** -0.5),
        'mla_kva_norm': gain(11, (L, MLA_KV_LORA)),
        'mla_w_ukv': nrm(12, (L, MLA_KV_LORA, MLA_HEADS * (MLA_NOPE + MLA_V)), MLA_KV_LORA ** -0.5),
        'qn_att': gain(13, (L, HEAD_DIM)),
        'kn_att': gain(14, (L, HEAD_DIM)),
        'qn_mla': gain(15, (L, MLA_NOPE + MLA_ROPE)),
        'kn_mla': gain(16, (L, MLA_NOPE + MLA_ROPE)),
        'qn_win': gain(17, (L, HEAD_DIM)),
        'kn_win': gain(18, (L, HEAD_DIM)),
        'qn_na': gain(19, (L, HEAD_DIM)),
        'kn_na': gain(20, (L, HEAD_DIM)),
        'win_sink': nrm(21, (L, WIN_HEADS), 1.0),
        'na_rpb': nrm(22, (L, NA_HEADS, 2 * NA_WIN_H - 1, 2 * NA_WIN_W - 1), 0.5),
        'w_branch': nrm(23, (L, N_BRANCH, BRANCH_W, D), BRANCH_W ** -0.5),
        'w_gate': nrm(24, (L, N_BRANCH, D, D), D ** -0.5),
        'b_gate': nrm(25, (L, N_BRANCH, D), 0.02),
        'w_out': nrm(26, (L, D, D), D ** -0.5),
        'w_router': nrm(27, (D, N_EXPERTS), D ** -0.5),
        'b_router': nrm(28, (N_EXPERTS,), 0.01),
        'moe_w1': nrm(29, (L, N_EXPERTS, D, D_EXPERT), D ** -0.5),
        'moe_w3': nrm(30, (L, N_EXPERTS, D, D_EXPERT), D ** -0.5),
        'moe_w2': nrm(31, (L, N_EXPERTS, D_EXPERT, D), D_EXPERT ** -0.5),
    }


def reference(x, c, ctx, c_ctx, w_mod, b_mod, norm_mix, norm_ffn, w_in,
              mla_qa_norm, mla_w_uq, mla_kva_norm, mla_w_ukv,
              qn_att, kn_att, qn_mla, kn_mla, qn_win, kn_win, qn_na, kn_na,
              win_sink, na_rpb, w_branch, w_gate, b_gate, w_out,
              w_router, b_router, moe_w1, moe_w3, moe_w2):
    c_s = jax.nn.silu(c)
    cc_s = jax.nn.silu(c_ctx)
    xc = ctx
    for l in range(DEPTH):
        need_ctx = l < DEPTH - 1
        mod = c_s @ w_mod[l] + b_mod[l]
        mod_c = cc_s @ w_mod[l] + b_mod[l]
        sh1, sc1, g1, sh2, sc2, g2 = jnp.split(mod[:, None, :], 6, axis=-1)
        sh1c, sc1c, g1c, sh2c, sc2c, g2c = jnp.split(mod_c, 6, axis=-1)
        head_params = (mla_qa_norm[l], mla_w_uq[l], mla_kva_norm[l], mla_w_ukv[l],
                       qn_att[l], kn_att[l], qn_mla[l], kn_mla[l],
                       qn_win[l], kn_win[l], qn_na[l], kn_na[l])
        merge_params = (w_branch[l], w_gate[l], b_gate[l], w_out[l])
        h = modulate(rms_norm(x, norm_mix[l]), sh1, sc1)
        hc = modulate(rms_norm(xc, norm_mix[l]), sh1c, sc1c)
        y, yc = token_mixer(h, hc, need_ctx, w_in[l], head_params, win_sink[l], na_rpb[l], merge_params)
        x = x + g1 * y
        h2 = modulate(rms_norm(x, norm_ffn[l]), sh2, sc2)
        x = x + g2 * moe(h2, w_router, b_router, moe_w1[l], moe_w3[l], moe_w2[l])
        if need_ctx:
            xc = xc + g1c * yc
            hc2 = modulate(rms_norm(xc, norm_ffn[l]), sh2c, sc2c)
            xc = xc + g2c * moe(hc2, w_router, b_router, moe_w1[l], moe_w3[l], moe_w2[l])
    return x
```

```python
import numpy as np
from contextlib import ExitStack
import concourse.bass as bass
import concourse.mybir as mybir
from concourse.bass_utils import run_bass_kernel_spmd

F32 = mybir.dt.float32
BF16 = mybir.dt.bfloat16
AF = mybir.ActivationFunctionType
ALU = mybir.AluOpType
AX = mybir.AxisListType

NCORES = 8
D = 2048
KC = 16
SEQ = 8192
NLAT = 2048
NCTX = 64
NT = NLAT + NCTX
CTX = 256
DIN = 4288
EPS = 1e-6
NEG = -30000.0
ENGS = ['sync', 'act', 'dve', 'pool', 'pe']
SAME_ENG_SYNC = True


class V:
    __slots__ = ('tile', 'ap')

    def __init__(self, tile, ap):
        self.tile, self.ap = tile, ap


class Tile:
    def __init__(self, t, name):
        self.t, self.name = t, name
        self.lw = None
        self.rd = {}
        self.semid = None
        self.cnt = 0

    def __getitem__(self, idx):
        return V(self, self.t[idx])


class Prog:
    def __init__(self, nc, es):
        self.nc, self.es = nc, es
        self.ops = {e: [] for e in ENGS}
        self.cops = {e: [] for e in ENGS}
        self.sem_pool = []
        self.semcnt = {}
        self.all_tiles = []
        self.uid = 0
        self.engsem = {e: es.enter_context(nc.semaphore('eng_' + e)) for e in ENGS}
        self.dsem = {}
        self.nsig = {e: 0 for e in ENGS}
        self.nassigned = {e: 0 for e in ENGS}
        self.waited = {e: {} for e in ENGS}
        self.sim_c, self.sim_d = {}, {}

    class Scope:
        def __init__(self, P):
            self.P, self.es, self.tiles = P, ExitStack(), []

        def __enter__(self):
            self.es.__enter__()
            return self

        def __exit__(self, *a):
            P = self.P
            P.barrier()
            P.flush()
            for t in self.tiles:
                if t.semid is not None:
                    P.sem_pool.append((t.semid, t.cnt))
                P.all_tiles.remove(t)
            return self.es.__exit__(*a)

    def scope(self):
        return Prog.Scope(self)

    def sb(self, sc, name, shape, dt):
        self.uid += 1
        name = f's{self.uid}_{name}'
        t = sc.es.enter_context(self.nc.sbuf_tensor(name, shape, dt))
        tl = Tile(t, name)
        self.all_tiles.append(tl)
        sc.tiles.append(tl)
        return tl

    def ps(self, sc, name, shape, dt=F32):
        self.uid += 1
        name = f'p{self.uid}_{name}'
        t = sc.es.enter_context(self.nc.psum_tensor(name, shape, dt))
        tl = Tile(t, name)
        self.all_tiles.append(tl)
        sc.tiles.append(tl)
        return tl

    def ring(self, sc, name, n, shape, dt, psum=False):
        return Ring([(self.ps if psum else self.sb)(sc, f'{name}{i}', shape, dt) for i in range(n)])

    def _deps(self, eng, reads, writes):
        deps = {}

        def add(tok):
            if tok is None:
                return
            if tok[0] == 'c':
                if tok[1] == eng and (eng == 'pe' or not SAME_ENG_SYNC):
                    return
                key = ('c', tok[1])
            else:
                key = ('d', tok[1])
            if deps.get(key, 0) < tok[2]:
                deps[key] = tok[2]

        for r in reads:
            add(r.lw)
        for w in writes:
            add(w.lw)
            for tok in w.rd.values():
                add(tok)
        return deps

    def op(self, eng, fn, reads=(), writes=()):
        reads = [r for r in reads if r is not None]
        deps = self._deps(eng, reads, writes)
        rec = dict(fn=fn, deps=deps, sig=False, kind='c')
        self.ops[eng].append(rec)
        self.cops[eng].append(rec)
        tok = ('c', eng, len(self.cops[eng]))
        for r in reads:
            r.rd[('c', eng)] = tok
        for w in writes:
            w.lw = tok
            w.rd = {}
        return rec

    def dma(self, q, fn, owner, reads=(), writes=()):
        deps = self._deps(q, reads, writes)
        if owner.semid is None:
            if self.sem_pool:
                owner.semid, owner.cnt = self.sem_pool.pop()
            else:
                owner.semid, owner.cnt = len(self.semcnt), 0
                self.dsem[owner.semid] = self.es.enter_context(self.nc.semaphore(f'd{owner.semid}'))
        owner.cnt += 1
        self.semcnt[owner.semid] = owner.cnt
        tok = ('d', owner.semid, owner.cnt)
        rec = dict(fn=fn, deps=deps, kind='d', semid=owner.semid)
        self.ops[q].append(rec)
        for r in reads:
            r.rd[('d', owner.semid)] = tok
        for w in writes:
            w.lw = tok
            w.rd = {}
        return rec

    def barrier(self):
        deps = {}
        for e in ENGS:
            if self.cops[e]:
                deps[('c', e)] = len(self.cops[e])
        for sid, c in self.semcnt.items():
            deps[('d', sid)] = c
        for e in ENGS:
            d = {k: v for k, v in deps.items() if k != ('c', e)}
            self.ops[e].append(dict(fn=None, deps=d, kind='b'))
        for t in self.all_tiles:
            t.lw = None
            t.rd = {}

    def check_progress(self):
        pos = {e: 0 for e in ENGS}
        moved = True
        while moved:
            moved = False
            for e in ENGS:
                ops = self.ops[e]
                while pos[e] < len(ops):
                    rec = ops[pos[e]]
                    ok = True
                    for key, val in rec['deps'].items():
                        have = self.sim_c.get(key[1], 0) if key[0] == 'c' else self.sim_d.get(key[1], 0)
                        if have < val:
                            ok = False
                            break
                    if not ok:
                        break
                    if rec['kind'] == 'c':
                        self.sim_c[e] = self.sim_c.get(e, 0) + 1
                    elif rec['kind'] == 'd':
                        self.sim_d[rec['semid']] = self.sim_d.get(rec['semid'], 0) + 1
                    pos[e] += 1
                    moved = True
        stuck = {e: (pos[e], len(self.ops[e])) for e in ENGS if pos[e] < len(self.ops[e])}
        assert not stuck, f"DEADLOCK in recorded program: {stuck}"

    def flush(self):
        nc = self.nc
        self.check_progress()
        for e in ENGS:
            for rec in self.ops[e]:
                for key, val in rec['deps'].items():
                    if key[0] == 'c':
                        self.cops[key[1]][val - 1]['sig'] = True
        for e in ENGS:
            for rec in self.cops[e][self.nassigned[e]:]:
                if rec['sig']:
                    self.nsig[e] += 1
                rec['signo'] = self.nsig[e]
            self.nassigned[e] = len(self.cops[e])
        engsem, dsem = self.engsem, self.dsem

        def run(ename, eng):
            waited = self.waited[ename]
            for rec in self.ops[ename]:
                for key, val in rec['deps'].items():
                    if key[0] == 'c':
                        sem = engsem[key[1]]
                        v = self.cops[key[1]][val - 1]['signo']
                    else:
                        sem = dsem[key[1]]
                        v = 16 * val
                    if waited.get(key, 0) >= v:
                        continue
                    waited[key] = v
                    eng.wait_ge(sem, v)
                if rec['fn'] is None:
                    continue
                ins = rec['fn'](eng)
                if rec['kind'] == 'd':
                    ins.then_inc(dsem[rec['semid']], 16)
                elif rec['sig']:
                    ins.then_inc(engsem[ename], 1)
            self.ops[ename] = []

        with nc.Block() as block:
            @block.sync
            def _(e):
                run('sync', e)

            @block.scalar
            def _(e):
                run('act', e)

            @block.vector
            def _(e):
                run('dve', e)

            @block.gpsimd
            def _(e):
                run('pool', e)

            @block.tensor
            def _(e):
                run('pe', e)


class Ring:
    def __init__(self, tiles):
        self.tiles, self.i = tiles, 0

    def next(self):
        t = self.tiles[self.i % len(self.tiles)]
        self.i += 1
        return t


def _t(*vs):
    return [v.tile for v in vs if isinstance(v, V)]


def _a(v):
    return v.ap if isinstance(v, V) else v


def mm(P, out, lhsT, rhs, start=True, stop=True):
    P.op('pe', lambda e: e.matmul(out.ap, lhsT.ap, rhs.ap, start=start, stop=stop),
         reads=_t(lhsT, rhs), writes=_t(out))


def transpose(P, out, in_, ident):
    P.op('pe', lambda e: e.transpose(out.ap, in_.ap, ident.ap), reads=_t(in_, ident), writes=_t(out))


def act(P, out, in_, func, scale=1.0, bias=None):
    if bias is None:
        P.op('act', lambda e: e.activation(out=out.ap, in_=in_.ap, func=func, scale=scale),
             reads=_t(in_), writes=_t(out))
    else:
        P.op('act', lambda e: e.activation(out=out.ap, in_=in_.ap, func=func, scale=scale, bias=_a(bias)),
             reads=_t(in_, bias), writes=_t(out))


def tt(P, out, in0, in1, op, eng='dve'):
    P.op(eng, lambda e: e.tensor_tensor(out=out.ap, in0=in0.ap, in1=in1.ap, op=op),
         reads=_t(in0, in1), writes=_t(out))


def ts(P, out, in0, s1, op0, s2=None, op1=None, eng='dve'):
    if op1 is None:
        P.op(eng, lambda e: e.tensor_scalar(out=out.ap, in0=in0.ap, scalar1=_a(s1), scalar2=None, op0=op0),
             reads=_t(in0, s1), writes=_t(out))
    else:
        P.op(eng, lambda e: e.tensor_scalar(out=out.ap, in0=in0.ap, scalar1=_a(s1), scalar2=_a(s2),
                                            op0=op0, op1=op1),
             reads=_t(in0, s1, s2), writes=_t(out))


def stt(P, out, in0, scalar, in1, op0, op1):
    P.op('dve', lambda e: e.scalar_tensor_tensor(out=out.ap, in0=in0.ap, scalar=_a(scalar), in1=in1.ap,
                                                 op0=op0, op1=op1),
         reads=_t(in0, scalar, in1), writes=_t(out))


def recip(P, out, in_):
    P.op('dve', lambda e: e.reciprocal(out=out.ap, in_=in_.ap), reads=_t(in_), writes=_t(out))


def copy(P, out, in_, eng='dve'):
    if eng == 'act':
        P.op('act', lambda e: e.activation(out=out.ap, in_=in_.ap, func=AF.Copy), reads=_t(in_), writes=_t(out))
    else:
        P.op(eng, lambda e: e.tensor_copy(out=out.ap, in_=in_.ap), reads=_t(in_), writes=_t(out))


def reduce(P, out, in_, op, axis=AX.X):
    P.op('dve', lambda e: e.tensor_reduce(out=out.ap, in_=in_.ap, axis=axis, op=op), reads=_t(in_), writes=_t(out))


def memset(P, out, val, eng='dve'):
    P.op(eng, lambda e: e.memset(out.ap, val), writes=_t(out))


def load(P, out, src, q='sync'):
    P.dma(q, lambda e: e.dma_start(out=out.ap, in_=src), owner=out.tile, writes=[out.tile])


def store(P, dst, in_, q='sync'):
    P.dma(q, lambda e: e.dma_start(out=dst, in_=in_.ap), owner=in_.tile, reads=[in_.tile])


BLOCKS = [(0, 512, 0), (512, 512, 0), (1024, 512, 0), (1536, 512, 0), (2048, 64, 1)]
GROUPS = [(0, 512, 'aq'), (512, 512, 'akv'), (1024, 704, 'mla'), (1728, 512, 'wq'), (2240, 512, 'wkv'),
          (2752, 512, 'nq'), (3264, 512, 'nk'), (3776, 512, 'nv')]
KROWS, VCOLS, QROWS = 1792, 1536, 2304
G_QA, G_KA, G_QW, G_KW, G_QN, G_KN, G_QM0, G_QM1, G_KM0, G_KM1, G_CQ, G_CKV = 0, 1, 2, 3, 4, 5, 6, 7, 8, 9, 10, 13
NGAIN = 15
MD_SH1, MD_A1, MD_G1, MD_SH2, MD_A2, MD_G2 = 0, 1, 2, 3, 4, 5


class Consts:
    pass


def load_consts(P, sc, Dm):
    C = Consts()
    C.ones_bf = P.sb(sc, 'ones_bf', [128, 128], BF16)
    memset(P, C.ones_bf[:, :], 1.0)
    C.ones_f = P.sb(sc, 'ones_f', [128, 128], F32)
    memset(P, C.ones_f[:, :], 1.0)
    C.eps = P.sb(sc, 'eps', [128, 1], F32)
    memset(P, C.eps[:, :], EPS)
    C.cf = P.sb(sc, 'cf', [128, 128], F32)
    load(P, C.cf[:, :], Dm['cf'])
    C.cb = P.sb(sc, 'cb', [128, 192], BF16)
    load(P, C.cb[:, :], Dm['cb'])
    return C


def norm_block(P, S, C, x, nt, Acol, shcol, who, dst, rs_out=None):
    pss = S.pss.next()
    for kc in range(KC):
        sq = S.sq.next()
        act(P, sq[:, :nt], x[:, kc, :nt], AF.Square)
        mm(P, pss[:, :nt], C.ones_bf[:, :], sq[:, :nt], start=(kc == 0), stop=(kc == KC - 1))
    sd = S.sd.next()
    act(P, sd[:, :nt], pss[:, :nt], AF.Sqrt, scale=1.0 / D, bias=C.eps[:, 0:1])
    rs = rs_out if rs_out is not None else S.rs.next()
    recip(P, rs[:, :nt], sd[:, :nt])
    for kc in range(KC):
        tmp = S.tmp.next()
        stt(P, tmp[:, :nt], x[:, kc, :nt], Acol(kc, who), rs[:, :nt], ALU.mult, ALU.mult)
        act(P, dst(kc), tmp[:, :nt], AF.Identity, bias=shcol(kc, who))
    return rs


def xload(P, dst_tile, src, nt, t0):
    for kh in range(4):
        load(P, dst_tile[:, 4 * kh:4 * kh + 4, :nt], src[:, 4 * kh:4 * kh + 4, t0:t0 + nt])


def wcast(P, dst_tile, src, nk, ncol, c0):
    step = max(1, nk // 4)
    for k0 in range(0, nk, step):
        k1 = min(nk, k0 + step)
        trk = getattr(dst_tile, 'tile', dst_tile)
        P.dma('pool', (lambda o, i: (lambda e: e.dma_start(out=o, in_=i)))(dst_tile.t[:, k0:k1, :ncol], src[:, k0:k1, c0:c0 + ncol]),
              owner=trk, writes=[trk])


def phase0(P, l, Dm, C):
    with P.scope() as sc:
        cs = P.sb(sc, 'cs', [128, KC, 2], F32)
        load(P, cs[:, :, :], Dm['csT'])
        act(P, cs[:, :, :], cs[:, :, :], AF.Silu)
        bm = P.sb(sc, 'bm', [128, 96], F32)
        load(P, bm[:, :], Dm['bmodT'][l])
        nrm = P.sb(sc, 'nrm', [128, 2, KC], F32)
        load(P, nrm[:, :, :], Dm['normT'][l])
        modT = P.sb(sc, 'modT', [128, 96, 2], F32)
        md = P.sb(sc, 'md', [128, 6, KC, 2], F32)
        csb = P.sb(sc, 'csb', [128, KC, 2], BF16)
        copy(P, csb[:, :, :], cs[:, :, :])
        wm = P.ring(sc, 'wm', 3, [128, KC, 512], BF16)
        psm = P.ring(sc, 'psm', 2, [128, 4, 2], F32, psum=True)
        for pc in range(24):
            w = wm.next()
            wcast(P, w, Dm['w_mod'][l, pc].rearrange("p (k c) -> p k c", k=KC), KC, 512, 0)
            pm = psm.next()
            for j in range(4):
                for kc in range(KC):
                    mm(P, pm[:, j, :], w[:, kc, j * 128:(j + 1) * 128], csb[:, kc, :], start=(kc == 0), stop=(kc == KC - 1))
            for n in range(2):
                tt(P, modT[:, pc * 4:pc * 4 + 4, n], pm[:, :, n], bm[:, pc * 4:pc * 4 + 4], ALU.add)
        for n in range(2):
            copy(P, md[:, MD_SH1, :, n], modT[:, 0:16, n])
            stt(P, md[:, MD_A1, :, n], modT[:, 16:32, n], 1.0, nrm[:, 0, :], ALU.add, ALU.mult)
            copy(P, md[:, MD_G1, :, n], modT[:, 32:48, n])
            copy(P, md[:, MD_SH2, :, n], modT[:, 48:64, n])
            stt(P, md[:, MD_A2, :, n], modT[:, 64:80, n], 1.0, nrm[:, 1, :], ALU.add, ALU.mult)
            copy(P, md[:, MD_G2, :, n], modT[:, 80:96, n])
        store(P, Dm['mod_d'][l], md[:, :, :, :])


def qk_unit_gen(P, S, C, pieces, Dtot, nt, t0):
    for pc in pieces:
        if 'proj' in pc:
            pc['src'] = pc['proj']()
    yield
    pss = S.pss.next()
    n = len(pieces)
    for i, pc in enumerate(pieces):
        dp = pc['dp']
        if pc.get('sq') is None:
            sq = S.sq.next()
            act(P, sq[:dp, :nt], pc['src'], AF.Square)
            sqv = sq[:dp, :nt]
        else:
            sqv = pc['sq']
        mm(P, pss[:, :nt], C.ones_bf[:dp, :], sqv, start=(i == 0), stop=(i == n - 1))
    yield
    sd = S.sd.next()
    act(P, sd[:, :nt], pss[:, :nt], AF.Sqrt, scale=1.0 / Dtot, bias=C.eps[:, 0:1])
    rs = S.rs.next()
    recip(P, rs[:, :nt], sd[:, :nt])
    ropes = []
    for pc in pieces:
        dp = pc['dp']
        kind, dst = pc['dst']
        if pc.get('rope') is None:
            if kind == 's':
                stt(P, dst, pc['src'], pc['gain'], rs[:dp, :nt], ALU.mult, ALU.mult)
            else:
                ob = S.ob.next()
                stt(P, ob[:dp, :nt], pc['src'], pc['gain'], rs[:dp, :nt], ALU.mult, ALU.mult)
                store(P, dst, ob[:dp, :nt])
        else:
            R, cos, sin = S.rope[pc['rope']]
            xn = S.xn.next()
            stt(P, xn[:dp, :nt], pc['src'], pc['gain'], rs[:dp, :nt], ALU.mult, ALU.mult)
            psr = S.psr.next()
            mm(P, psr[:dp, :nt], R, xn[:dp, :nt])
            ropes.append((pc, xn, psr, cos, sin))
    yield
    for (pc, xn, psr, cos, sin) in ropes:
        dp = pc['dp']
        kind, dst = pc['dst']
        t1 = S.t1.next()
        tt(P, t1[:dp, :nt], xn[:dp, :nt], cos[:dp, t0:t0 + nt], ALU.mult, eng='pool')
        t2 = S.t2.next()
        tt(P, t2[:dp, :nt], psr[:dp, :nt], sin[:dp, t0:t0 + nt], ALU.mult)
        ob = S.ob.next()
        tt(P, ob[:dp, :nt], t1[:dp, :nt], t2[:dp, :nt], ALU.add)
        store(P, dst, ob[:dp, :nt])


def qk_unit(P, S, C, pieces, Dtot, nt, t0):
    for _ in qk_unit_gen(P, S, C, pieces, Dtot, nt, t0):
        pass


class Pipe:
    def __init__(self):
        self.q = []

    def _advance(self):
        for g in reversed(list(self.q)):
            try:
                next(g)
            except StopIteration:
                self.q.remove(g)

    def push(self, g):
        next(g)
        self._advance()
        self.q.append(g)

    def flush(self):
        while self.q:
            self._advance()


def phase1(P, l, Dm, C, xin):
    with P.scope() as sc:
        hT = P.sb(sc, 'hT', [128, KC, NT], BF16)
        md = P.sb(sc, 'md1', [128, 6, KC, 2], F32)
        load(P, md[:, :, :, :], Dm['mod_d'][l])
        S = Consts()
        S.sq = P.ring(sc, 'sq', 3, [128, 512], BF16)
        S.sd = P.ring(sc, 'sd', 2, [128, 512], F32)
        S.rs = P.ring(sc, 'rs', 2, [128, 512], F32)
        S.pss = P.ring(sc, 'pss', 2, [128, 512], F32, psum=True)
        with P.scope() as s2:
            xb = P.ring(s2, 'xb', 2, [128, KC, 512], F32)
            S.tmp = P.ring(s2, 'tmp', 2, [128, 512], F32)
            for (t0, nt, who) in BLOCKS:
                x = xb.next()
                xload(P, x, xin, nt, t0)
                norm_block(P, S, C, x, nt, lambda kc, w: md[:, MD_A1, kc, w:w + 1], lambda kc, w: md[:, MD_SH1, kc, w:w + 1],
                           who, lambda kc: hT[:, kc, t0:t0 + nt])
                for kh in range(4):
                    store(P, Dm['hT_d'][:, 4 * kh:4 * kh + 4, t0:t0 + nt], hT[:, 4 * kh:4 * kh + 4, t0:t0 + nt])
        with P.scope() as s2:
            G = P.sb(s2, 'gains', [128, NGAIN], F32)
            load(P, G[:, :], Dm['gains'][l])
            rp = P.sb(s2, 'ropet', [128, 4, NT], F32)
            for i in range(4):
                load(P, rp[:, i, :], Dm['rope'][:, i, :])
            S.rope = {'r128': (C.cb[:, 0:128], rp.t[:, 0, :], rp.t[:, 1, :]), 'r64': (C.cb[:64, 128:192], rp.t[:, 2, :], rp.t[:, 3, :])}
            S.rope = {k: (v[0], _RV(rp, v[1]), _RV(rp, v[2])) for k, v in S.rope.items()}
            wuq = P.sb(s2, 'wuq', [128, 3, 768], BF16)
            wcast(P, wuq, Dm['mla_w_uq'][l].rearrange("(kc p) c -> p kc c", p=128), 3, 768, 0)
            wukv = P.sb(s2, 'wukv', [128, 2, 1024], BF16)
            wcast(P, wukv, Dm['mla_w_ukv_p'][l].rearrange("(kc p) c -> p kc c", p=128), 2, 1024, 0)
            wr = P.ring(s2, 'win', 2, [128, KC, 704], BF16)
            S.xn = P.ring(s2, 'xn', 2, [128, 512], BF16)
            S.t1 = P.ring(s2, 't1', 2, [128, 512], F32)
            S.t2 = P.ring(s2, 't2', 2, [128, 512], F32)
            S.ob = P.ring(s2, 'ob', 3, [128, 512], BF16)
            S.psr = P.ring(s2, 'psr', 2, [128, 512], F32, psum=True)
            proj = P.ring(s2, 'proj', 4, [128, 512], F32, psum=True)
            cqn = P.sb(s2, 'cqn', [128, 3, 512], BF16)
            ckvn = P.sb(s2, 'ckvn', [128, 2, 512], BF16)
            krf = P.sb(s2, 'krf', [64, 512], F32)
            krsq = P.sb(s2, 'krsq', [64, 512], BF16)
            vb = P.ring(s2, 'vb', 2, [128, 512], BF16)
            win_l = Dm['w_in'][l]
            qT, kO, vO = Dm['qT_d'], Dm['k_own'], Dm['v_own']

            def proj_fm(w, col, m, t0, nt, nk=KC, act_=None):
                ps = proj.next()
                for kc in range(nk):
                    a = hT[:, kc, t0:t0 + nt] if act_ is None else act_[:, kc, :nt]
                    mm(P, ps[:m, :nt], w[:, kc, col:col + m], a, start=(kc == 0), stop=(kc == nk - 1))
                return ps

            def proj_tm(w, col, ncols, t0, nt, vcol, nk=KC, act_=None):
                for sub in range((nt + 127) // 128):
                    ntk = min(128, nt - 128 * sub)
                    ps = proj.next()
                    for kc in range(nk):
                        a = hT[:, kc, t0 + 128 * sub:t0 + 128 * sub + ntk] if act_ is None else act_[:, kc, 128 * sub:128 * sub + ntk]
                        mm(P, ps[:ntk, :ncols], a, w[:, kc, col:col + ncols], start=(kc == 0), stop=(kc == nk - 1))
                    v = vb.next()
                    copy(P, v[:ntk, :ncols], ps[:ntk, :ncols], eng='act')
                    store(P, vO[t0 + 128 * sub:t0 + 128 * sub + ntk, vcol:vcol + ncols], v[:ntk, :ncols])

            pipe = Pipe()

            def heads(w, nh, col0, gcol, rope, dst, row0, t0, nt):
                for h in range(nh):
                    pj = (lambda c_=col0 + 128 * h: proj_fm(w, c_, 128, t0, nt)[:, :nt])
                    pipe.push(qk_unit_gen(P, S, C, [dict(proj=pj, dp=128, gain=G[:, gcol:gcol + 1], rope=rope,
                                                         dst=('d', dst[row0 + 128 * h:row0 + 128 * h + 128, t0:t0 + nt]))], 128, nt, t0))

            wts = {}

            def prefetch(gi):
                if gi < len(GROUPS) and gi not in wts:
                    wts[gi] = wr.next()
                    c0_, nc_ = GROUPS[gi][0], GROUPS[gi][1]
                    wcast(P, wts[gi], win_l[:, KC * c0_:KC * (c0_ + nc_)].rearrange("p (k c) -> p k c", k=KC), KC, nc_, 0)

            prefetch(0)
            for gi, (c0, ncol, kind) in enumerate(GROUPS):
                prefetch(gi + 1)
                w = wts.pop(gi)
                if kind in ('aq', 'akv', 'wq', 'wkv', 'nq', 'nk'):
                    for (t0, nt, who) in BLOCKS:
                        if kind == 'aq':
                            heads(w, 4, 0, G_QA, 'r128', qT, 0, t0, nt)
                        elif kind == 'akv':
                            heads(w, 2, 0, G_KA, 'r128', kO, 0, t0, nt)
                        elif kind == 'wq':
                            heads(w, 4, 0, G_QW, 'r128', qT, 1280, t0, nt)
                        elif kind == 'wkv':
                            heads(w, 2, 0, G_KW, 'r128', kO, 1024, t0, nt)
                        elif kind == 'nq':
                            heads(w, 4, 0, G_QN, None, qT, 1792, t0, nt)
                        elif kind == 'nk':
                            heads(w, 4, 0, G_KN, None, kO, 1280, t0, nt)
                    pipe.flush()
                for (t0, nt, who) in BLOCKS:
                    if kind == 'akv':
                        proj_tm(w, 256, 256, t0, nt, 0)
                    elif kind == 'wkv':
                        proj_tm(w, 256, 256, t0, nt, 768)
                    elif kind == 'nv':
                        proj_tm(w, 0, 512, t0, nt, 1024)
                    elif kind == 'mla':
                        pcs = [proj_fm(w, 128 * i, 128, t0, nt) for i in range(3)]
                        qk_unit(P, S, C, [dict(src=pcs[i][:, :nt], dp=128, gain=G[:, G_CQ + i:G_CQ + i + 1],
                                               dst=('s', cqn[:, i, :nt])) for i in range(3)], 384, nt, t0)
                        pcs = [proj_fm(w, 384 + 128 * i, 128, t0, nt) for i in range(2)]
                        qk_unit(P, S, C, [dict(src=pcs[i][:, :nt], dp=128, gain=G[:, G_CKV + i:G_CKV + i + 1],
                                               dst=('s', ckvn[:, i, :nt])) for i in range(2)], 256, nt, t0)
                        pk = proj_fm(w, 640, 64, t0, nt)
                        copy(P, krf[:, :nt], pk[:64, :nt])
                        act(P, krsq[:, :nt], pk[:64, :nt], AF.Square)
                        for h in range(4):
                            pn = proj_fm(wuq, 192 * h, 128, t0, nt, nk=3, act_=cqn)
                            pr = proj_fm(wuq, 192 * h + 128, 64, t0, nt, nk=3, act_=cqn)
                            r0 = 512 + 192 * h
                            qk_unit(P, S, C, [
                                dict(src=pn[:, :nt], dp=128, gain=G[:, G_QM0:G_QM0 + 1], dst=('d', qT[r0:r0 + 128, t0:t0 + nt])),
                                dict(src=pr[:64, :nt], dp=64, gain=G[:64, G_QM1:G_QM1 + 1], rope='r64',
                                     dst=('d', qT[r0 + 128:r0 + 192, t0:t0 + nt]))], 192, nt, t0)
                        for h in range(4):
                            pn = proj_fm(wukv, 128 * h, 128, t0, nt, nk=2, act_=ckvn)
                            r0 = 256 + 192 * h
                            qk_unit(P, S, C, [
                                dict(src=pn[:, :nt], dp=128, gain=G[:, G_KM0:G_KM0 + 1], dst=('d', kO[r0:r0 + 128, t0:t0 + nt])),
                                dict(src=krf[:, :nt], dp=64, gain=G[:64, G_KM1:G_KM1 + 1], rope='r64', sq=krsq[:, :nt],
                                     dst=('d', kO[r0 + 128:r0 + 192, t0:t0 + nt]))], 192, nt, t0)
                        proj_tm(wukv, 512, 512, t0, nt, 256, nk=2, act_=ckvn)


class _RV:
    def __init__(self, tile, ap):
        self.tile, self.ap = tile, ap

    def __getitem__(self, idx):
        return V(self.tile, self.ap[idx])


def attn_block(P, S, C, nt, steps, out_dram, scale, sink=None, LA=3):
    o_ps = S.ops_.next()
    d_ps = S.dps.next()
    n = len(steps)
    sts = {}

    def issue_st(i):
        st = S.st.next()
        kp = steps[i][0]
        for j, (kT, qv) in enumerate(kp):
            mm(P, st[:, :nt], kT, qv, start=(j == 0), stop=(j == len(kp) - 1))
        sts[i] = st

    tbs = {}
    for i, (kp, vch, bias) in enumerate(steps):
        if bias is not None:
            tbs[i] = S.tb.next()
            load(P, tbs[i][:, :nt], bias)
    for i in range(min(LA, n)):
        issue_st(i)
    for i, (kp, vch, bias) in enumerate(steps):
        if i + LA < n:
            issue_st(i + LA)
        st = sts.pop(i)
        pt = S.pt.next()
        if bias is None:
            act(P, pt[:, :nt], st[:, :nt], AF.Exp, scale=scale)
        else:
            tb = tbs[i]
            sf = S.sf.next()
            stt(P, sf[:, :nt], st[:, :nt], scale, tb[:, :nt], ALU.mult, ALU.add)
            act(P, pt[:, :nt], sf[:, :nt], AF.Exp)
        mm(P, o_ps[:, :nt], vch, pt[:, :nt], start=(i == 0), stop=(i == n - 1))
        mm(P, d_ps[:, :nt], C.ones_bf[:, :], pt[:, :nt], start=(i == 0), stop=(i == n - 1))
    rd = S.rd.next()
    if sink is not None:
        ts(P, rd[:, :nt], d_ps[:, :nt], sink, ALU.add)
        recip(P, rd[:, :nt], rd[:, :nt])
    else:
        recip(P, rd[:, :nt], d_ps[:, :nt])
    ob = S.ob.next()
    tt(P, ob[:, :nt], o_ps[:, :nt], rd[:, :nt], ALU.mult)
    store(P, out_dram, ob[:, :nt])


def convert_weights(P, l, Dm):
    cv = Tile(None, 'cvt')
    cv.base = 0
    r = lambda ap: ap.rearrange("p (a b) -> p a b", b=2048)

    jobs = []

    def cvd(dst, src):
        def job(o=r(dst), i=r(src)):
            rec = P.dma('pool', (lambda e: e.dma_start(out=o, in_=i)), owner=cv)
            cv.base += 1
            if cv.base > 2:
                rec['deps'][('d', cv.semid)] = cv.cnt - 2
        jobs.append(job)

    for jg in range(4):
        for i in range(4):
            cvd(Dm['wb_gate'][i, jg], Dm['w_gate'][l, i, jg])
            cvd(Dm['wb_branch'][i, jg], Dm['w_branch'][l, i, jg])
    for jg in range(4):
        cvd(Dm['wb_out'][jg], Dm['w_out'][l, jg])
    for e in range(16):
        cvd(Dm['wb_w1'][e], Dm['moe_w1'][l, e])
        cvd(Dm['wb_w3'][e], Dm['moe_w3'][l, e])
        cvd(Dm['wb_w2'][e], Dm['moe_w2'][l, e])
    return jobs


def phase2(P, l, Dm, C, need_ctx):
    blocks = BLOCKS if need_ctx else BLOCKS[:4]
    kg, vg, qT, oT = Dm['kg'], Dm['vg'], Dm['qT_d'], Dm['oT_d']
    kown, vown, khalo, vhalo = Dm['k_own'], Dm['v_own'], Dm['khalo'], Dm['vhalo']
    with P.scope() as sc:
        cjobs = convert_weights(P, l, Dm)
        S = Consts()
        S.st = P.ring(sc, 'st', 4, [128, 512], F32, psum=True)
        S.ops_ = P.ring(sc, 'ops', 2, [128, 512], F32, psum=True)
        S.dps = P.ring(sc, 'dps', 2, [128, 512], F32, psum=True)
        S.pt = P.ring(sc, 'pt', 6, [128, 512], BF16)
        S.tb = P.ring(sc, 'tb', 10, [128, 512], F32)
        S.sf = P.ring(sc, 'sf', 3, [128, 512], F32)
        S.rd = P.ring(sc, 'rd', 2, [128, 512], F32)
        S.ob = P.ring(sc, 'ob2', 3, [128, 512], BF16)
        vr = lambda ap: ap.rearrange("(c p) d -> p c d", p=128)
        with P.scope() as s2:
            kn = P.ring(s2, 'kn', 2, [128, 8448], BF16)
            kr = P.ring(s2, 'kr', 2, [64, 8448], BF16)
            vv = P.ring(s2, 'vv', 2, [128, 66, 128], BF16)
            qn = P.ring(s2, 'qn', 2, [128, NT], BF16)
            qr = P.ring(s2, 'qr', 2, [64, NT], BF16)

            def load_k(kt, row0, nr):
                for r in range(4):
                    load(P, kt[:nr, 256 + 2048 * r:256 + 2048 * (r + 1)], kg[r, row0:row0 + nr, 0:2048])
                    load(P, kt[:nr, 64 * r:64 * r + 64], kg[r, row0:row0 + nr, 2048:2112])

            def load_v(vt, col0):
                for r in range(4):
                    load(P, vt[:, 2 + 16 * r:2 + 16 * r + 16, :], vr(vg[r, 0:2048, col0:col0 + 128]))
                    load(P, vt[64 * (r % 2):64 * (r % 2) + 64, r // 2, :], vg[r, 2048:2112, col0:col0 + 128])

            units = [('A', 0), ('A', 1), ('M', 0), ('M', 1), ('M', 2), ('M', 3)]
            for ui, (mx, u) in enumerate(units):
                k1 = kn.next()
                v1 = vv.next()
                k2 = None
                if mx == 'A':
                    load_k(k1, 128 * u, 128)
                    load_v(v1, 128 * u)
                    qheads = [2 * u, 2 * u + 1]
                    scale = 128 ** -0.5
                else:
                    k2 = kr.next()
                    load_k(k1, 256 + 192 * u, 128)
                    load_k(k2, 256 + 192 * u + 128, 64)
                    load_v(v1, 256 + 128 * u)
                    qheads = [u]
                    scale = 192 ** -0.5
                if ui == 0:
                    for job in cjobs:
                        job()
                for h in qheads:
                    q1 = qn.next()
                    q2 = None
                    if mx == 'A':
                        load(P, q1[:, :], qT[128 * h:128 * h + 128, :])
                        oh = h
                    else:
                        q2 = qr.next()
                        load(P, q1[:, :], qT[512 + 192 * h:512 + 192 * h + 128, :])
                        load(P, q2[:, :], qT[512 + 192 * h + 128:512 + 192 * h + 192, :])
                        oh = 4 + h
                    for (t0, nt, who) in blocks:
                        steps = []
                        for c in (range(66) if who == 0 else range(2)):
                            kp = [(k1[:, 128 * c:128 * c + 128], q1[:, t0:t0 + nt])]
                            if k2 is not None:
                                kp.append((k2[:, 128 * c:128 * c + 128], q2[:, t0:t0 + nt]))
                            steps.append((kp, v1[:, c, :], None))
                        attn_block(P, S, C, nt, steps, oT[128 * oh:128 * oh + 128, t0:t0 + nt], scale)
        with P.scope() as s2:
            kw = P.ring(s2, 'kw', 2, [128, 2816], BF16)
            vw = P.ring(s2, 'vw', 2, [128, 22, 128], BF16)
            qw = P.ring(s2, 'qw', 2, [128, NT], BF16)
            esink = P.sb(s2, 'esink', [128, 4], F32)
            load(P, esink[:, :], Dm['sinkT'][l])
            act(P, esink[:, :], esink[:, :], AF.Exp)
            scale = 128 ** -0.5

            def load_band(k1, v1, krow, vcol):
                hr, hc = krow - 1024, vcol - 768
                for r in range(4):
                    load(P, k1[:, 64 * r:64 * r + 64], kg[r, krow:krow + 128, 2048:2112], q='pool')
                    load(P, v1[64 * (r % 2):64 * (r % 2) + 64, r // 2, :], vg[r, 2048:2112, vcol:vcol + 128], q='pool')
                load(P, k1[:, 256:512], khalo[hr:hr + 128, 0:256], q='pool')
                load(P, k1[:, 512:2560], kown[krow:krow + 128, 0:2048], q='pool')
                load(P, k1[:, 2560:2816], khalo[hr:hr + 128, 256:512], q='pool')
                load(P, v1[:, 2:4, :], vr(vhalo[0:256, hc:hc + 128]), q='pool')
                load(P, v1[:, 4:20, :], vr(vown[0:2048, vcol:vcol + 128]), q='pool')
                load(P, v1[:, 20:22, :], vr(vhalo[256:512, hc:hc + 128]), q='pool')

            def band_head(k1, v1, qrow, oh, nrel, koff, tab, sink):
                q1 = qw.next()
                load(P, q1[:, :], qT[qrow:qrow + 128, :], q='pool')
                for (t0, nt, who) in blocks:
                    steps = [([(k1[:, 128 * c:128 * c + 128], q1[:, t0:t0 + nt])], v1[:, c, :], None) for c in range(2)]
                    if who == 0:
                        Q = t0 // 512
                        for rel in range(nrel):
                            c = 2 + (koff + 512 * Q) // 128 + rel
                            steps.append(([(k1[:, 128 * c:128 * c + 128], q1[:, t0:t0 + nt])], v1[:, c, :], tab(Q, rel)))
                    attn_block(P, S, C, nt, steps, oT[128 * oh:128 * oh + 128, t0:t0 + nt], scale, sink=sink)

            for kvh in range(2):
                k1, v1 = kw.next(), vw.next()
                load_band(k1, v1, 1024 + 128 * kvh, 768 + 128 * kvh)
                for gi in range(2):
                    h = 2 * kvh + gi
                    band_head(k1, v1, 1280 + 128 * h, 8 + h, 6, 128, lambda Q, rel: Dm['wtab'][Q, rel], esink[:, h:h + 1])
            for h in range(4):
                k1, v1 = kw.next(), vw.next()
                load_band(k1, v1, 1280 + 128 * h, 1024 + 128 * h)
                band_head(k1, v1, 1792 + 128 * h, 12 + h, 8, 0, (lambda hh: (lambda Q, rel: Dm['ntab'][l, hh, Q, rel]))(h), None)


BIG = 1.0e4


def routing(P, S, C, lg, ntk, brb, comb):
    r = S.rt
    sc_, bi, t3, mb, mb2, e1, e2, w = (r[i] for i in range(8))
    g1, g2, gs, gmask, pen = (S.rs4[i] for i in range(5))
    gm, m1, m2, ws = (S.rs1[i] for i in range(4))
    act(P, sc_[:ntk, :], lg, AF.Sigmoid)
    tt(P, bi[:ntk, :], sc_[:ntk, :], brb[:ntk, :], ALU.add)
    v3 = lambda t: V(t, t.t[:ntk, :].rearrange("p (g e) -> p g e", g=4))
    reduce(P, g1[:ntk, :], v3(bi), ALU.max)
    for g in range(4):
        ts(P, t3[:ntk, 4 * g:4 * g + 4], bi[:ntk, 4 * g:4 * g + 4], g1[:ntk, g:g + 1], ALU.is_equal, -BIG, ALU.mult)
    tt(P, t3[:ntk, :], t3[:ntk, :], bi[:ntk, :], ALU.add)
    reduce(P, g2[:ntk, :], v3(t3), ALU.max)
    tt(P, gs[:ntk, :], g1[:ntk, :], g2[:ntk, :], ALU.add)
    reduce(P, gm[:ntk, :], gs[:ntk, :], ALU.max)
    ts(P, gmask[:ntk, :], gs[:ntk, :], gm[:ntk, 0:1], ALU.is_ge)
    ts(P, pen[:ntk, :], gmask[:ntk, :], 1.0, ALU.subtract, BIG, ALU.mult)
    for g in range(4):
        ts(P, mb[:ntk, 4 * g:4 * g + 4], bi[:ntk, 4 * g:4 * g + 4], gmask[:ntk, g:g + 1], ALU.mult, pen[:ntk, g:g + 1], ALU.add)
    reduce(P, m1[:ntk, :], mb[:ntk, :], ALU.max)
    ts(P, e1[:ntk, :], mb[:ntk, :], m1[:ntk, 0:1], ALU.is_equal)
    stt(P, mb2[:ntk, :], e1[:ntk, :], -BIG, mb[:ntk, :], ALU.mult, ALU.add)
    reduce(P, m2[:ntk, :], mb2[:ntk, :], ALU.max)
    ts(P, e2[:ntk, :], mb2[:ntk, :], m2[:ntk, 0:1], ALU.is_equal)
    tt(P, e1[:ntk, :], e1[:ntk, :], e2[:ntk, :], ALU.add)
    tt(P, w[:ntk, :], e1[:ntk, :], sc_[:ntk, :], ALU.mult)
    reduce(P, ws[:ntk, :], w[:ntk, :], ALU.add)
    recip(P, ws[:ntk, :], ws[:ntk, :])
    ts(P, comb, w[:ntk, :], ws[:ntk, 0:1], ALU.mult)


def phase3(P, l, Dm, C, xin, xout, last):
    blocks = BLOCKS[:4] if last else BLOCKS
    with P.scope() as sc:
        md = P.sb(sc, 'md3', [128, 6, KC, 2], F32)
        load(P, md[:, :, :, :], Dm['mod_d'][l])
        bg = P.sb(sc, 'bg', [128, 4, KC], F32)
        load(P, bg[:, :, :], Dm['bgateT'][l])
        wrt = P.sb(sc, 'wrt', [128, KC, 16], F32)
        load(P, wrt[:, :, :], Dm['wrT'])
        brb = P.sb(sc, 'brb', [128, 16], F32)
        load(P, brb[:, :], Dm['brb'])
        wr2 = [P.sb(sc, f'wr2{n}', [128, KC, 16], F32) for n in range(2)]
        cbr = [P.sb(sc, f'cbr{n}', [128, 16], F32) for n in range(2)]
        S = Consts()
        S.sq = P.ring(sc, 'sq3', 2, [128, 512], BF16)
        S.sd = P.ring(sc, 'sd3', 1, [128, 512], F32)
        S.rs = P.ring(sc, 'rs3', 1, [128, 512], F32)
        S.tmp = P.ring(sc, 'tmp3', 2, [128, 512], F32)
        S.pss = P.ring(sc, 'pss3', 1, [128, 512], F32, psum=True)
        psA = P.ring(sc, 'psA', 2, [128, 512], F32, psum=True)
        psB = P.ring(sc, 'psB', 2, [128, 512], F32, psum=True)
        psC = S.pss
        S.rt = [P.sb(sc, f'rt{i}', [128, 16], F32) for i in range(8)]
        S.rs4 = [P.sb(sc, f'rq{i}', [128, 4], F32) for i in range(5)]
        S.rs1 = [P.sb(sc, f'ro{i}', [128, 1], F32) for i in range(4)]
        shb = S.tmp
        for n in range(2):
            for kc in range(KC):
                ts(P, wr2[n][:, kc, :], wrt[:, kc, :], md[:, MD_A2, kc, n:n + 1], ALU.mult)
            pc = psC.next()
            for kc in range(KC):
                sb_ = shb.next()
                ts(P, sb_[:, 0:128], C.ones_f[:, :], md[:, MD_SH2, kc, n:n + 1], ALU.mult)
                mm(P, pc[:, 0:16], sb_[:, 0:128], wrt[:, kc, :], start=(kc == 0), stop=(kc == KC - 1))
            copy(P, cbr[n][:, :], pc[:, 0:16])
        xb = P.sb(sc, 'xb3', [128, KC, 512], F32)
        hb = P.sb(sc, 'hb3', [128, KC, 512], BF16)
        obr = P.ring(sc, 'ob3', 2, [128, 4, 512], BF16)
        accT = P.sb(sc, 'accT', [128, KC, 512], BF16)
        acc4 = P.sb(sc, 'acc4', [128, 4, 512], F32)
        gtr = P.ring(sc, 'gt', 2, [128, 512], BF16)
        hid = P.ring(sc, 'hid', 2, [128, 4, 512], BF16)
        sar = P.ring(sc, 'sa', 4, [128, 512], BF16)
        cbs = P.ring(sc, 'cbs', 2, [128, 512], F32)
        combT = P.sb(sc, 'combT', [16, 512], F32)
        comb = P.ring(sc, 'comb', 2, [128, 16], F32)
        lgr = P.ring(sc, 'lg', 2, [128, 16], F32)
        rst = P.ring(sc, 'rst', 2, [128, 1], F32)
        wring = P.ring(sc, 'wst', 3, [128, 8192], BF16)
        wringB = P.ring(sc, 'wstb', 2, [128, 8192], BF16)
        psO = P.ring(sc, 'psO', 3, [128, 512], F32, psum=True)
        cme = P.ring(sc, 'cme', 1, [16, 512], F32)
        wbr = P.ring(sc, 'wbr', 2, [128, 4, 512], BF16)
        v3 = lambda ap, k: ap.rearrange("p (k c) -> p k c", k=k)
        wgs = [[v3(Dm['wb_gate'][i, jg], KC) for jg in range(4)] for i in range(4)]
        wbs = [[v3(Dm['wb_branch'][i, jg], 4) for jg in range(4)] for i in range(4)]
        wos = [v3(Dm['wb_out'][jg], KC) for jg in range(4)]
        w1s = [v3(Dm['wb_w1'][e], KC) for e in range(16)]
        w3s = [v3(Dm['wb_w3'][e], KC) for e in range(16)]
        w2s = [v3(Dm['wb_w2'][e], 4) for e in range(16)]

        def wload(dst, src, nk):
            trk = getattr(dst, 'tile', dst)
            h = nk // 2
            for (k0, k1) in ((0, h), (h, nk)):
                P.dma('sync', (lambda o, i: (lambda e: e.dma_start(out=o, in_=i)))(dst.t[:, k0:k1, :], src[:, k0:k1, :]),
                      owner=trk, writes=[trk])

        def wslot(src, nk, ncol, c0, ring=None):
            t = (ring or wring).next()
            v = Tile3(t, nk, ncol)
            wload(v, src, nk)
            return v

        for (t0, nt, who) in blocks:
            xload(P, xb, xin, nt, t0)
            for kh in range(4):
                load(P, hb[:, 4 * kh:4 * kh + 4, :nt], Dm['hT_d'][:, 4 * kh:4 * kh + 4, t0:t0 + nt])
            for jg in range(4):
                for i in range(4):
                    wg = wslot(wgs[i][jg], KC, 512, 0)
                    wb = wbr.next()
                    wload(wb, wbs[i][jg], 4)
                    ob = obr.next()
                    load(P, ob[:, :, :nt], oT_view(Dm['oT_d'])[:, 4 * i:4 * i + 4, t0:t0 + nt])
                    for jj in range(4):
                        j = jg * 4 + jj
                        pg = psA.next()
                        for kc in range(KC):
                            mm(P, pg[:, :nt], wg[:, kc, jj * 128:jj * 128 + 128], hb[:, kc, :nt], start=(kc == 0), stop=(kc == KC - 1))
                        gt = gtr.next()
                        act(P, gt[:, :nt], pg[:, :nt], AF.Sigmoid, bias=bg[:, i, j:j + 1])
                        pb = psB.next()
                        for kc in range(4):
                            mm(P, pb[:, :nt], wb[:, kc, jj * 128:jj * 128 + 128], ob[:, kc, :nt], start=(kc == 0), stop=(kc == 3))
                        if i == 0:
                            tt(P, acc4[:, jj, :nt], gt[:, :nt], pb[:, :nt], ALU.mult)
                        else:
                            tm = S.tmp.next()
                            tt(P, tm[:, :nt], gt[:, :nt], pb[:, :nt], ALU.mult)
                            tt(P, acc4[:, jj, :nt], acc4[:, jj, :nt], tm[:, :nt], ALU.add)
                for jj in range(4):
                    copy(P, accT[:, jg * 4 + jj, :nt], acc4[:, jj, :nt], eng='act')
            for jg in range(4):
                wo = wslot(wos[jg], KC, 512, 0)
                for jj in range(4):
                    j = jg * 4 + jj
                    py = psA.next()
                    for kc in range(KC):
                        mm(P, py[:, :nt], wo[:, kc, jj * 128:jj * 128 + 128], accT[:, kc, :nt], start=(kc == 0), stop=(kc == KC - 1))
                    stt(P, xb[:, j, :nt], py[:, :nt], md[:, MD_G1, j, who:who + 1], xb[:, j, :nt], ALU.mult, ALU.add)
            rs = S.rs.next()
            norm_block(P, S, C, xb, nt, lambda kc, w: md[:, MD_A2, kc, w:w + 1], lambda kc, w: md[:, MD_SH2, kc, w:w + 1],
                       who, lambda kc: hb[:, kc, :nt], rs_out=rs)
            for sub in range((nt + 127) // 128):
                ntk = min(128, nt - 128 * sub)
                a, b = 128 * sub, 128 * sub + ntk
                pl = psB.next()
                for kc in range(KC):
                    mm(P, pl[:ntk, 0:16], xb[:, kc, a:b], wr2[who][:, kc, :], start=(kc == 0), stop=(kc == KC - 1))
                pr = psB.next()
                mm(P, pr[:ntk, 0:1], rs[0:1, a:b], C.ones_f[0:1, 0:1])
                rt_ = rst.next()
                copy(P, rt_[:ntk, :], pr[:ntk, 0:1])
                lg = lgr.next()
                stt(P, lg[:ntk, :], pl[:ntk, 0:16], rt_[:ntk, 0:1], cbr[who][:ntk, :], ALU.mult, ALU.add)
                cm = comb.next()
                routing(P, S, C, lg[:ntk, :], ntk, brb, cm[:ntk, :])
                pT = psB.next()
                transpose(P, pT[:16, :ntk], cm[:ntk, :], C.cf[:ntk, 0:ntk])
                copy(P, combT[:, a:b], pT[:16, :ntk])
            def stage_a1(f, w1, sas):
                pa = psA.next()
                for kc in range(KC):
                    mm(P, pa[:, :nt], w1[:, kc, f * 128:f * 128 + 128], hb[:, kc, :nt], start=(kc == 0), stop=(kc == KC - 1))
                sa = sar.next()
                act(P, sa[:, :nt], pa[:, :nt], AF.Silu)
                sas.append(sa)

            def stage_a2(f, w3, sas, cb_, hd):
                pb = psB.next()
                for kc in range(KC):
                    mm(P, pb[:, :nt], w3[:, kc, f * 128:f * 128 + 128], hb[:, kc, :nt], start=(kc == 0), stop=(kc == KC - 1))
                tm = S.tmp.next()
                tt(P, tm[:, :nt], sas[f][:, :nt], pb[:, :nt], ALU.mult)
                tt(P, hd[:, f, :nt], tm[:, :nt], cb_[:, :nt], ALU.mult)

            def stage_b_chunk(j, w2, hd):
                po = psO.next()
                for f in range(4):
                    mm(P, po[:, :nt], w2[:, f, j * 128:j * 128 + 128], hd[:, f, :nt], start=(f == 0), stop=(f == 3))
                stt(P, xb[:, j, :nt], po[:, :nt], md[:, MD_G2, j, who:who + 1], xb[:, j, :nt], ALU.mult, ALU.add)

            def comb_sel(e):
                ce = cme.next()
                ts(P, ce[:, :nt], combT[:, :nt], C.cf[:16, e:e + 1], ALU.mult)
                return ce

            def comb_bcast(ce):
                pc = psC.next()
                mm(P, pc[:, :nt], C.ones_f[:16, :], ce[:, :nt])
                cb_n = cbs.next()
                copy(P, cb_n[:, :nt], pc[:, :nt], eng='act')
                return cb_n

            prev = None
            cb_next = comb_bcast(comb_sel(0))
            for e in range(17):
                sas = []
                if e < 16:
                    w1 = wslot(w1s[e], KC, 512, 0)
                    w3 = wslot(w3s[e], KC, 512, 0)
                    w2 = wslot(w2s[e], 4, 2048, 0, ring=wringB)
                    cb_ = cb_next
                    ce_n = comb_sel(e + 1) if e < 15 else None
                    hd = hid.next()
                for slot in range(8):
                    if e < 16:
                        if slot < 4:
                            stage_a1(slot, w1, sas)
                        else:
                            stage_a2(slot - 4, w3, sas, cb_, hd)
                    if prev is not None:
                        for j in (2 * slot, 2 * slot + 1):
                            stage_b_chunk(j, prev[0], prev[1])
                if e < 15:
                    cb_next = comb_bcast(ce_n)
                prev = (w2, hd) if e < 16 else None
            if t0 + nt <= xout.shape[2]:
                for kh in range(4):
                    store(P, xout[:, 4 * kh:4 * kh + 4, t0:t0 + nt], xb[:, 4 * kh:4 * kh + 4, :nt])


def oT_view(ap):
    return ap.rearrange("(h p) t -> p h t", p=128)


class Tile3:
    def __init__(self, tile, nk, ncol):
        self.tile = tile
        self.t = tile.t[:, 0:nk * ncol].rearrange("p (k c) -> p k c", k=nk)

    def __getitem__(self, idx):
        return V(self.tile, self.t[idx])


LAYERED = {'bmodT', 'normT', 'w_mod', 'gains', 'mla_w_uq', 'mla_w_ukv_p', 'w_in', 'sinkT', 'ntab', 'bgateT',
           'w_gate', 'w_branch', 'w_out', 'moe_w1', 'moe_w3', 'moe_w2', 'mod_d'}
SHAPES = {
    'xT': ([128, KC, NT], F32), 'csT': ([128, KC, 2], F32), 'bmodT': ([128, 96], F32), 'normT': ([128, 2, KC], F32),
    'w_mod': ([24, 128, KC * 512], F32), 'gains': ([128, NGAIN], F32), 'rope': ([128, 4, NT], F32),
    'mla_w_uq': ([384, 768], F32), 'mla_w_ukv_p': ([256, 1024], F32), 'w_in': ([128, KC * DIN], F32),
    'cf': ([128, 128], F32), 'cb': ([128, 192], BF16), 'sinkT': ([128, 4], F32),
    'wtab': ([4, 6, 128, 512], F32), 'ntab': ([4, 4, 8, 128, 512], F32), 'bgateT': ([128, 4, KC], F32),
    'wrT': ([128, KC, 16], F32), 'brb': ([128, 16], F32), 'w_gate': ([4, 4, 128, KC * 512], F32), 'w_branch': ([4, 4, 128, 4 * 512], F32),
    'w_out': ([4, 128, KC * 512], F32), 'moe_w1': ([16, 128, KC * 512], F32), 'moe_w3': ([16, 128, KC * 512], F32), 'moe_w2': ([16, 128, 4 * D], F32),
    'mod_d': ([128, 6, KC, 2], F32), 'hT_d': ([128, KC, NT], BF16), 'qT_d': ([QROWS, NT], BF16),
    'k_own': ([KROWS, NT], BF16), 'v_own': ([NT, VCOLS], BF16), 'kg': ([4, KROWS, NT], BF16), 'vg': ([4, NT, VCOLS], BF16),
    'khalo': ([768, 512], BF16), 'vhalo': ([512, 768], BF16), 'oT_d': ([2048, NT], BF16),
    'xs1': ([128, KC, NT], F32), 'outT': ([128, KC, NLAT], F32),
    'wb_gate': ([4, 4, 128, KC * 512], BF16), 'wb_branch': ([4, 4, 128, 4 * 512], BF16), 'wb_out': ([4, 128, KC * 512], BF16),
    'wb_w1': ([16, 128, KC * 512], BF16), 'wb_w3': ([16, 128, KC * 512], BF16), 'wb_w2': ([16, 128, 4 * D], BF16),
}
PROD = {'mod_d': 'p0', 'hT_d': 'p1', 'qT_d': 'p1', 'k_own': 'p1', 'v_own': 'p1', 'oT_d': 'p2',
        'kg': 'ag', 'vg': 'ag', 'khalo': 'ag', 'vhalo': 'ag',
        'wb_gate': 'p2', 'wb_branch': 'p2', 'wb_out': 'p2', 'wb_w1': 'p2', 'wb_w3': 'p2', 'wb_w2': 'p2'}
CONS = {'mod_d': ['p1', 'p3'], 'hT_d': ['p3'], 'qT_d': ['p2'], 'k_own': ['p2', 'ag'], 'v_own': ['p2', 'ag'], 'oT_d': ['p3'],
        'kg': ['p2'], 'vg': ['p2'], 'khalo': ['p2'], 'vhalo': ['p2'],
        'wb_gate': ['p3'], 'wb_branch': ['p3'], 'wb_out': ['p3'], 'wb_w1': ['p3'], 'wb_w3': ['p3'], 'wb_w2': ['p3']}


class DramMap:
    def __init__(self, nc, launch, all_launches, li):
        self.nc, self.launch, self.all, self.li = nc, launch, all_launches, li
        self.t = {}
        self.inputs, self.outputs = [], []
        self.cur_layer = 0

    def _kind(self, base, l):
        if base in ('xs1',):
            prod, cons = ('p3', 0), [('p1', 1), ('p3', 1)]
        elif base == 'outT':
            return 'ExternalOutput'
        elif base in PROD:
            prod, cons = (PROD[base], l), [(c, l) for c in CONS[base]]
        else:
            return 'ExternalInput'
        here = prod in self.launch
        later = any(c in L for L in self.all[self.li + 1:] for c in cons)
        if here:
            return 'ExternalOutput' if later else 'Internal'
        return 'ExternalInput'

    def get(self, base, l=None):
        name = base if l is None else f'{base}{l}'
        if name not in self.t:
            shape, dt = SHAPES[base]
            kind = self._kind(base, l)
            self.t[name] = self.nc.dram_tensor(name, shape, dt, kind=kind).ap()
            if kind == 'ExternalInput':
                self.inputs.append(name)
            elif kind == 'ExternalOutput':
                self.outputs.append(name)
        return self.t[name]

    def __getitem__(self, base):
        if base in LAYERED:
            return _Lay(self, base)
        if base in PROD:
            return self.get(base, self.cur_layer)
        return self.get(base)


class _Lay:
    def __init__(self, dm, base):
        self.dm, self.base = dm, base

    def __getitem__(self, idx):
        if isinstance(idx, tuple):
            ap = self.dm.get(self.base, idx[0])
            return ap[idx[1:]] if len(idx) > 2 else ap[idx[1]]
        return self.dm.get(self.base, idx)


def build(launch, all_launches, li):
    nc = bass.Bass("TRN2", target_bir_lowering=False)
    with ExitStack() as es:
        P = Prog(nc, es)
        Dm = DramMap(nc, launch, all_launches, li)
        with P.scope() as sc:
            C = load_consts(P, sc, Dm)
            for (ph, l) in launch:
                Dm.cur_layer = l
                xin = Dm.get('xT') if l == 0 else Dm.get('xs1')
                if ph == 'p0':
                    phase0(P, l, Dm, C)
                elif ph == 'p1':
                    phase1(P, l, Dm, C, xin)
                elif ph == 'p2':
                    phase2(P, l, Dm, C, need_ctx=(l == 0))
                elif ph == 'p3':
                    xout = Dm.get('xs1') if l == 0 else Dm.get('outT')
                    phase3(P, l, Dm, C, xin, xout, last=(l == 1))
    return nc, Dm


def _fm(a):
    T = a.shape[0]
    return np.ascontiguousarray(a.reshape(T, KC, 128).transpose(2, 1, 0))


def _pk(w, nk):
    C_ = w.shape[1]
    return np.ascontiguousarray(w.reshape(nk, 128, C_).transpose(1, 0, 2).reshape(128, nk * C_))


def _pkcols(w, nk, cw):
    return np.stack([_pk(w[:, c:c + cw], nk) for c in range(0, w.shape[1], cw)])


def _rope_tables(s):
    tab = np.zeros((128, 4, NT), np.float32)
    tab[:, 0, :] = 1.0
    tab[:, 2, :] = 1.0
    t = np.arange(NLAT) + s * NLAT
    rows, cols = (t // 64).astype(np.float32), (t % 64).astype(np.float32)
    for (ci, si, half) in ((0, 1, 64), (2, 3, 32)):
        d2 = half // 2
        freqs = (np.float32(10000.0) ** (-np.arange(d2, dtype=np.float32) / np.float32(d2))).astype(np.float32)
        for part, pos in ((0, rows), (1, cols)):
            ang = (pos[None, :] * freqs[:, None]).astype(np.float32)
            c, sn = np.cos(ang).astype(np.float32), np.sin(ang).astype(np.float32)
            base = part * half
            tab[base:base + d2, ci, :NLAT] = c
            tab[base + d2:base + 2 * d2, ci, :NLAT] = c
            tab[base:base + d2, si, :NLAT] = sn
            tab[base + d2:base + 2 * d2, si, :NLAT] = sn
        if half == 32:
            tab[64:, ci, :] = 0.0
    return tab


def _rot_mats():
    import ml_dtypes
    cb = np.zeros((128, 192), np.float32)
    for (off, half) in ((0, 64), (128, 32)):
        d2 = half // 2
        for part in range(2):
            b = part * half
            for i in range(d2):
                m = b + i
                cb[m + d2, off + m] = -1.0
                m2 = b + d2 + i
                cb[m2 - d2, off + m2] = 1.0
    return cb.astype(ml_dtypes.bfloat16)


def _const_f():
    return np.eye(128, dtype=np.float32)


def _wtab(s):
    tab = np.full((4, 6, 128, 512), NEG, np.float32)
    k = np.arange(128)[:, None]
    q = np.arange(512)[None, :]
    for Q in range(4):
        for rel in range(6):
            kpos = -128 + 512 * Q + 128 * rel + k
            qpos = 512 * Q + q
            g = s * NLAT + kpos
            ok = (np.abs(qpos - kpos) <= 128) & (g >= 0) & (g < SEQ)
            tab[Q, rel][np.broadcast_to(ok, (128, 512))] = 0.0
    return tab


def _ntab(s, rpb):
    tab = np.full((4, 4, 8, 128, 512), NEG, np.float32)
    k = np.arange(128)[:, None]
    q = np.arange(512)[None, :]
    yy, cx = k // 64, k % 64
    rr, c = q // 64, q % 64
    cs = np.clip(c - 8, 0, 48)
    for Q in range(4):
        R = 32 * s + 8 * Q + rr
        rs = np.clip(R - 4, 0, 120)
        for rel in range(8):
            Y = 32 * s + 8 * Q - 4 + 2 * rel + yy
            ok = (Y >= rs) & (Y <= rs + 7) & (cx >= cs) & (cx <= cs + 15) & (Y >= 0) & (Y < 128)
            dy = np.clip(Y - R + 7, 0, 14)
            dx = np.clip(cx - c + 15, 0, 30)
            dyb, dxb, okb = np.broadcast_to(dy, (128, 512)), np.broadcast_to(dx, (128, 512)), np.broadcast_to(ok, (128, 512))
            for h in range(4):
                v = rpb[h][dyb, dxb]
                tab[h, Q, rel] = np.where(okb, v, np.float32(NEG))
    return tab


LAUNCHES = [[('p0', 0), ('p1', 0)], [('p2', 0), ('p3', 0), ('p0', 1), ('p1', 1)], [('p2', 1), ('p3', 1)]]


def _static_inputs(inp):
    f32 = lambda a: np.ascontiguousarray(np.asarray(a, dtype=np.float32))
    x, c, ctx, c_ctx = f32(inp['x']), f32(inp['c']), f32(inp['ctx']), f32(inp['c_ctx'])
    shared = {}
    shared['cf'] = _const_f()
    shared['cb'] = _rot_mats()
    shared['wrT'] = np.ascontiguousarray(f32(inp['w_router']).reshape(KC, 128, 16).transpose(1, 0, 2))
    shared['brb'] = np.ascontiguousarray(np.broadcast_to(f32(inp['b_router'])[None, :], (128, 16)))
    for l in range(2):
        shared[f'bmodT{l}'] = np.ascontiguousarray(f32(inp['b_mod'])[l].reshape(96, 128).T)
        nm = np.stack([f32(inp['norm_mix'])[l].reshape(KC, 128).T, f32(inp['norm_ffn'])[l].reshape(KC, 128).T], axis=1)
        shared[f'normT{l}'] = np.ascontiguousarray(nm)
        shared[f'w_mod{l}'] = _pkcols(f32(inp['w_mod'])[l], KC, 512)
        g = np.zeros((128, NGAIN), np.float32)
        for col, key in ((G_QA, 'qn_att'), (G_KA, 'kn_att'), (G_QW, 'qn_win'), (G_KW, 'kn_win'), (G_QN, 'qn_na'), (G_KN, 'kn_na')):
            g[:, col] = f32(inp[key])[l]
        g[:, G_QM0] = f32(inp['qn_mla'])[l][:128]
        g[:64, G_QM1] = f32(inp['qn_mla'])[l][128:]
        g[:, G_KM0] = f32(inp['kn_mla'])[l][:128]
        g[:64, G_KM1] = f32(inp['kn_mla'])[l][128:]
        g[:, G_CQ:G_CQ + 3] = f32(inp['mla_qa_norm'])[l].reshape(3, 128).T
        g[:, G_CKV:G_CKV + 2] = f32(inp['mla_kva_norm'])[l].reshape(2, 128).T
        shared[f'gains{l}'] = g
        shared[f'mla_w_uq{l}'] = f32(inp['mla_w_uq'])[l]
        wk = f32(inp['mla_w_ukv'])[l].reshape(256, 4, 256)
        shared[f'mla_w_ukv_p{l}'] = np.ascontiguousarray(np.concatenate([wk[:, :, :128].reshape(256, 512), wk[:, :, 128:].reshape(256, 512)], axis=1))
        wi = f32(inp['w_in'])[l]
        shared[f'w_in{l}'] = np.ascontiguousarray(np.concatenate([_pk(wi[:, c0:c0 + nc_], KC) for (c0, nc_, _k) in GROUPS], axis=1))
        shared[f'sinkT{l}'] = np.ascontiguousarray(np.broadcast_to(f32(inp['win_sink'])[l][None, :], (128, 4)))
        shared[f'bgateT{l}'] = np.ascontiguousarray(f32(inp['b_gate'])[l].reshape(4, KC, 128).transpose(2, 0, 1))
        shared[f'w_gate{l}'] = np.stack([_pkcols(f32(inp['w_gate'])[l, i], KC, 512) for i in range(4)])
        shared[f'w_branch{l}'] = np.stack([_pkcols(f32(inp['w_branch'])[l, i], 4, 512) for i in range(4)])
        shared[f'w_out{l}'] = _pkcols(f32(inp['w_out'])[l], KC, 512)
        shared[f'moe_w1{l}'] = np.stack([_pk(f32(inp['moe_w1'])[l, e], KC) for e in range(16)])
        shared[f'moe_w3{l}'] = np.stack([_pk(f32(inp['moe_w3'])[l, e], KC) for e in range(16)])
        shared[f'moe_w2{l}'] = np.stack([_pk(f32(inp['moe_w2'])[l, e], 4) for e in range(16)])
    per_core = []
    for core in range(NCORES):
        b, s = core // 4, core % 4
        d = {}
        xt = np.concatenate([x[b, s * NLAT:(s + 1) * NLAT], ctx[b, s * NCTX:(s + 1) * NCTX]], axis=0)
        d['xT'] = _fm(xt)
        d['csT'] = np.ascontiguousarray(np.stack([c[b], c_ctx], axis=0).reshape(2, KC, 128).transpose(2, 1, 0))
        d['rope'] = _rope_tables(s)
        d['wtab'] = _wtab(s)
        for l in range(2):
            d[f'ntab{l}'] = _ntab(s, f32(inp['na_rpb'])[l])
        per_core.append(d)
    return shared, per_core


def _gather(outs, l):
    res = []
    for core in range(NCORES):
        b, s = core // 4, core % 4
        grp = [outs[4 * b + r] for r in range(4)]
        d = {}
        d[f'kg{l}'] = np.stack([g[f'k_own{l}'] for g in grp], axis=0)
        d[f'vg{l}'] = np.stack([g[f'v_own{l}'] for g in grp], axis=0)
        ko, vo = outs[core][f'k_own{l}'], outs[core][f'v_own{l}']
        kh = np.zeros((768, 512), ko.dtype)
        vh = np.zeros((512, 768), vo.dtype)
        if s > 0:
            kh[:, 0:256] = grp[s - 1][f'k_own{l}'][1024:1792, NLAT - 256:NLAT]
            vh[0:256, :] = grp[s - 1][f'v_own{l}'][NLAT - 256:NLAT, 768:1536]
        if s < 3:
            kh[:, 256:512] = grp[s + 1][f'k_own{l}'][1024:1792, 0:256]
            vh[256:512, :] = grp[s + 1][f'v_own{l}'][0:256, 768:1536]
        d[f'khalo{l}'], d[f'vhalo{l}'] = kh, vh
        res.append(d)
    return res


_CACHE = {}


def kernel(**inputs):
    shared, per_core = _static_inputs(inputs)
    avail = [dict(shared, **per_core[c]) for c in range(NCORES)]
    final = None
    for li, launch in enumerate(LAUNCHES):
        if li not in _CACHE:
            _CACHE[li] = build(launch, LAUNCHES, li)
        nc, Dm = _CACHE[li]
        in_maps = [{n: avail[c][n] for n in Dm.inputs} for c in range(NCORES)]
        res = run_bass_kernel_spmd(nc, in_maps, core_ids=list(range(NCORES)))
        outs = res.results
        for c in range(NCORES):
            for n in Dm.outputs:
                avail[c][n] = outs[c][n]
        for (ph, l) in launch:
            if ph == 'p1':
                g = _gather(avail, l)
                for c in range(NCORES):
                    avail[c].update(g[c])
        final = outs
    out = np.zeros((2, SEQ, D), np.float32)
    for core in range(NCORES):
        b, s = core // 4, core % 4
        o = final[core]['outT']
        out[b, s * NLAT:(s + 1) * NLAT] = o.transpose(2, 1, 0).reshape(NLAT, D)
    return out
```

```python
import numpy as np
from contextlib import ExitStack
import concourse.bass as bass
import concourse.mybir as mybir
from concourse.bass_utils import run_bass_kernel_spmd

F32 = mybir.dt.float32
BF16 = mybir.dt.bfloat16
AF = mybir.ActivationFunctionType
ALU = mybir.AluOpType
AX = mybir.AxisListType

NCORES = 8
D = 2048
KC = 16
SEQ = 8192
NLAT = 2048
NCTX = 64
NT = NLAT + NCTX
CTX = 256
DIN = 4288
EPS = 1e-6
NEG = -30000.0
ENGS = ['sync', 'act', 'dve', 'pool', 'pe']
SAME_ENG_SYNC = True


class V:
    __slots__ = ('tile', 'ap')

    def __init__(self, tile, ap):
        self.tile, self.ap = tile, ap


class Tile:
    def __init__(self, t, name):
        self.t, self.name = t, name
        self.lw = None
        self.rd = {}
        self.semid = None
        self.cnt = 0

    def __getitem__(self, idx):
        return V(self, self.t[idx])


class Prog:
    def __init__(self, nc, es):
        self.nc, self.es = nc, es
        self.ops = {e: [] for e in ENGS}
        self.cops = {e: [] for e in ENGS}
        self.sem_pool = []
        self.semcnt = {}
        self.all_tiles = []
        self.uid = 0
        self.engsem = {e: es.enter_context(nc.semaphore('eng_' + e)) for e in ENGS}
        self.dsem = {}
        self.nsig = {e: 0 for e in ENGS}
        self.nassigned = {e: 0 for e in ENGS}
        self.waited = {e: {} for e in ENGS}
        self.sim_c, self.sim_d = {}, {}

    class Scope:
        def __init__(self, P):
            self.P, self.es, self.tiles = P, ExitStack(), []

        def __enter__(self):
            self.es.__enter__()
            return self

        def __exit__(self, *a):
            P = self.P
            P.barrier()
            P.flush()
            for t in self.tiles:
                if t.semid is not None:
                    P.sem_pool.append((t.semid, t.cnt))
                P.all_tiles.remove(t)
            return self.es.__exit__(*a)

    def scope(self):
        return Prog.Scope(self)

    def sb(self, sc, name, shape, dt):
        self.uid += 1
        name = f's{self.uid}_{name}'
        t = sc.es.enter_context(self.nc.sbuf_tensor(name, shape, dt))
        tl = Tile(t, name)
        self.all_tiles.append(tl)
        sc.tiles.append(tl)
        return tl

    def ps(self, sc, name, shape, dt=F32):
        self.uid += 1
        name = f'p{self.uid}_{name}'
        t = sc.es.enter_context(self.nc.psum_tensor(name, shape, dt))
        tl = Tile(t, name)
        self.all_tiles.append(tl)
        sc.tiles.append(tl)
        return tl

    def ring(self, sc, name, n, shape, dt, psum=False):
        return Ring([(self.ps if psum else self.sb)(sc, f'{name}{i}', shape, dt) for i in range(n)])

    def _deps(self, eng, reads, writes):
        deps = {}

        def add(tok):
            if tok is None:
                return
            if tok[0] == 'c':
                if tok[1] == eng and (eng == 'pe' or not SAME_ENG_SYNC):
                    return
                key = ('c', tok[1])
            else:
                key = ('d', tok[1])
            if deps.get(key, 0) < tok[2]:
                deps[key] = tok[2]

        for r in reads:
            add(r.lw)
        for w in writes:
            add(w.lw)
            for tok in w.rd.values():
                add(tok)
        return deps

    def op(self, eng, fn, reads=(), writes=()):
        reads = [r for r in reads if r is not None]
        deps = self._deps(eng, reads, writes)
        rec = dict(fn=fn, deps=deps, sig=False, kind='c')
        self.ops[eng].append(rec)
        self.cops[eng].append(rec)
        tok = ('c', eng, len(self.cops[eng]))
        for r in reads:
            r.rd[('c', eng)] = tok
        for w in writes:
            w.lw = tok
            w.rd = {}
        return rec

    def dma(self, q, fn, owner, reads=(), writes=()):
        deps = self._deps(q, reads, writes)
        if owner.semid is None:
            if self.sem_pool:
                owner.semid, owner.cnt = self.sem_pool.pop()
            else:
                owner.semid, owner.cnt = len(self.semcnt), 0
                self.dsem[owner.semid] = self.es.enter_context(self.nc.semaphore(f'd{owner.semid}'))
        owner.cnt += 1
        self.semcnt[owner.semid] = owner.cnt
        tok = ('d', owner.semid, owner.cnt)
        rec = dict(fn=fn, deps=deps, kind='d', semid=owner.semid)
        self.ops[q].append(rec)
        for r in reads:
            r.rd[('d', owner.semid)] = tok
        for w in writes:
            w.lw = tok
            w.rd = {}
        return rec

    def barrier(self):
        deps = {}
        for e in ENGS:
            if self.cops[e]:
                deps[('c', e)] = len(self.cops[e])
        for sid, c in self.semcnt.items():
            deps[('d', sid)] = c
        for e in ENGS:
            d = {k: v for k, v in deps.items() if k != ('c', e)}
            self.ops[e].append(dict(fn=None, deps=d, kind='b'))
        for t in self.all_tiles:
            t.lw = None
            t.rd = {}

    def check_progress(self):
        pos = {e: 0 for e in ENGS}
        moved = True
        while moved:
            moved = False
            for e in ENGS:
                ops = self.ops[e]
                while pos[e] < len(ops):
                    rec = ops[pos[e]]
                    ok = True
                    for key, val in rec['deps'].items():
                        have = self.sim_c.get(key[1], 0) if key[0] == 'c' else self.sim_d.get(key[1], 0)
                        if have < val:
                            ok = False
                            break
                    if not ok:
                        break
                    if rec['kind'] == 'c':
                        self.sim_c[e] = self.sim_c.get(e, 0) + 1
                    elif rec['kind'] == 'd':
                        self.sim_d[rec['semid']] = self.sim_d.get(rec['semid'], 0) + 1
                    pos[e] += 1
                    moved = True
        stuck = {e: (pos[e], len(self.ops[e])) for e in ENGS if pos[e] < len(self.ops[e])}
        assert not stuck, f"DEADLOCK in recorded program: {stuck}"

    def flush(self):
        nc = self.nc
        self.check_progress()
        for e in ENGS:
            for rec in self.ops[e]:
                for key, val in rec['deps'].items():
                    if key[0] == 'c':
                        self.cops[key[1]][val - 1]['sig'] = True
        for e in ENGS:
            for rec in self.cops[e][self.nassigned[e]:]:
                if rec['sig']:
                    self.nsig[e] += 1
                rec['signo'] = self.nsig[e]
            self.nassigned[e] = len(self.cops[e])
        engsem, dsem = self.engsem, self.dsem

        def run(ename, eng):
            waited = self.waited[ename]
            for rec in self.ops[ename]:
                for key, val in rec['deps'].items():
                    if key[0] == 'c':
                        sem = engsem[key[1]]
                        v = self.cops[key[1]][val - 1]['signo']
                    else:
                        sem = dsem[key[1]]
                        v = 16 * val
                    if waited.get(key, 0) >= v:
                        continue
                    waited[key] = v
                    eng.wait_ge(sem, v)
                if rec['fn'] is None:
                    continue
                ins = rec['fn'](eng)
                if rec['kind'] == 'd':
                    ins.then_inc(dsem[rec['semid']], 16)
                elif rec['sig']:
                    ins.then_inc(engsem[ename], 1)
            self.ops[ename] = []

        with nc.Block() as block:
            @block.sync
            def _(e):
                run('sync', e)

            @block.scalar
            def _(e):
                run('act', e)

            @block.vector
            def _(e):
                run('dve', e)

            @block.gpsimd
            def _(e):
                run('pool', e)

            @block.tensor
            def _(e):
                run('pe', e)


class Ring:
    def __init__(self, tiles):
        self.tiles, self.i = tiles, 0

    def next(self):
        t = self.tiles[self.i % len(self.tiles)]
        self.i += 1
        return t


def _t(*vs):
    return [v.tile for v in vs if isinstance(v, V)]


def _a(v):
    return v.ap if isinstance(v, V) else v


def mm(P, out, lhsT, rhs, start=True, stop=True):
    P.op('pe', lambda e: e.matmul(out.ap, lhsT.ap, rhs.ap, start=start, stop=stop),
         reads=_t(lhsT, rhs), writes=_t(out))


def transpose(P, out, in_, ident):
    P.op('pe', lambda e: e.transpose(out.ap, in_.ap, ident.ap), reads=_t(in_, ident), writes=_t(out))


def act(P, out, in_, func, scale=1.0, bias=None):
    if bias is None:
        P.op('act', lambda e: e.activation(out=out.ap, in_=in_.ap, func=func, scale=scale),
             reads=_t(in_), writes=_t(out))
    else:
        P.op('act', lambda e: e.activation(out=out.ap, in_=in_.ap, func=func, scale=scale, bias=_a(bias)),
             reads=_t(in_, bias), writes=_t(out))


def tt(P, out, in0, in1, op, eng='dve'):
    P.op(eng, lambda e: e.tensor_tensor(out=out.ap, in0=in0.ap, in1=in1.ap, op=op),
         reads=_t(in0, in1), writes=_t(out))


def ts(P, out, in0, s1, op0, s2=None, op1=None, eng='dve'):
    if op1 is None:
        P.op(eng, lambda e: e.tensor_scalar(out=out.ap, in0=in0.ap, scalar1=_a(s1), scalar2=None, op0=op0),
             reads=_t(in0, s1), writes=_t(out))
    else:
        P.op(eng, lambda e: e.tensor_scalar(out=out.ap, in0=in0.ap, scalar1=_a(s1), scalar2=_a(s2),
                                            op0=op0, op1=op1),
             reads=_t(in0, s1, s2), writes=_t(out))


def stt(P, out, in0, scalar, in1, op0, op1):
    P.op('dve', lambda e: e.scalar_tensor_tensor(out=out.ap, in0=in0.ap, scalar=_a(scalar), in1=in1.ap,
                                                 op0=op0, op1=op1),
         reads=_t(in0, scalar, in1), writes=_t(out))


def recip(P, out, in_):
    P.op('dve', lambda e: e.reciprocal(out=out.ap, in_=in_.ap), reads=_t(in_), writes=_t(out))


def copy(P, out, in_, eng='dve'):
    if eng == 'act':
        P.op('act', lambda e: e.activation(out=out.ap, in_=in_.ap, func=AF.Copy), reads=_t(in_), writes=_t(out))
    else:
        P.op(eng, lambda e: e.tensor_copy(out=out.ap, in_=in_.ap), reads=_t(in_), writes=_t(out))


def reduce(P, out, in_, op, axis=AX.X):
    P.op('dve', lambda e: e.tensor_reduce(out=out.ap, in_=in_.ap, axis=axis, op=op), reads=_t(in_), writes=_t(out))


def memset(P, out, val, eng='dve'):
    P.op(eng, lambda e: e.memset(out.ap, val), writes=_t(out))


def load(P, out, src, q='sync'):
    P.dma(q, lambda e: e.dma_start(out=out.ap, in_=src), owner=out.tile, writes=[out.tile])


def store(P, dst, in_, q='sync'):
    P.dma(q, lambda e: e.dma_start(out=dst, in_=in_.ap), owner=in_.tile, reads=[in_.tile])


BLOCKS = [(0, 512, 0), (512, 512, 0), (1024, 512, 0), (1536, 512, 0), (2048, 64, 1)]
GROUPS = [(0, 512, 'aq'), (512, 512, 'akv'), (1024, 704, 'mla'), (1728, 512, 'wq'), (2240, 512, 'wkv'),
          (2752, 512, 'nq'), (3264, 512, 'nk'), (3776, 512, 'nv')]
KROWS, VCOLS, QROWS = 1792, 1536, 2304
G_QA, G_KA, G_QW, G_KW, G_QN, G_KN, G_QM0, G_QM1, G_KM0, G_KM1, G_CQ, G_CKV = 0, 1, 2, 3, 4, 5, 6, 7, 8, 9, 10, 13
NGAIN = 15
MD_SH1, MD_A1, MD_G1, MD_SH2, MD_A2, MD_G2 = 0, 1, 2, 3, 4, 5


class Consts:
    pass


def load_consts(P, sc, Dm):
    C = Consts()
    C.ones_bf = P.sb(sc, 'ones_bf', [128, 128], BF16)
    memset(P, C.ones_bf[:, :], 1.0)
    C.ones_f = P.sb(sc, 'ones_f', [128, 128], F32)
    memset(P, C.ones_f[:, :], 1.0)
    C.eps = P.sb(sc, 'eps', [128, 1], F32)
    memset(P, C.eps[:, :], EPS)
    C.cf = P.sb(sc, 'cf', [128, 128], F32)
    load(P, C.cf[:, :], Dm['cf'])
    C.cb = P.sb(sc, 'cb', [128, 192], BF16)
    load(P, C.cb[:, :], Dm['cb'])
    return C


def norm_block(P, S, C, x, nt, Acol, shcol, who, dst, rs_out=None):
    pss = S.pss.next()
    for kc in range(KC):
        sq = S.sq.next()
        act(P, sq[:, :nt], x[:, kc, :nt], AF.Square)
        mm(P, pss[:, :nt], C.ones_bf[:, :], sq[:, :nt], start=(kc == 0), stop=(kc == KC - 1))
    sd = S.sd.next()
    act(P, sd[:, :nt], pss[:, :nt], AF.Sqrt, scale=1.0 / D, bias=C.eps[:, 0:1])
    rs = rs_out if rs_out is not None else S.rs.next()
    recip(P, rs[:, :nt], sd[:, :nt])
    for kc in range(KC):
        tmp = S.tmp.next()
        stt(P, tmp[:, :nt], x[:, kc, :nt], Acol(kc, who), rs[:, :nt], ALU.mult, ALU.mult)
        act(P, dst(kc), tmp[:, :nt], AF.Identity, bias=shcol(kc, who))
    return rs


def xload(P, dst_tile, src, nt, t0):
    for kh in range(4):
        load(P, dst_tile[:, 4 * kh:4 * kh + 4, :nt], src[:, 4 * kh:4 * kh + 4, t0:t0 + nt])


def wcast(P, dst_tile, src, nk, ncol, c0):
    step = max(1, nk // 4)
    for k0 in range(0, nk, step):
        k1 = min(nk, k0 + step)
        trk = getattr(dst_tile, 'tile', dst_tile)
        P.dma('pool', (lambda o, i: (lambda e: e.dma_start(out=o, in_=i)))(dst_tile.t[:, k0:k1, :ncol], src[:, k0:k1, c0:c0 + ncol]),
              owner=trk, writes=[trk])


def phase0(P, l, Dm, C):
    with P.scope() as sc:
        cs = P.sb(sc, 'cs', [128, KC, 2], F32)
        load(P, cs[:, :, :], Dm['csT'])
        act(P, cs[:, :, :], cs[:, :, :], AF.Silu)
        bm = P.sb(sc, 'bm', [128, 96], F32)
        load(P, bm[:, :], Dm['bmodT'][l])
        nrm = P.sb(sc, 'nrm', [128, 2, KC], F32)
        load(P, nrm[:, :, :], Dm['normT'][l])
        modT = P.sb(sc, 'modT', [128, 96, 2], F32)
        md = P.sb(sc, 'md', [128, 6, KC, 2], F32)
        csb = P.sb(sc, 'csb', [128, KC, 2], BF16)
        copy(P, csb[:, :, :], cs[:, :, :])
        wm = P.ring(sc, 'wm', 3, [128, KC, 512], BF16)
        psm = P.ring(sc, 'psm', 2, [128, 4, 2], F32, psum=True)
        for pc in range(24):
            w = wm.next()
            wcast(P, w, Dm['w_mod'][l, pc].rearrange("p (k c) -> p k c", k=KC), KC, 512, 0)
            pm = psm.next()
            for j in range(4):
                for kc in range(KC):
                    mm(P, pm[:, j, :], w[:, kc, j * 128:(j + 1) * 128], csb[:, kc, :], start=(kc == 0), stop=(kc == KC - 1))
            for n in range(2):
                tt(P, modT[:, pc * 4:pc * 4 + 4, n], pm[:, :, n], bm[:, pc * 4:pc * 4 + 4], ALU.add)
        for n in range(2):
            copy(P, md[:, MD_SH1, :, n], modT[:, 0:16, n])
            stt(P, md[:, MD_A1, :, n], modT[:, 16:32, n], 1.0, nrm[:, 0, :], ALU.add, ALU.mult)
            copy(P, md[:, MD_G1, :, n], modT[:, 32:48, n])
            copy(P, md[:, MD_SH2, :, n], modT[:, 48:64, n])
            stt(P, md[:, MD_A2, :, n], modT[:, 64:80, n], 1.0, nrm[:, 1, :], ALU.add, ALU.mult)
            copy(P, md[:, MD_G2, :, n], modT[:, 80:96, n])
        store(P, Dm['mod_d'][l], md[:, :, :, :])


def qk_unit_gen(P, S, C, pieces, Dtot, nt, t0):
    for pc in pieces:
        if 'proj' in pc:
            pc['src'] = pc['proj']()
    yield
    pss = S.pss.next()
    n = len(pieces)
    for i, pc in enumerate(pieces):
        dp = pc['dp']
        if pc.get('sq') is None:
            sq = S.sq.next()
            act(P, sq[:dp, :nt], pc['src'], AF.Square)
            sqv = sq[:dp, :nt]
        else:
            sqv = pc['sq']
        mm(P, pss[:, :nt], C.ones_bf[:dp, :], sqv, start=(i == 0), stop=(i == n - 1))
    yield
    sd = S.sd.next()
    act(P, sd[:, :nt], pss[:, :nt], AF.Sqrt, scale=1.0 / Dtot, bias=C.eps[:, 0:1])
    rs = S.rs.next()
    recip(P, rs[:, :nt], sd[:, :nt])
    ropes = []
    for pc in pieces:
        dp = pc['dp']
        kind, dst = pc['dst']
        if pc.get('rope') is None:
            if kind == 's':
                stt(P, dst, pc['src'], pc['gain'], rs[:dp, :nt], ALU.mult, ALU.mult)
            else:
                ob = S.ob.next()
                stt(P, ob[:dp, :nt], pc['src'], pc['gain'], rs[:dp, :nt], ALU.mult, ALU.mult)
                store(P, dst, ob[:dp, :nt])
        else:
            R, cos, sin = S.rope[pc['rope']]
            xn = S.xn.next()
            stt(P, xn[:dp, :nt], pc['src'], pc['gain'], rs[:dp, :nt], ALU.mult, ALU.mult)
            psr = S.psr.next()
            mm(P, psr[:dp, :nt], R, xn[:dp, :nt])
            ropes.append((pc, xn, psr, cos, sin))
    yield
    for (pc, xn, psr, cos, sin) in ropes:
        dp = pc['dp']
        kind, dst = pc['dst']
        t1 = S.t1.next()
        tt(P, t1[:dp, :nt], xn[:dp, :nt], cos[:dp, t0:t0 + nt], ALU.mult, eng='pool')
        t2 = S.t2.next()
        tt(P, t2[:dp, :nt], psr[:dp, :nt], sin[:dp, t0:t0 + nt], ALU.mult)
        ob = S.ob.next()
        tt(P, ob[:dp, :nt], t1[:dp, :nt], t2[:dp, :nt], ALU.add)
        store(P, dst, ob[:dp, :nt])


def qk_unit(P, S, C, pieces, Dtot, nt, t0):
    for _ in qk_unit_gen(P, S, C, pieces, Dtot, nt, t0):
        pass


class Pipe:
    def __init__(self):
        self.q = []

    def _advance(self):
        for g in reversed(list(self.q)):
            try:
                next(g)
            except StopIteration:
                self.q.remove(g)

    def push(self, g):
        next(g)
        self._advance()
        self.q.append(g)

    def flush(self):
        while self.q:
            self._advance()


def phase1(P, l, Dm, C, xin):
    with P.scope() as sc:
        hT = P.sb(sc, 'hT', [128, KC, NT], BF16)
        md = P.sb(sc, 'md1', [128, 6, KC, 2], F32)
        load(P, md[:, :, :, :], Dm['mod_d'][l])
        S = Consts()
        S.sq = P.ring(sc, 'sq', 3, [128, 512], BF16)
        S.sd = P.ring(sc, 'sd', 2, [128, 512], F32)
        S.rs = P.ring(sc, 'rs', 2, [128, 512], F32)
        S.pss = P.ring(sc, 'pss', 2, [128, 512], F32, psum=True)
        with P.scope() as s2:
            xb = P.ring(s2, 'xb', 2, [128, KC, 512], F32)
            S.tmp = P.ring(s2, 'tmp', 2, [128, 512], F32)
            for (t0, nt, who) in BLOCKS:
                x = xb.next()
                xload(P, x, xin, nt, t0)
                norm_block(P, S, C, x, nt, lambda kc, w: md[:, MD_A1, kc, w:w + 1], lambda kc, w: md[:, MD_SH1, kc, w:w + 1],
                           who, lambda kc: hT[:, kc, t0:t0 + nt])
                for kh in range(4):
                    store(P, Dm['hT_d'][:, 4 * kh:4 * kh + 4, t0:t0 + nt], hT[:, 4 * kh:4 * kh + 4, t0:t0 + nt])
        with P.scope() as s2:
            G = P.sb(s2, 'gains', [128, NGAIN], F32)
            load(P, G[:, :], Dm['gains'][l])
            rp = P.sb(s2, 'ropet', [128, 4, NT], F32)
            for i in range(4):
                load(P, rp[:, i, :], Dm['rope'][:, i, :])
            S.rope = {'r128': (C.cb[:, 0:128], rp.t[:, 0, :], rp.t[:, 1, :]), 'r64': (C.cb[:64, 128:192], rp.t[:, 2, :], rp.t[:, 3, :])}
            S.rope = {k: (v[0], _RV(rp, v[1]), _RV(rp, v[2])) for k, v in S.rope.items()}
            wuq = P.sb(s2, 'wuq', [128, 3, 768], BF16)
            wcast(P, wuq, Dm['mla_w_uq'][l].rearrange("(kc p) c -> p kc c", p=128), 3, 768, 0)
            wukv = P.sb(s2, 'wukv', [128, 2, 1024], BF16)
            wcast(P, wukv, Dm['mla_w_ukv_p'][l].rearrange("(kc p) c -> p kc c", p=128), 2, 1024, 0)
            wr = P.ring(s2, 'win', 2, [128, KC, 704], BF16)
            S.xn = P.ring(s2, 'xn', 2, [128, 512], BF16)
            S.t1 = P.ring(s2, 't1', 2, [128, 512], F32)
            S.t2 = P.ring(s2, 't2', 2, [128, 512], F32)
            S.ob = P.ring(s2, 'ob', 3, [128, 512], BF16)
            S.psr = P.ring(s2, 'psr', 2, [128, 512], F32, psum=True)
            proj = P.ring(s2, 'proj', 4, [128, 512], F32, psum=True)
            cqn = P.sb(s2, 'cqn', [128, 3, 512], BF16)
            ckvn = P.sb(s2, 'ckvn', [128, 2, 512], BF16)
            krf = P.sb(s2, 'krf', [64, 512], F32)
            krsq = P.sb(s2, 'krsq', [64, 512], BF16)
            vb = P.ring(s2, 'vb', 2, [128, 512], BF16)
            win_l = Dm['w_in'][l]
            qT, kO, vO = Dm['qT_d'], Dm['k_own'], Dm['v_own']

            def proj_fm(w, col, m, t0, nt, nk=KC, act_=None):
                ps = proj.next()
                for kc in range(nk):
                    a = hT[:, kc, t0:t0 + nt] if act_ is None else act_[:, kc, :nt]
                    mm(P, ps[:m, :nt], w[:, kc, col:col + m], a, start=(kc == 0), stop=(kc == nk - 1))
                return ps

            def proj_tm(w, col, ncols, t0, nt, vcol, nk=KC, act_=None):
                for sub in range((nt + 127) // 128):
                    ntk = min(128, nt - 128 * sub)
                    ps = proj.next()
                    for kc in range(nk):
                        a = hT[:, kc, t0 + 128 * sub:t0 + 128 * sub + ntk] if act_ is None else act_[:, kc, 128 * sub:128 * sub + ntk]
                        mm(P, ps[:ntk, :ncols], a, w[:, kc, col:col + ncols], start=(kc == 0), stop=(kc == nk - 1))
                    v = vb.next()
                    copy(P, v[:ntk, :ncols], ps[:ntk, :ncols], eng='act')
                    store(P, vO[t0 + 128 * sub:t0 + 128 * sub + ntk, vcol:vcol + ncols], v[:ntk, :ncols])

            pipe = Pipe()

            def heads(w, nh, col0, gcol, rope, dst, row0, t0, nt):
                for h in range(nh):
                    pj = (lambda c_=col0 + 128 * h: proj_fm(w, c_, 128, t0, nt)[:, :nt])
                    pipe.push(qk_unit_gen(P, S, C, [dict(proj=pj, dp=128, gain=G[:, gcol:gcol + 1], rope=rope,
                                                         dst=('d', dst[row0 + 128 * h:row0 + 128 * h + 128, t0:t0 + nt]))], 128, nt, t0))

            wts = {}

            def prefetch(gi):
                if gi < len(GROUPS) and gi not in wts:
                    wts[gi] = wr.next()
                    c0_, nc_ = GROUPS[gi][0], GROUPS[gi][1]
                    wcast(P, wts[gi], win_l[:, KC * c0_:KC * (c0_ + nc_)].rearrange("p (k c) -> p k c", k=KC), KC, nc_, 0)

            prefetch(0)
            for gi, (c0, ncol, kind) in enumerate(GROUPS):
                prefetch(gi + 1)
                w = wts.pop(gi)
                if kind in ('aq', 'akv', 'wq', 'wkv', 'nq', 'nk'):
                    for (t0, nt, who) in BLOCKS:
                        if kind == 'aq':
                            heads(w, 4, 0, G_QA, 'r128', qT, 0, t0, nt)
                        elif kind == 'akv':
                            heads(w, 2, 0, G_KA, 'r128', kO, 0, t0, nt)
                        elif kind == 'wq':
                            heads(w, 4, 0, G_QW, 'r128', qT, 1280, t0, nt)
                        elif kind == 'wkv':
                            heads(w, 2, 0, G_KW, 'r128', kO, 1024, t0, nt)
                        elif kind == 'nq':
                            heads(w, 4, 0, G_QN, None, qT, 1792, t0, nt)
                        elif kind == 'nk':
                            heads(w, 4, 0, G_KN, None, kO, 1280, t0, nt)
                    pipe.flush()
                for (t0, nt, who) in BLOCKS:
                    if kind == 'akv':
                        proj_tm(w, 256, 256, t0, nt, 0)
                    elif kind == 'wkv':
                        proj_tm(w, 256, 256, t0, nt, 768)
                    elif kind == 'nv':
                        proj_tm(w, 0, 512, t0, nt, 1024)
                    elif kind == 'mla':
                        pcs = [proj_fm(w, 128 * i, 128, t0, nt) for i in range(3)]
                        qk_unit(P, S, C, [dict(src=pcs[i][:, :nt], dp=128, gain=G[:, G_CQ + i:G_CQ + i + 1],
                                               dst=('s', cqn[:, i, :nt])) for i in range(3)], 384, nt, t0)
                        pcs = [proj_fm(w, 384 + 128 * i, 128, t0, nt) for i in range(2)]
                        qk_unit(P, S, C, [dict(src=pcs[i][:, :nt], dp=128, gain=G[:, G_CKV + i:G_CKV + i + 1],
                                               dst=('s', ckvn[:, i, :nt])) for i in range(2)], 256, nt, t0)
                        pk = proj_fm(w, 640, 64, t0, nt)
                        copy(P, krf[:, :nt], pk[:64, :nt])
                        act(P, krsq[:, :nt], pk[:64, :nt], AF.Square)
                        for h in range(4):
                            pn = proj_fm(wuq, 192 * h, 128, t0, nt, nk=3, act_=cqn)
                            pr = proj_fm(wuq, 192 * h + 128, 64, t0, nt, nk=3, act_=cqn)
                            r0 = 512 + 192 * h
                            qk_unit(P, S, C, [
                                dict(src=pn[:, :nt], dp=128, gain=G[:, G_QM0:G_QM0 + 1], dst=('d', qT[r0:r0 + 128, t0:t0 + nt])),
                                dict(src=pr[:64, :nt], dp=64, gain=G[:64, G_QM1:G_QM1 + 1], rope='r64',
                                     dst=('d', qT[r0 + 128:r0 + 192, t0:t0 + nt]))], 192, nt, t0)
                        for h in range(4):
                            pn = proj_fm(wukv, 128 * h, 128, t0, nt, nk=2, act_=ckvn)
                            r0 = 256 + 192 * h
                            qk_unit(P, S, C, [
                                dict(src=pn[:, :nt], dp=128, gain=G[:, G_KM0:G_KM0 + 1], dst=('d', kO[r0:r0 + 128, t0:t0 + nt])),
                                dict(src=krf[:, :nt], dp=64, gain=G[:64, G_KM1:G_KM1 + 1], rope='r64', sq=krsq[:, :nt],
                                     dst=('d', kO[r0 + 128:r0 + 192, t0:t0 + nt]))], 192, nt, t0)
                        proj_tm(wukv, 512, 512, t0, nt, 256, nk=2, act_=ckvn)


class _RV:
    def __init__(self, tile, ap):
        self.tile, self.ap = tile, ap

    def __getitem__(self, idx):
        return V(self.tile, self.ap[idx])


def attn_block(P, S, C, nt, steps, out_dram, scale, sink=None, LA=3):
    o_ps = S.ops_.next()
    d_ps = S.dps.next()
    n = len(steps)
    sts = {}

    def issue_st(i):
        st = S.st.next()
        kp = steps[i][0]
        for j, (kT, qv) in enumerate(kp):
            mm(P, st[:, :nt], kT, qv, start=(j == 0), stop=(j == len(kp) - 1))
        sts[i] = st

    tbs = {}
    for i, (kp, vch, bias) in enumerate(steps):
        if bias is not None:
            tbs[i] = S.tb.next()
            load(P, tbs[i][:, :nt], bias)
    for i in range(min(LA, n)):
        issue_st(i)
    for i, (kp, vch, bias) in enumerate(steps):
        if i + LA < n:
            issue_st(i + LA)
        st = sts.pop(i)
        pt = S.pt.next()
        if bias is None:
            act(P, pt[:, :nt], st[:, :nt], AF.Exp, scale=scale)
        else:
            tb = tbs[i]
            sf = S.sf.next()
            stt(P, sf[:, :nt], st[:, :nt], scale, tb[:, :nt], ALU.mult, ALU.add)
            act(P, pt[:, :nt], sf[:, :nt], AF.Exp)
        mm(P, o_ps[:, :nt], vch, pt[:, :nt], start=(i == 0), stop=(i == n - 1))
        mm(P, d_ps[:, :nt], C.ones_bf[:, :], pt[:, :nt], start=(i == 0), stop=(i == n - 1))
    rd = S.rd.next()
    if sink is not None:
        ts(P, rd[:, :nt], d_ps[:, :nt], sink, ALU.add)
        recip(P, rd[:, :nt], rd[:, :nt])
    else:
        recip(P, rd[:, :nt], d_ps[:, :nt])
    ob = S.ob.next()
    tt(P, ob[:, :nt], o_ps[:, :nt], rd[:, :nt], ALU.mult)
    store(P, out_dram, ob[:, :nt])


def convert_weights(P, l, Dm):
    cv = Tile(None, 'cvt')
    cv.base = 0
    r = lambda ap: ap.rearrange("p (a b) -> p a b", b=2048)

    jobs = []

    def cvd(dst, src):
        def job(o=r(dst), i=r(src)):
            na = o.shape[1]
            for (a0, a1) in ((0, na // 2), (na // 2, na)):
                if a1 == a0:
                    continue
                rec = P.dma('pool', (lambda oo, ii: (lambda e: e.dma_start(out=oo, in_=ii)))(o[:, a0:a1, :], i[:, a0:a1, :]), owner=cv)
                cv.base += 1
                if cv.base > 1:
                    rec['deps'][('d', cv.semid)] = cv.cnt - 1
        jobs.append(job)

    for jg in range(4):
        for i in range(4):
            cvd(Dm['wb_gate'][i, jg], Dm['w_gate'][l, i, jg])
            cvd(Dm['wb_branch'][i, jg], Dm['w_branch'][l, i, jg])
    for jg in range(4):
        cvd(Dm['wb_out'][jg], Dm['w_out'][l, jg])
    for e in range(16):
        cvd(Dm['wb_w1'][e], Dm['moe_w1'][l, e])
        cvd(Dm['wb_w3'][e], Dm['moe_w3'][l, e])
        cvd(Dm['wb_w2'][e], Dm['moe_w2'][l, e])
    return jobs


def phase2(P, l, Dm, C, need_ctx):
    blocks = BLOCKS if need_ctx else BLOCKS[:4]
    kg, vg, qT, oT = Dm['kg'], Dm['vg'], Dm['qT_d'], Dm['oT_d']
    kown, vown, khalo, vhalo = Dm['k_own'], Dm['v_own'], Dm['khalo'], Dm['vhalo']
    with P.scope() as sc:
        cjobs = convert_weights(P, l, Dm)
        S = Consts()
        S.st = P.ring(sc, 'st', 4, [128, 512], F32, psum=True)
        S.ops_ = P.ring(sc, 'ops', 2, [128, 512], F32, psum=True)
        S.dps = P.ring(sc, 'dps', 2, [128, 512], F32, psum=True)
        S.pt = P.ring(sc, 'pt', 6, [128, 512], BF16)
        S.tb = P.ring(sc, 'tb', 10, [128, 512], F32)
        S.sf = P.ring(sc, 'sf', 3, [128, 512], F32)
        S.rd = P.ring(sc, 'rd', 2, [128, 512], F32)
        S.ob = P.ring(sc, 'ob2', 3, [128, 512], BF16)
        vr = lambda ap: ap.rearrange("(c p) d -> p c d", p=128)
        with P.scope() as s2:
            kn = P.ring(s2, 'kn', 2, [128, 8448], BF16)
            kr = P.ring(s2, 'kr', 2, [64, 8448], BF16)
            vv = P.ring(s2, 'vv', 2, [128, 66, 128], BF16)
            qn = P.ring(s2, 'qn', 2, [128, NT], BF16)
            qr = P.ring(s2, 'qr', 2, [64, NT], BF16)

            def load_k(kt, row0, nr):
                for r in range(4):
                    load(P, kt[:nr, 256 + 2048 * r:256 + 2048 * (r + 1)], kg[r, row0:row0 + nr, 0:2048])
                    load(P, kt[:nr, 64 * r:64 * r + 64], kg[r, row0:row0 + nr, 2048:2112])

            def load_v(vt, col0):
                for r in range(4):
                    load(P, vt[:, 2 + 16 * r:2 + 16 * r + 16, :], vr(vg[r, 0:2048, col0:col0 + 128]))
                    load(P, vt[64 * (r % 2):64 * (r % 2) + 64, r // 2, :], vg[r, 2048:2112, col0:col0 + 128])

            units = [('A', 0), ('A', 1), ('M', 0), ('M', 1), ('M', 2), ('M', 3)]
            for ui, (mx, u) in enumerate(units):
                k1 = kn.next()
                v1 = vv.next()
                k2 = None
                if mx == 'A':
                    load_k(k1, 128 * u, 128)
                    load_v(v1, 128 * u)
                    qheads = [2 * u, 2 * u + 1]
                    scale = 128 ** -0.5
                else:
                    k2 = kr.next()
                    load_k(k1, 256 + 192 * u, 128)
                    load_k(k2, 256 + 192 * u + 128, 64)
                    load_v(v1, 256 + 128 * u)
                    qheads = [u]
                    scale = 192 ** -0.5
                if ui == 0:
                    for job in cjobs:
                        job()
                for h in qheads:
                    q1 = qn.next()
                    q2 = None
                    if mx == 'A':
                        load(P, q1[:, :], qT[128 * h:128 * h + 128, :])
                        oh = h
                    else:
                        q2 = qr.next()
                        load(P, q1[:, :], qT[512 + 192 * h:512 + 192 * h + 128, :])
                        load(P, q2[:, :], qT[512 + 192 * h + 128:512 + 192 * h + 192, :])
                        oh = 4 + h
                    for (t0, nt, who) in blocks:
                        steps = []
                        for c in (range(66) if who == 0 else range(2)):
                            kp = [(k1[:, 128 * c:128 * c + 128], q1[:, t0:t0 + nt])]
                            if k2 is not None:
                                kp.append((k2[:, 128 * c:128 * c + 128], q2[:, t0:t0 + nt]))
                            steps.append((kp, v1[:, c, :], None))
                        attn_block(P, S, C, nt, steps, oT[128 * oh:128 * oh + 128, t0:t0 + nt], scale)
        with P.scope() as s2:
            kw = P.ring(s2, 'kw', 2, [128, 2816], BF16)
            vw = P.ring(s2, 'vw', 2, [128, 22, 128], BF16)
            qw = P.ring(s2, 'qw', 2, [128, NT], BF16)
            esink = P.sb(s2, 'esink', [128, 4], F32)
            load(P, esink[:, :], Dm['sinkT'][l])
            act(P, esink[:, :], esink[:, :], AF.Exp)
            scale = 128 ** -0.5

            def load_band(k1, v1, krow, vcol):
                hr, hc = krow - 1024, vcol - 768
                for r in range(4):
                    load(P, k1[:, 64 * r:64 * r + 64], kg[r, krow:krow + 128, 2048:2112], q='pool')
                    load(P, v1[64 * (r % 2):64 * (r % 2) + 64, r // 2, :], vg[r, 2048:2112, vcol:vcol + 128], q='pool')
                load(P, k1[:, 256:512], khalo[hr:hr + 128, 0:256], q='pool')
                load(P, k1[:, 512:2560], kown[krow:krow + 128, 0:2048], q='pool')
                load(P, k1[:, 2560:2816], khalo[hr:hr + 128, 256:512], q='pool')
                load(P, v1[:, 2:4, :], vr(vhalo[0:256, hc:hc + 128]), q='pool')
                load(P, v1[:, 4:20, :], vr(vown[0:2048, vcol:vcol + 128]), q='pool')
                load(P, v1[:, 20:22, :], vr(vhalo[256:512, hc:hc + 128]), q='pool')

            def band_head(k1, v1, qrow, oh, nrel, koff, tab, sink):
                q1 = qw.next()
                load(P, q1[:, :], qT[qrow:qrow + 128, :], q='pool')
                for (t0, nt, who) in blocks:
                    steps = [([(k1[:, 128 * c:128 * c + 128], q1[:, t0:t0 + nt])], v1[:, c, :], None) for c in range(2)]
                    if who == 0:
                        Q = t0 // 512
                        for rel in range(nrel):
                            c = 2 + (koff + 512 * Q) // 128 + rel
                            steps.append(([(k1[:, 128 * c:128 * c + 128], q1[:, t0:t0 + nt])], v1[:, c, :], tab(Q, rel)))
                    attn_block(P, S, C, nt, steps, oT[128 * oh:128 * oh + 128, t0:t0 + nt], scale, sink=sink)

            for kvh in range(2):
                k1, v1 = kw.next(), vw.next()
                load_band(k1, v1, 1024 + 128 * kvh, 768 + 128 * kvh)
                for gi in range(2):
                    h = 2 * kvh + gi
                    band_head(k1, v1, 1280 + 128 * h, 8 + h, 6, 128, lambda Q, rel: Dm['wtab'][Q, rel], esink[:, h:h + 1])
            for h in range(4):
                k1, v1 = kw.next(), vw.next()
                load_band(k1, v1, 1280 + 128 * h, 1024 + 128 * h)
                band_head(k1, v1, 1792 + 128 * h, 12 + h, 8, 0, (lambda hh: (lambda Q, rel: Dm['ntab'][l, hh, Q, rel]))(h), None)


BIG = 1.0e4


def routing(P, S, C, lg, ntk, brb, comb):
    r = S.rt
    sc_, bi, t3, mb, mb2, e1, e2, w = (r[i] for i in range(8))
    g1, g2, gs, gmask, pen = (S.rs4[i] for i in range(5))
    gm, m1, m2, ws = (S.rs1[i] for i in range(4))
    act(P, sc_[:ntk, :], lg, AF.Sigmoid)
    tt(P, bi[:ntk, :], sc_[:ntk, :], brb[:ntk, :], ALU.add)
    v3 = lambda t: V(t, t.t[:ntk, :].rearrange("p (g e) -> p g e", g=4))
    reduce(P, g1[:ntk, :], v3(bi), ALU.max)
    for g in range(4):
        ts(P, t3[:ntk, 4 * g:4 * g + 4], bi[:ntk, 4 * g:4 * g + 4], g1[:ntk, g:g + 1], ALU.is_equal, -BIG, ALU.mult)
    tt(P, t3[:ntk, :], t3[:ntk, :], bi[:ntk, :], ALU.add)
    reduce(P, g2[:ntk, :], v3(t3), ALU.max)
    tt(P, gs[:ntk, :], g1[:ntk, :], g2[:ntk, :], ALU.add)
    reduce(P, gm[:ntk, :], gs[:ntk, :], ALU.max)
    ts(P, gmask[:ntk, :], gs[:ntk, :], gm[:ntk, 0:1], ALU.is_ge)
    ts(P, pen[:ntk, :], gmask[:ntk, :], 1.0, ALU.subtract, BIG, ALU.mult)
    for g in range(4):
        ts(P, mb[:ntk, 4 * g:4 * g + 4], bi[:ntk, 4 * g:4 * g + 4], gmask[:ntk, g:g + 1], ALU.mult, pen[:ntk, g:g + 1], ALU.add)
    reduce(P, m1[:ntk, :], mb[:ntk, :], ALU.max)
    ts(P, e1[:ntk, :], mb[:ntk, :], m1[:ntk, 0:1], ALU.is_equal)
    stt(P, mb2[:ntk, :], e1[:ntk, :], -BIG, mb[:ntk, :], ALU.mult, ALU.add)
    reduce(P, m2[:ntk, :], mb2[:ntk, :], ALU.max)
    ts(P, e2[:ntk, :], mb2[:ntk, :], m2[:ntk, 0:1], ALU.is_equal)
    tt(P, e1[:ntk, :], e1[:ntk, :], e2[:ntk, :], ALU.add)
    tt(P, w[:ntk, :], e1[:ntk, :], sc_[:ntk, :], ALU.mult)
    reduce(P, ws[:ntk, :], w[:ntk, :], ALU.add)
    recip(P, ws[:ntk, :], ws[:ntk, :])
    ts(P, comb, w[:ntk, :], ws[:ntk, 0:1], ALU.mult)


def phase3(P, l, Dm, C, xin, xout, last):
    blocks = BLOCKS[:4] if last else BLOCKS
    with P.scope() as sc:
        md = P.sb(sc, 'md3', [128, 6, KC, 2], F32)
        load(P, md[:, :, :, :], Dm['mod_d'][l])
        bg = P.sb(sc, 'bg', [128, 4, KC], F32)
        load(P, bg[:, :, :], Dm['bgateT'][l])
        wrt = P.sb(sc, 'wrt', [128, KC, 16], F32)
        load(P, wrt[:, :, :], Dm['wrT'])
        brb = P.sb(sc, 'brb', [128, 16], F32)
        load(P, brb[:, :], Dm['brb'])
        wr2 = [P.sb(sc, f'wr2{n}', [128, KC, 16], F32) for n in range(2)]
        cbr = [P.sb(sc, f'cbr{n}', [128, 16], F32) for n in range(2)]
        S = Consts()
        S.sq = P.ring(sc, 'sq3', 2, [128, 512], BF16)
        S.sd = P.ring(sc, 'sd3', 1, [128, 512], F32)
        S.rs = P.ring(sc, 'rs3', 1, [128, 512], F32)
        S.tmp = P.ring(sc, 'tmp3', 2, [128, 512], F32)
        S.pss = P.ring(sc, 'pss3', 1, [128, 512], F32, psum=True)
        psA = P.ring(sc, 'psA', 2, [128, 512], F32, psum=True)
        psB = P.ring(sc, 'psB', 2, [128, 512], F32, psum=True)
        psC = S.pss
        S.rt = [P.sb(sc, f'rt{i}', [128, 16], F32) for i in range(8)]
        S.rs4 = [P.sb(sc, f'rq{i}', [128, 4], F32) for i in range(5)]
        S.rs1 = [P.sb(sc, f'ro{i}', [128, 1], F32) for i in range(4)]
        shb = S.tmp
        for n in range(2):
            for kc in range(KC):
                ts(P, wr2[n][:, kc, :], wrt[:, kc, :], md[:, MD_A2, kc, n:n + 1], ALU.mult)
            pc = psC.next()
            for kc in range(KC):
                sb_ = shb.next()
                ts(P, sb_[:, 0:128], C.ones_f[:, :], md[:, MD_SH2, kc, n:n + 1], ALU.mult)
                mm(P, pc[:, 0:16], sb_[:, 0:128], wrt[:, kc, :], start=(kc == 0), stop=(kc == KC - 1))
            copy(P, cbr[n][:, :], pc[:, 0:16])
        xb = P.sb(sc, 'xb3', [128, KC, 512], F32)
        hb = P.sb(sc, 'hb3', [128, KC, 512], BF16)
        obr = P.ring(sc, 'ob3', 2, [128, 4, 512], BF16)
        accT = P.sb(sc, 'accT', [128, KC, 512], BF16)
        acc4 = P.sb(sc, 'acc4', [128, 4, 512], F32)
        gtr = P.ring(sc, 'gt', 2, [128, 512], BF16)
        hid = P.ring(sc, 'hid', 2, [128, 4, 512], BF16)
        sar = P.ring(sc, 'sa', 4, [128, 512], BF16)
        cbs = P.ring(sc, 'cbs', 2, [128, 512], F32)
        combT = P.sb(sc, 'combT', [16, 512], F32)
        comb = P.ring(sc, 'comb', 2, [128, 16], F32)
        lgr = P.ring(sc, 'lg', 2, [128, 16], F32)
        rst = P.ring(sc, 'rst', 2, [128, 1], F32)
        wring = P.ring(sc, 'wst', 3, [128, 8192], BF16)
        wringB = P.ring(sc, 'wstb', 2, [128, 8192], BF16)
        psO = P.ring(sc, 'psO', 3, [128, 512], F32, psum=True)
        cme = P.ring(sc, 'cme', 1, [16, 512], F32)
        wbr = P.ring(sc, 'wbr', 2, [128, 4, 512], BF16)
        v3 = lambda ap, k: ap.rearrange("p (k c) -> p k c", k=k)
        wgs = [[v3(Dm['wb_gate'][i, jg], KC) for jg in range(4)] for i in range(4)]
        wbs = [[v3(Dm['wb_branch'][i, jg], 4) for jg in range(4)] for i in range(4)]
        wos = [v3(Dm['wb_out'][jg], KC) for jg in range(4)]
        w1s = [v3(Dm['wb_w1'][e], KC) for e in range(16)]
        w3s = [v3(Dm['wb_w3'][e], KC) for e in range(16)]
        w2s = [v3(Dm['wb_w2'][e], 4) for e in range(16)]

        def wload(dst, src, nk):
            trk = getattr(dst, 'tile', dst)
            h = nk // 2
            for (k0, k1) in ((0, h), (h, nk)):
                P.dma('sync', (lambda o, i: (lambda e: e.dma_start(out=o, in_=i)))(dst.t[:, k0:k1, :], src[:, k0:k1, :]),
                      owner=trk, writes=[trk])

        def wslot(src, nk, ncol, c0, ring=None):
            t = (ring or wring).next()
            v = Tile3(t, nk, ncol)
            wload(v, src, nk)
            return v

        for (t0, nt, who) in blocks:
            xload(P, xb, xin, nt, t0)
            for kh in range(4):
                load(P, hb[:, 4 * kh:4 * kh + 4, :nt], Dm['hT_d'][:, 4 * kh:4 * kh + 4, t0:t0 + nt])
            for jg in range(4):
                for i in range(4):
                    wg = wslot(wgs[i][jg], KC, 512, 0)
                    wb = wbr.next()
                    wload(wb, wbs[i][jg], 4)
                    ob = obr.next()
                    load(P, ob[:, :, :nt], oT_view(Dm['oT_d'])[:, 4 * i:4 * i + 4, t0:t0 + nt])
                    for jj in range(4):
                        j = jg * 4 + jj
                        pg = psA.next()
                        for kc in range(KC):
                            mm(P, pg[:, :nt], wg[:, kc, jj * 128:jj * 128 + 128], hb[:, kc, :nt], start=(kc == 0), stop=(kc == KC - 1))
                        gt = gtr.next()
                        act(P, gt[:, :nt], pg[:, :nt], AF.Sigmoid, bias=bg[:, i, j:j + 1])
                        pb = psB.next()
                        for kc in range(4):
                            mm(P, pb[:, :nt], wb[:, kc, jj * 128:jj * 128 + 128], ob[:, kc, :nt], start=(kc == 0), stop=(kc == 3))
                        if i == 0:
                            tt(P, acc4[:, jj, :nt], gt[:, :nt], pb[:, :nt], ALU.mult)
                        else:
                            tm = S.tmp.next()
                            tt(P, tm[:, :nt], gt[:, :nt], pb[:, :nt], ALU.mult)
                            tt(P, acc4[:, jj, :nt], acc4[:, jj, :nt], tm[:, :nt], ALU.add)
                for jj in range(4):
                    copy(P, accT[:, jg * 4 + jj, :nt], acc4[:, jj, :nt], eng='act')
            for jg in range(4):
                wo = wslot(wos[jg], KC, 512, 0)
                for jj in range(4):
                    j = jg * 4 + jj
                    py = psA.next()
                    for kc in range(KC):
                        mm(P, py[:, :nt], wo[:, kc, jj * 128:jj * 128 + 128], accT[:, kc, :nt], start=(kc == 0), stop=(kc == KC - 1))
                    stt(P, xb[:, j, :nt], py[:, :nt], md[:, MD_G1, j, who:who + 1], xb[:, j, :nt], ALU.mult, ALU.add)
            rs = S.rs.next()
            norm_block(P, S, C, xb, nt, lambda kc, w: md[:, MD_A2, kc, w:w + 1], lambda kc, w: md[:, MD_SH2, kc, w:w + 1],
                       who, lambda kc: hb[:, kc, :nt], rs_out=rs)
            for sub in range((nt + 127) // 128):
                ntk = min(128, nt - 128 * sub)
                a, b = 128 * sub, 128 * sub + ntk
                pl = psB.next()
                for kc in range(KC):
                    mm(P, pl[:ntk, 0:16], xb[:, kc, a:b], wr2[who][:, kc, :], start=(kc == 0), stop=(kc == KC - 1))
                pr = psB.next()
                mm(P, pr[:ntk, 0:1], rs[0:1, a:b], C.ones_f[0:1, 0:1])
                rt_ = rst.next()
                copy(P, rt_[:ntk, :], pr[:ntk, 0:1])
                lg = lgr.next()
                stt(P, lg[:ntk, :], pl[:ntk, 0:16], rt_[:ntk, 0:1], cbr[who][:ntk, :], ALU.mult, ALU.add)
                cm = comb.next()
                routing(P, S, C, lg[:ntk, :], ntk, brb, cm[:ntk, :])
                pT = psB.next()
                transpose(P, pT[:16, :ntk], cm[:ntk, :], C.cf[:ntk, 0:ntk])
                copy(P, combT[:, a:b], pT[:16, :ntk])
            def stage_a1(f, w1, sas):
                pa = psA.next()
                for kc in range(KC):
                    mm(P, pa[:, :nt], w1[:, kc, f * 128:f * 128 + 128], hb[:, kc, :nt], start=(kc == 0), stop=(kc == KC - 1))
                sa = sar.next()
                act(P, sa[:, :nt], pa[:, :nt], AF.Silu)
                sas.append(sa)

            def stage_a2(f, w3, sas, cb_, hd):
                pb = psB.next()
                for kc in range(KC):
                    mm(P, pb[:, :nt], w3[:, kc, f * 128:f * 128 + 128], hb[:, kc, :nt], start=(kc == 0), stop=(kc == KC - 1))
                tm = S.tmp.next()
                tt(P, tm[:, :nt], sas[f][:, :nt], pb[:, :nt], ALU.mult)
                tt(P, hd[:, f, :nt], tm[:, :nt], cb_[:, :nt], ALU.mult)

            def stage_b_chunk(j, w2, hd):
                po = psO.next()
                for f in range(4):
                    mm(P, po[:, :nt], w2[:, f, j * 128:j * 128 + 128], hd[:, f, :nt], start=(f == 0), stop=(f == 3))
                stt(P, xb[:, j, :nt], po[:, :nt], md[:, MD_G2, j, who:who + 1], xb[:, j, :nt], ALU.mult, ALU.add)

            def comb_sel(e):
                ce = cme.next()
                ts(P, ce[:, :nt], combT[:, :nt], C.cf[:16, e:e + 1], ALU.mult)
                return ce

            def comb_bcast(ce):
                pc = psC.next()
                mm(P, pc[:, :nt], C.ones_f[:16, :], ce[:, :nt])
                cb_n = cbs.next()
                copy(P, cb_n[:, :nt], pc[:, :nt], eng='act')
                return cb_n

            prev = None
            cb_next = comb_bcast(comb_sel(0))
            for e in range(17):
                sas = []
                if e < 16:
                    w1 = wslot(w1s[e], KC, 512, 0)
                    w3 = wslot(w3s[e], KC, 512, 0)
                    w2 = wslot(w2s[e], 4, 2048, 0, ring=wringB)
                    cb_ = cb_next
                    ce_n = comb_sel(e + 1) if e < 15 else None
                    hd = hid.next()
                for slot in range(8):
                    if e < 16:
                        if slot < 4:
                            stage_a1(slot, w1, sas)
                        else:
                            stage_a2(slot - 4, w3, sas, cb_, hd)
                    if prev is not None:
                        for j in (2 * slot, 2 * slot + 1):
                            stage_b_chunk(j, prev[0], prev[1])
                if e < 15:
                    cb_next = comb_bcast(ce_n)
                prev = (w2, hd) if e < 16 else None
            if t0 + nt <= xout.shape[2]:
                for kh in range(4):
                    store(P, xout[:, 4 * kh:4 * kh + 4, t0:t0 + nt], xb[:, 4 * kh:4 * kh + 4, :nt])


def oT_view(ap):
    return ap.rearrange("(h p) t -> p h t", p=128)


class Tile3:
    def __init__(self, tile, nk, ncol):
        self.tile = tile
        self.t = tile.t[:, 0:nk * ncol].rearrange("p (k c) -> p k c", k=nk)

    def __getitem__(self, idx):
        return V(self.tile, self.t[idx])


LAYERED = {'bmodT', 'normT', 'w_mod', 'gains', 'mla_w_uq', 'mla_w_ukv_p', 'w_in', 'sinkT', 'ntab', 'bgateT',
           'w_gate', 'w_branch', 'w_out', 'moe_w1', 'moe_w3', 'moe_w2', 'mod_d'}
SHAPES = {
    'xT': ([128, KC, NT], F32), 'csT': ([128, KC, 2], F32), 'bmodT': ([128, 96], F32), 'normT': ([128, 2, KC], F32),
    'w_mod': ([24, 128, KC * 512], F32), 'gains': ([128, NGAIN], F32), 'rope': ([128, 4, NT], F32),
    'mla_w_uq': ([384, 768], F32), 'mla_w_ukv_p': ([256, 1024], F32), 'w_in': ([128, KC * DIN], F32),
    'cf': ([128, 128], F32), 'cb': ([128, 192], BF16), 'sinkT': ([128, 4], F32),
    'wtab': ([4, 6, 128, 512], F32), 'ntab': ([4, 4, 8, 128, 512], F32), 'bgateT': ([128, 4, KC], F32),
    'wrT': ([128, KC, 16], F32), 'brb': ([128, 16], F32), 'w_gate': ([4, 4, 128, KC * 512], F32), 'w_branch': ([4, 4, 128, 4 * 512], F32),
    'w_out': ([4, 128, KC * 512], F32), 'moe_w1': ([16, 128, KC * 512], F32), 'moe_w3': ([16, 128, KC * 512], F32), 'moe_w2': ([16, 128, 4 * D], F32),
    'mod_d': ([128, 6, KC, 2], F32), 'hT_d': ([128, KC, NT], BF16), 'qT_d': ([QROWS, NT], BF16),
    'k_own': ([KROWS, NT], BF16), 'v_own': ([NT, VCOLS], BF16), 'kg': ([4, KROWS, NT], BF16), 'vg': ([4, NT, VCOLS], BF16),
    'khalo': ([768, 512], BF16), 'vhalo': ([512, 768], BF16), 'oT_d': ([2048, NT], BF16),
    'xs1': ([128, KC, NT], F32), 'outT': ([128, KC, NLAT], F32),
    'wb_gate': ([4, 4, 128, KC * 512], BF16), 'wb_branch': ([4, 4, 128, 4 * 512], BF16), 'wb_out': ([4, 128, KC * 512], BF16),
    'wb_w1': ([16, 128, KC * 512], BF16), 'wb_w3': ([16, 128, KC * 512], BF16), 'wb_w2': ([16, 128, 4 * D], BF16),
}
PROD = {'mod_d': 'p0', 'hT_d': 'p1', 'qT_d': 'p1', 'k_own': 'p1', 'v_own': 'p1', 'oT_d': 'p2',
        'kg': 'ag', 'vg': 'ag', 'khalo': 'ag', 'vhalo': 'ag',
        'wb_gate': 'p2', 'wb_branch': 'p2', 'wb_out': 'p2', 'wb_w1': 'p2', 'wb_w3': 'p2', 'wb_w2': 'p2'}
CONS = {'mod_d': ['p1', 'p3'], 'hT_d': ['p3'], 'qT_d': ['p2'], 'k_own': ['p2', 'ag'], 'v_own': ['p2', 'ag'], 'oT_d': ['p3'],
        'kg': ['p2'], 'vg': ['p2'], 'khalo': ['p2'], 'vhalo': ['p2'],
        'wb_gate': ['p3'], 'wb_branch': ['p3'], 'wb_out': ['p3'], 'wb_w1': ['p3'], 'wb_w3': ['p3'], 'wb_w2': ['p3']}


class DramMap:
    def __init__(self, nc, launch, all_launches, li):
        self.nc, self.launch, self.all, self.li = nc, launch, all_launches, li
        self.t = {}
        self.inputs, self.outputs = [], []
        self.cur_layer = 0

    def _kind(self, base, l):
        if base in ('xs1',):
            prod, cons = ('p3', 0), [('p1', 1), ('p3', 1)]
        elif base == 'outT':
            return 'ExternalOutput'
        elif base in PROD:
            prod, cons = (PROD[base], l), [(c, l) for c in CONS[base]]
        else:
            return 'ExternalInput'
        here = prod in self.launch
        later = any(c in L for L in self.all[self.li + 1:] for c in cons)
        if here:
            return 'ExternalOutput' if later else 'Internal'
        return 'ExternalInput'

    def get(self, base, l=None):
        name = base if l is None else f'{base}{l}'
        if name not in self.t:
            shape, dt = SHAPES[base]
            kind = self._kind(base, l)
            self.t[name] = self.nc.dram_tensor(name, shape, dt, kind=kind).ap()
            if kind == 'ExternalInput':
                self.inputs.append(name)
            elif kind == 'ExternalOutput':
                self.outputs.append(name)
        return self.t[name]

    def __getitem__(self, base):
        if base in LAYERED:
            return _Lay(self, base)
        if base in PROD:
            return self.get(base, self.cur_layer)
        return self.get(base)


class _Lay:
    def __init__(self, dm, base):
        self.dm, self.base = dm, base

    def __getitem__(self, idx):
        if isinstance(idx, tuple):
            ap = self.dm.get(self.base, idx[0])
            return ap[idx[1:]] if len(idx) > 2 else ap[idx[1]]
        return self.dm.get(self.base, idx)


def build(launch, all_launches, li):
    nc = bass.Bass("TRN2", target_bir_lowering=False)
    with ExitStack() as es:
        P = Prog(nc, es)
        Dm = DramMap(nc, launch, all_launches, li)
        with P.scope() as sc:
            C = load_consts(P, sc, Dm)
            for (ph, l) in launch:
                Dm.cur_layer = l
                xin = Dm.get('xT') if l == 0 else Dm.get('xs1')
                if ph == 'p0':
                    phase0(P, l, Dm, C)
                elif ph == 'p1':
                    phase1(P, l, Dm, C, xin)
                elif ph == 'p2':
                    phase2(P, l, Dm, C, need_ctx=(l == 0))
                elif ph == 'p3':
                    xout = Dm.get('xs1') if l == 0 else Dm.get('outT')
                    phase3(P, l, Dm, C, xin, xout, last=(l == 1))
    return nc, Dm


def _fm(a):
    T = a.shape[0]
    return np.ascontiguousarray(a.reshape(T, KC, 128).transpose(2, 1, 0))


def _pk(w, nk):
    C_ = w.shape[1]
    return np.ascontiguousarray(w.reshape(nk, 128, C_).transpose(1, 0, 2).reshape(128, nk * C_))


def _pkcols(w, nk, cw):
    return np.stack([_pk(w[:, c:c + cw], nk) for c in range(0, w.shape[1], cw)])


def _rope_tables(s):
    tab = np.zeros((128, 4, NT), np.float32)
    tab[:, 0, :] = 1.0
    tab[:, 2, :] = 1.0
    t = np.arange(NLAT) + s * NLAT
    rows, cols = (t // 64).astype(np.float32), (t % 64).astype(np.float32)
    for (ci, si, half) in ((0, 1, 64), (2, 3, 32)):
        d2 = half // 2
        freqs = (np.float32(10000.0) ** (-np.arange(d2, dtype=np.float32) / np.float32(d2))).astype(np.float32)
        for part, pos in ((0, rows), (1, cols)):
            ang = (pos[None, :] * freqs[:, None]).astype(np.float32)
            c, sn = np.cos(ang).astype(np.float32), np.sin(ang).astype(np.float32)
            base = part * half
            tab[base:base + d2, ci, :NLAT] = c
            tab[base + d2:base + 2 * d2, ci, :NLAT] = c
            tab[base:base + d2, si, :NLAT] = sn
            tab[base + d2:base + 2 * d2, si, :NLAT] = sn
        if half == 32:
            tab[64:, ci, :] = 0.0
    return tab


def _rot_mats():
    import ml_dtypes
    cb = np.zeros((128, 192), np.float32)
    for (off, half) in ((0, 64), (128, 32)):
        d2 = half // 2
        for part in range(2):
            b = part * half
            for i in range(d2):
                m = b + i
                cb[m + d2, off + m] = -1.0
                m2 = b + d2 + i
                cb[m2 - d2, off + m2] = 1.0
    return cb.astype(ml_dtypes.bfloat16)


def _const_f():
    return np.eye(128, dtype=np.float32)


def _wtab(s):
    tab = np.full((4, 6, 128, 512), NEG, np.float32)
    k = np.arange(128)[:, None]
    q = np.arange(512)[None, :]
    for Q in range(4):
        for rel in range(6):
            kpos = -128 + 512 * Q + 128 * rel + k
            qpos = 512 * Q + q
            g = s * NLAT + kpos
            ok = (np.abs(qpos - kpos) <= 128) & (g >= 0) & (g < SEQ)
            tab[Q, rel][np.broadcast_to(ok, (128, 512))] = 0.0
    return tab


def _ntab(s, rpb):
    tab = np.full((4, 4, 8, 128, 512), NEG, np.float32)
    k = np.arange(128)[:, None]
    q = np.arange(512)[None, :]
    yy, cx = k // 64, k % 64
    rr, c = q // 64, q % 64
    cs = np.clip(c - 8, 0, 48)
    for Q in range(4):
        R = 32 * s + 8 * Q + rr
        rs = np.clip(R - 4, 0, 120)
        for rel in range(8):
            Y = 32 * s + 8 * Q - 4 + 2 * rel + yy
            ok = (Y >= rs) & (Y <= rs + 7) & (cx >= cs) & (cx <= cs + 15) & (Y >= 0) & (Y < 128)
            dy = np.clip(Y - R + 7, 0, 14)
            dx = np.clip(cx - c + 15, 0, 30)
            dyb, dxb, okb = np.broadcast_to(dy, (128, 512)), np.broadcast_to(dx, (128, 512)), np.broadcast_to(ok, (128, 512))
            for h in range(4):
                v = rpb[h][dyb, dxb]
                tab[h, Q, rel] = np.where(okb, v, np.float32(NEG))
    return tab


LAUNCHES = [[('p0', 0), ('p1', 0)], [('p2', 0), ('p3', 0), ('p0', 1), ('p1', 1)], [('p2', 1), ('p3', 1)]]


def _static_inputs(inp):
    f32 = lambda a: np.ascontiguousarray(np.asarray(a, dtype=np.float32))
    x, c, ctx, c_ctx = f32(inp['x']), f32(inp['c']), f32(inp['ctx']), f32(inp['c_ctx'])
    shared = {}
    shared['cf'] = _const_f()
    shared['cb'] = _rot_mats()
    shared['wrT'] = np.ascontiguousarray(f32(inp['w_router']).reshape(KC, 128, 16).transpose(1, 0, 2))
    shared['brb'] = np.ascontiguousarray(np.broadcast_to(f32(inp['b_router'])[None, :], (128, 16)))
    for l in range(2):
        shared[f'bmodT{l}'] = np.ascontiguousarray(f32(inp['b_mod'])[l].reshape(96, 128).T)
        nm = np.stack([f32(inp['norm_mix'])[l].reshape(KC, 128).T, f32(inp['norm_ffn'])[l].reshape(KC, 128).T], axis=1)
        shared[f'normT{l}'] = np.ascontiguousarray(nm)
        shared[f'w_mod{l}'] = _pkcols(f32(inp['w_mod'])[l], KC, 512)
        g = np.zeros((128, NGAIN), np.float32)
        for col, key in ((G_QA, 'qn_att'), (G_KA, 'kn_att'), (G_QW, 'qn_win'), (G_KW, 'kn_win'), (G_QN, 'qn_na'), (G_KN, 'kn_na')):
            g[:, col] = f32(inp[key])[l]
        g[:, G_QM0] = f32(inp['qn_mla'])[l][:128]
        g[:64, G_QM1] = f32(inp['qn_mla'])[l][128:]
        g[:, G_KM0] = f32(inp['kn_mla'])[l][:128]
        g[:64, G_KM1] = f32(inp['kn_mla'])[l][128:]
        g[:, G_CQ:G_CQ + 3] = f32(inp['mla_qa_norm'])[l].reshape(3, 128).T
        g[:, G_CKV:G_CKV + 2] = f32(inp['mla_kva_norm'])[l].reshape(2, 128).T
        shared[f'gains{l}'] = g
        shared[f'mla_w_uq{l}'] = f32(inp['mla_w_uq'])[l]
        wk = f32(inp['mla_w_ukv'])[l].reshape(256, 4, 256)
        shared[f'mla_w_ukv_p{l}'] = np.ascontiguousarray(np.concatenate([wk[:, :, :128].reshape(256, 512), wk[:, :, 128:].reshape(256, 512)], axis=1))
        wi = f32(inp['w_in'])[l]
        shared[f'w_in{l}'] = np.ascontiguousarray(np.concatenate([_pk(wi[:, c0:c0 + nc_], KC) for (c0, nc_, _k) in GROUPS], axis=1))
        shared[f'sinkT{l}'] = np.ascontiguousarray(np.broadcast_to(f32(inp['win_sink'])[l][None, :], (128, 4)))
        shared[f'bgateT{l}'] = np.ascontiguousarray(f32(inp['b_gate'])[l].reshape(4, KC, 128).transpose(2, 0, 1))
        shared[f'w_gate{l}'] = np.stack([_pkcols(f32(inp['w_gate'])[l, i], KC, 512) for i in range(4)])
        shared[f'w_branch{l}'] = np.stack([_pkcols(f32(inp['w_branch'])[l, i], 4, 512) for i in range(4)])
        shared[f'w_out{l}'] = _pkcols(f32(inp['w_out'])[l], KC, 512)
        shared[f'moe_w1{l}'] = np.stack([_pk(f32(inp['moe_w1'])[l, e], KC) for e in range(16)])
        shared[f'moe_w3{l}'] = np.stack([_pk(f32(inp['moe_w3'])[l, e], KC) for e in range(16)])
        shared[f'moe_w2{l}'] = np.stack([_pk(f32(inp['moe_w2'])[l, e], 4) for e in range(16)])
    per_core = []
    for core in range(NCORES):
        b, s = core // 4, core % 4
        d = {}
        xt = np.concatenate([x[b, s * NLAT:(s + 1) * NLAT], ctx[b, s * NCTX:(s + 1) * NCTX]], axis=0)
        d['xT'] = _fm(xt)
        d['csT'] = np.ascontiguousarray(np.stack([c[b], c_ctx], axis=0).reshape(2, KC, 128).transpose(2, 1, 0))
        d['rope'] = _rope_tables(s)
        d['wtab'] = _wtab(s)
        for l in range(2):
            d[f'ntab{l}'] = _ntab(s, f32(inp['na_rpb'])[l])
        per_core.append(d)
    return shared, per_core


def _gather(outs, l):
    res = []
    for core in range(NCORES):
        b, s = core // 4, core % 4
        grp = [outs[4 * b + r] for r in range(4)]
        d = {}
        d[f'kg{l}'] = np.stack([g[f'k_own{l}'] for g in grp], axis=0)
        d[f'vg{l}'] = np.stack([g[f'v_own{l}'] for g in grp], axis=0)
        ko, vo = outs[core][f'k_own{l}'], outs[core][f'v_own{l}']
        kh = np.zeros((768, 512), ko.dtype)
        vh = np.zeros((512, 768), vo.dtype)
        if s > 0:
            kh[:, 0:256] = grp[s - 1][f'k_own{l}'][1024:1792, NLAT - 256:NLAT]
            vh[0:256, :] = grp[s - 1][f'v_own{l}'][NLAT - 256:NLAT, 768:1536]
        if s < 3:
            kh[:, 256:512] = grp[s + 1][f'k_own{l}'][1024:1792, 0:256]
            vh[256:512, :] = grp[s + 1][f'v_own{l}'][0:256, 768:1536]
        d[f'khalo{l}'], d[f'vhalo{l}'] = kh, vh
        res.append(d)
    return res


_CACHE = {}


def kernel(**inputs):
    shared, per_core = _static_inputs(inputs)
    avail = [dict(shared, **per_core[c]) for c in range(NCORES)]
    final = None
    for li, launch in enumerate(LAUNCHES):
        if li not in _CACHE:
            _CACHE[li] = build(launch, LAUNCHES, li)
        nc, Dm = _CACHE[li]
        in_maps = [{n: avail[c][n] for n in Dm.inputs} for c in range(NCORES)]
        res = run_bass_kernel_spmd(nc, in_maps, core_ids=list(range(NCORES)))
        outs = res.results
        for c in range(NCORES):
            for n in Dm.outputs:
                avail[c][n] = outs[c][n]
        for (ph, l) in launch:
            if ph == 'p1':
                g = _gather(avail, l)
                for c in range(NCORES):
                    avail[c].update(g[c])
        final = outs
    out = np.zeros((2, SEQ, D), np.float32)
    for core in range(NCORES):
        b, s = core // 4, core % 4
        o = final[core]['outT']
        out[b, s * NLAT:(s + 1) * NLAT] = o.transpose(2, 1, 0).reshape(NLAT, D)
    return out
```

```python
import numpy as np
from contextlib import ExitStack
import concourse.bass as bass
import concourse.mybir as mybir
from concourse.bass_utils import run_bass_kernel_spmd

F32 = mybir.dt.float32
BF16 = mybir.dt.bfloat16
AF = mybir.ActivationFunctionType
ALU = mybir.AluOpType
AX = mybir.AxisListType

NCORES = 8
D = 2048
KC = 16
SEQ = 8192
NLAT = 2048
NCTX = 64
NT = NLAT + NCTX
CTX = 256
DIN = 4288
EPS = 1e-6
NEG = -30000.0
ENGS = ['sync', 'act', 'dve', 'pool', 'pe']
SAME_ENG_SYNC = True


class V:
    __slots__ = ('tile', 'ap')

    def __init__(self, tile, ap):
        self.tile, self.ap = tile, ap


class Tile:
    def __init__(self, t, name):
        self.t, self.name = t, name
        self.lw = None
        self.rd = {}
        self.semid = None
        self.cnt = 0

    def __getitem__(self, idx):
        return V(self, self.t[idx])


class Prog:
    def __init__(self, nc, es):
        self.nc, self.es = nc, es
        self.ops = {e: [] for e in ENGS}
        self.cops = {e: [] for e in ENGS}
        self.sem_pool = []
        self.semcnt = {}
        self.all_tiles = []
        self.uid = 0
        self.engsem = {e: es.enter_context(nc.semaphore('eng_' + e)) for e in ENGS}
        self.dsem = {}
        self.nsig = {e: 0 for e in ENGS}
        self.nassigned = {e: 0 for e in ENGS}
        self.waited = {e: {} for e in ENGS}
        self.sim_c, self.sim_d = {}, {}

    class Scope:
        def __init__(self, P):
            self.P, self.es, self.tiles = P, ExitStack(), []

        def __enter__(self):
            self.es.__enter__()
            return self

        def __exit__(self, *a):
            P = self.P
            P.barrier()
            P.flush()
            for t in self.tiles:
                if t.semid is not None:
                    P.sem_pool.append((t.semid, t.cnt))
                P.all_tiles.remove(t)
            return self.es.__exit__(*a)

    def scope(self):
        return Prog.Scope(self)

    def sb(self, sc, name, shape, dt):
        self.uid += 1
        name = f's{self.uid}_{name}'
        t = sc.es.enter_context(self.nc.sbuf_tensor(name, shape, dt))
        tl = Tile(t, name)
        self.all_tiles.append(tl)
        sc.tiles.append(tl)
        return tl

    def ps(self, sc, name, shape, dt=F32):
        self.uid += 1
        name = f'p{self.uid}_{name}'
        t = sc.es.enter_context(self.nc.psum_tensor(name, shape, dt))
        tl = Tile(t, name)
        self.all_tiles.append(tl)
        sc.tiles.append(tl)
        return tl

    def ring(self, sc, name, n, shape, dt, psum=False):
        return Ring([(self.ps if psum else self.sb)(sc, f'{name}{i}', shape, dt) for i in range(n)])

    def _deps(self, eng, reads, writes):
        deps = {}

        def add(tok):
            if tok is None:
                return
            if tok[0] == 'c':
                if tok[1] == eng and (eng == 'pe' or not SAME_ENG_SYNC):
                    return
                key = ('c', tok[1])
            else:
                key = ('d', tok[1])
            if deps.get(key, 0) < tok[2]:
                deps[key] = tok[2]

        for r in reads:
            add(r.lw)
        for w in writes:
            add(w.lw)
            for tok in w.rd.values():
                add(tok)
        return deps

    def op(self, eng, fn, reads=(), writes=()):
        reads = [r for r in reads if r is not None]
        deps = self._deps(eng, reads, writes)
        rec = dict(fn=fn, deps=deps, sig=False, kind='c')
        self.ops[eng].append(rec)
        self.cops[eng].append(rec)
        tok = ('c', eng, len(self.cops[eng]))
        for r in reads:
            r.rd[('c', eng)] = tok
        for w in writes:
            w.lw = tok
            w.rd = {}
        return rec

    def dma(self, q, fn, owner, reads=(), writes=()):
        deps = self._deps(q, reads, writes)
        if owner.semid is None:
            if self.sem_pool:
                owner.semid, owner.cnt = self.sem_pool.pop()
            else:
                owner.semid, owner.cnt = len(self.semcnt), 0
                self.dsem[owner.semid] = self.es.enter_context(self.nc.semaphore(f'd{owner.semid}'))
        owner.cnt += 1
        self.semcnt[owner.semid] = owner.cnt
        tok = ('d', owner.semid, owner.cnt)
        rec = dict(fn=fn, deps=deps, kind='d', semid=owner.semid)
        self.ops[q].append(rec)
        for r in reads:
            r.rd[('d', owner.semid)] = tok
        for w in writes:
            w.lw = tok
            w.rd = {}
        return rec

    def barrier(self):
        deps = {}
        for e in ENGS:
            if self.cops[e]:
                deps[('c', e)] = len(self.cops[e])
        for sid, c in self.semcnt.items():
            deps[('d', sid)] = c
        for e in ENGS:
            d = {k: v for k, v in deps.items() if k != ('c', e)}
            self.ops[e].append(dict(fn=None, deps=d, kind='b'))
        for t in self.all_tiles:
            t.lw = None
            t.rd = {}

    def check_progress(self):
        pos = {e: 0 for e in ENGS}
        moved = True
        while moved:
            moved = False
            for e in ENGS:
                ops = self.ops[e]
                while pos[e] < len(ops):
                    rec = ops[pos[e]]
                    ok = True
                    for key, val in rec['deps'].items():
                        have = self.sim_c.get(key[1], 0) if key[0] == 'c' else self.sim_d.get(key[1], 0)
                        if have < val:
                            ok = False
                            break
                    if not ok:
                        break
                    if rec['kind'] == 'c':
                        self.sim_c[e] = self.sim_c.get(e, 0) + 1
                    elif rec['kind'] == 'd':
                        self.sim_d[rec['semid']] = self.sim_d.get(rec['semid'], 0) + 1
                    pos[e] += 1
                    moved = True
        stuck = {e: (pos[e], len(self.ops[e])) for e in ENGS if pos[e] < len(self.ops[e])}
        assert not stuck, f"DEADLOCK in recorded program: {stuck}"

    def flush(self):
        nc = self.nc
        self.check_progress()
        for e in ENGS:
            for rec in self.ops[e]:
                for key, val in rec['deps'].items():
                    if key[0] == 'c':
                        self.cops[key[1]][val - 1]['sig'] = True
        for e in ENGS:
            for rec in self.cops[e][self.nassigned[e]:]:
                if rec['sig']:
                    self.nsig[e] += 1
                rec['signo'] = self.nsig[e]
            self.nassigned[e] = len(self.cops[e])
        engsem, dsem = self.engsem, self.dsem

        def run(ename, eng):
            waited = self.waited[ename]
            for rec in self.ops[ename]:
                for key, val in rec['deps'].items():
                    if key[0] == 'c':
                        sem = engsem[key[1]]
                        v = self.cops[key[1]][val - 1]['signo']
                    else:
                        sem = dsem[key[1]]
                        v = 16 * val
                    if waited.get(key, 0) >= v:
                        continue
                    waited[key] = v
                    eng.wait_ge(sem, v)
                if rec['fn'] is None:
                    continue
                ins = rec['fn'](eng)
                if rec['kind'] == 'd':
                    ins.then_inc(dsem[rec['semid']], 16)
                elif rec['sig']:
                    ins.then_inc(engsem[ename], 1)
            self.ops[ename] = []

        with nc.Block() as block:
            @block.sync
            def _(e):
                run('sync', e)

            @block.scalar
            def _(e):
                run('act', e)

            @block.vector
            def _(e):
                run('dve', e)

            @block.gpsimd
            def _(e):
                run('pool', e)

            @block.tensor
            def _(e):
                run('pe', e)


class Ring:
    def __init__(self, tiles):
        self.tiles, self.i = tiles, 0

    def next(self):
        t = self.tiles[self.i % len(self.tiles)]
        self.i += 1
        return t


def _t(*vs):
    return [v.tile for v in vs if isinstance(v, V)]


def _a(v):
    return v.ap if isinstance(v, V) else v


def mm(P, out, lhsT, rhs, start=True, stop=True):
    P.op('pe', lambda e: e.matmul(out.ap, lhsT.ap, rhs.ap, start=start, stop=stop),
         reads=_t(lhsT, rhs), writes=_t(out))


def transpose(P, out, in_, ident):
    P.op('pe', lambda e: e.transpose(out.ap, in_.ap, ident.ap), reads=_t(in_, ident), writes=_t(out))


def act(P, out, in_, func, scale=1.0, bias=None):
    if bias is None:
        P.op('act', lambda e: e.activation(out=out.ap, in_=in_.ap, func=func, scale=scale),
             reads=_t(in_), writes=_t(out))
    else:
        P.op('act', lambda e: e.activation(out=out.ap, in_=in_.ap, func=func, scale=scale, bias=_a(bias)),
             reads=_t(in_, bias), writes=_t(out))


def tt(P, out, in0, in1, op, eng='dve'):
    P.op(eng, lambda e: e.tensor_tensor(out=out.ap, in0=in0.ap, in1=in1.ap, op=op),
         reads=_t(in0, in1), writes=_t(out))


def ts(P, out, in0, s1, op0, s2=None, op1=None, eng='dve'):
    if op1 is None:
        P.op(eng, lambda e: e.tensor_scalar(out=out.ap, in0=in0.ap, scalar1=_a(s1), scalar2=None, op0=op0),
             reads=_t(in0, s1), writes=_t(out))
    else:
        P.op(eng, lambda e: e.tensor_scalar(out=out.ap, in0=in0.ap, scalar1=_a(s1), scalar2=_a(s2),
                                            op0=op0, op1=op1),
             reads=_t(in0, s1, s2), writes=_t(out))


def stt(P, out, in0, scalar, in1, op0, op1):
    P.op('dve', lambda e: e.scalar_tensor_tensor(out=out.ap, in0=in0.ap, scalar=_a(scalar), in1=in1.ap,
                                                 op0=op0, op1=op1),
         reads=_t(in0, scalar, in1), writes=_t(out))


def recip(P, out, in_):
    P.op('dve', lambda e: e.reciprocal(out=out.ap, in_=in_.ap), reads=_t(in_), writes=_t(out))


def copy(P, out, in_, eng='dve'):
    if eng == 'act':
        P.op('act', lambda e: e.activation(out=out.ap, in_=in_.ap, func=AF.Copy), reads=_t(in_), writes=_t(out))
    else:
        P.op(eng, lambda e: e.tensor_copy(out=out.ap, in_=in_.ap), reads=_t(in_), writes=_t(out))


def reduce(P, out, in_, op, axis=AX.X):
    P.op('dve', lambda e: e.tensor_reduce(out=out.ap, in_=in_.ap, axis=axis, op=op), reads=_t(in_), writes=_t(out))


def memset(P, out, val, eng='dve'):
    P.op(eng, lambda e: e.memset(out.ap, val), writes=_t(out))


def load(P, out, src, q='sync'):
    P.dma(q, lambda e: e.dma_start(out=out.ap, in_=src), owner=out.tile, writes=[out.tile])


def store(P, dst, in_, q='sync'):
    P.dma(q, lambda e: e.dma_start(out=dst, in_=in_.ap), owner=in_.tile, reads=[in_.tile])


BLOCKS = [(0, 512, 0), (512, 512, 0), (1024, 512, 0), (1536, 512, 0), (2048, 64, 1)]
GROUPS = [(0, 512, 'aq'), (512, 512, 'akv'), (1024, 704, 'mla'), (1728, 512, 'wq'), (2240, 512, 'wkv'),
          (2752, 512, 'nq'), (3264, 512, 'nk'), (3776, 512, 'nv')]
KROWS, VCOLS, QROWS = 1792, 1536, 2304
G_QA, G_KA, G_QW, G_KW, G_QN, G_KN, G_QM0, G_QM1, G_KM0, G_KM1, G_CQ, G_CKV = 0, 1, 2, 3, 4, 5, 6, 7, 8, 9, 10, 13
NGAIN = 15
MD_SH1, MD_A1, MD_G1, MD_SH2, MD_A2, MD_G2 = 0, 1, 2, 3, 4, 5


class Consts:
    pass


def load_consts(P, sc, Dm):
    C = Consts()
    C.ones_bf = P.sb(sc, 'ones_bf', [128, 128], BF16)
    memset(P, C.ones_bf[:, :], 1.0)
    C.ones_f = P.sb(sc, 'ones_f', [128, 128], F32)
    memset(P, C.ones_f[:, :], 1.0)
    C.eps = P.sb(sc, 'eps', [128, 1], F32)
    memset(P, C.eps[:, :], EPS)
    C.cf = P.sb(sc, 'cf', [128, 128], F32)
    load(P, C.cf[:, :], Dm['cf'])
    C.cb = P.sb(sc, 'cb', [128, 192], BF16)
    load(P, C.cb[:, :], Dm['cb'])
    return C


def norm_block(P, S, C, x, nt, Acol, shcol, who, dst, rs_out=None):
    pss = S.pss.next()
    for kc in range(KC):
        sq = S.sq.next()
        act(P, sq[:, :nt], x[:, kc, :nt], AF.Square)
        mm(P, pss[:, :nt], C.ones_bf[:, :], sq[:, :nt], start=(kc == 0), stop=(kc == KC - 1))
    sd = S.sd.next()
    act(P, sd[:, :nt], pss[:, :nt], AF.Sqrt, scale=1.0 / D, bias=C.eps[:, 0:1])
    rs = rs_out if rs_out is not None else S.rs.next()
    recip(P, rs[:, :nt], sd[:, :nt])
    for kc in range(KC):
        tmp = S.tmp.next()
        stt(P, tmp[:, :nt], x[:, kc, :nt], Acol(kc, who), rs[:, :nt], ALU.mult, ALU.mult)
        act(P, dst(kc), tmp[:, :nt], AF.Identity, bias=shcol(kc, who))
    return rs


def xload(P, dst_tile, src, nt, t0):
    for kh in range(4):
        load(P, dst_tile[:, 4 * kh:4 * kh + 4, :nt], src[:, 4 * kh:4 * kh + 4, t0:t0 + nt])


def wcast(P, dst_tile, src, nk, ncol, c0):
    step = max(1, nk // 4)
    for k0 in range(0, nk, step):
        k1 = min(nk, k0 + step)
        trk = getattr(dst_tile, 'tile', dst_tile)
        P.dma('pool', (lambda o, i: (lambda e: e.dma_start(out=o, in_=i)))(dst_tile.t[:, k0:k1, :ncol], src[:, k0:k1, c0:c0 + ncol]),
              owner=trk, writes=[trk])


def phase0(P, l, Dm, C):
    with P.scope() as sc:
        cs = P.sb(sc, 'cs', [128, KC, 2], F32)
        load(P, cs[:, :, :], Dm['csT'])
        act(P, cs[:, :, :], cs[:, :, :], AF.Silu)
        bm = P.sb(sc, 'bm', [128, 96], F32)
        load(P, bm[:, :], Dm['bmodT'][l])
        nrm = P.sb(sc, 'nrm', [128, 2, KC], F32)
        load(P, nrm[:, :, :], Dm['normT'][l])
        modT = P.sb(sc, 'modT', [128, 96, 2], F32)
        md = P.sb(sc, 'md', [128, 6, KC, 2], F32)
        csb = P.sb(sc, 'csb', [128, KC, 2], BF16)
        copy(P, csb[:, :, :], cs[:, :, :])
        wm = P.ring(sc, 'wm', 3, [128, KC, 512], BF16)
        psm = P.ring(sc, 'psm', 2, [128, 4, 2], F32, psum=True)
        for pc in range(24):
            w = wm.next()
            wcast(P, w, Dm['w_mod'][l, pc].rearrange("p (k c) -> p k c", k=KC), KC, 512, 0)
            pm = psm.next()
            for j in range(4):
                for kc in range(KC):
                    mm(P, pm[:, j, :], w[:, kc, j * 128:(j + 1) * 128], csb[:, kc, :], start=(kc == 0), stop=(kc == KC - 1))
            for n in range(2):
                tt(P, modT[:, pc * 4:pc * 4 + 4, n], pm[:, :, n], bm[:, pc * 4:pc * 4 + 4], ALU.add)
        for n in range(2):
            copy(P, md[:, MD_SH1, :, n], modT[:, 0:16, n])
            stt(P, md[:, MD_A1, :, n], modT[:, 16:32, n], 1.0, nrm[:, 0, :], ALU.add, ALU.mult)
            copy(P, md[:, MD_G1, :, n], modT[:, 32:48, n])
            copy(P, md[:, MD_SH2, :, n], modT[:, 48:64, n])
            stt(P, md[:, MD_A2, :, n], modT[:, 64:80, n], 1.0, nrm[:, 1, :], ALU.add, ALU.mult)
            copy(P, md[:, MD_G2, :, n], modT[:, 80:96, n])
        store(P, Dm['mod_d'][l], md[:, :, :, :])


def qk_unit_gen(P, S, C, pieces, Dtot, nt, t0):
    for pc in pieces:
        if 'proj' in pc:
            pc['src'] = pc['proj']()
    yield
    pss = S.pss.next()
    n = len(pieces)
    for i, pc in enumerate(pieces):
        dp = pc['dp']
        if pc.get('sq') is None:
            sq = S.sq.next()
            act(P, sq[:dp, :nt], pc['src'], AF.Square)
            sqv = sq[:dp, :nt]
        else:
            sqv = pc['sq']
        mm(P, pss[:, :nt], C.ones_bf[:dp, :], sqv, start=(i == 0), stop=(i == n - 1))
    yield
    sd = S.sd.next()
    act(P, sd[:, :nt], pss[:, :nt], AF.Sqrt, scale=1.0 / Dtot, bias=C.eps[:, 0:1])
    rs = S.rs.next()
    recip(P, rs[:, :nt], sd[:, :nt])
    ropes = []
    for pc in pieces:
        dp = pc['dp']
        kind, dst = pc['dst']
        if pc.get('rope') is None:
            if kind == 's':
                stt(P, dst, pc['src'], pc['gain'], rs[:dp, :nt], ALU.mult, ALU.mult)
            else:
                ob = S.ob.next()
                stt(P, ob[:dp, :nt], pc['src'], pc['gain'], rs[:dp, :nt], ALU.mult, ALU.mult)
                store(P, dst, ob[:dp, :nt])
        else:
            R, cos, sin = S.rope[pc['rope']]
            xn = S.xn.next()
            stt(P, xn[:dp, :nt], pc['src'], pc['gain'], rs[:dp, :nt], ALU.mult, ALU.mult)
            psr = S.psr.next()
            mm(P, psr[:dp, :nt], R, xn[:dp, :nt])
            ropes.append((pc, xn, psr, cos, sin))
    yield
    for (pc, xn, psr, cos, sin) in ropes:
        dp = pc['dp']
        kind, dst = pc['dst']
        t1 = S.t1.next()
        tt(P, t1[:dp, :nt], xn[:dp, :nt], cos[:dp, t0:t0 + nt], ALU.mult, eng='pool')
        t2 = S.t2.next()
        tt(P, t2[:dp, :nt], psr[:dp, :nt], sin[:dp, t0:t0 + nt], ALU.mult)
        ob = S.ob.next()
        tt(P, ob[:dp, :nt], t1[:dp, :nt], t2[:dp, :nt], ALU.add)
        store(P, dst, ob[:dp, :nt])


def qk_unit(P, S, C, pieces, Dtot, nt, t0):
    for _ in qk_unit_gen(P, S, C, pieces, Dtot, nt, t0):
        pass


class Pipe:
    def __init__(self):
        self.q = []

    def _advance(self):
        for g in reversed(list(self.q)):
            try:
                next(g)
            except StopIteration:
                self.q.remove(g)

    def push(self, g):
        next(g)
        self._advance()
        self.q.append(g)

    def flush(self):
        while self.q:
            self._advance()


def phase1(P, l, Dm, C, xin):
    with P.scope() as sc:
        hT = P.sb(sc, 'hT', [128, KC, NT], BF16)
        md = P.sb(sc, 'md1', [128, 6, KC, 2], F32)
        load(P, md[:, :, :, :], Dm['mod_d'][l])
        S = Consts()
        S.sq = P.ring(sc, 'sq', 3, [128, 512], BF16)
        S.sd = P.ring(sc, 'sd', 2, [128, 512], F32)
        S.rs = P.ring(sc, 'rs', 2, [128, 512], F32)
        S.pss = P.ring(sc, 'pss', 2, [128, 512], F32, psum=True)
        with P.scope() as s2:
            xb = P.ring(s2, 'xb', 2, [128, KC, 512], F32)
            S.tmp = P.ring(s2, 'tmp', 2, [128, 512], F32)
            for (t0, nt, who) in BLOCKS:
                x = xb.next()
                xload(P, x, xin, nt, t0)
                norm_block(P, S, C, x, nt, lambda kc, w: md[:, MD_A1, kc, w:w + 1], lambda kc, w: md[:, MD_SH1, kc, w:w + 1],
                           who, lambda kc: hT[:, kc, t0:t0 + nt])
                for kh in range(4):
                    store(P, Dm['hT_d'][:, 4 * kh:4 * kh + 4, t0:t0 + nt], hT[:, 4 * kh:4 * kh + 4, t0:t0 + nt])
        with P.scope() as s2:
            G = P.sb(s2, 'gains', [128, NGAIN], F32)
            load(P, G[:, :], Dm['gains'][l])
            rp = P.sb(s2, 'ropet', [128, 4, NT], F32)
            for i in range(4):
                load(P, rp[:, i, :], Dm['rope'][:, i, :])
            S.rope = {'r128': (C.cb[:, 0:128], rp.t[:, 0, :], rp.t[:, 1, :]), 'r64': (C.cb[:64, 128:192], rp.t[:, 2, :], rp.t[:, 3, :])}
            S.rope = {k: (v[0], _RV(rp, v[1]), _RV(rp, v[2])) for k, v in S.rope.items()}
            wuq = P.sb(s2, 'wuq', [128, 3, 768], BF16)
            wcast(P, wuq, Dm['mla_w_uq'][l].rearrange("(kc p) c -> p kc c", p=128), 3, 768, 0)
            wukv = P.sb(s2, 'wukv', [128, 2, 1024], BF16)
            wcast(P, wukv, Dm['mla_w_ukv_p'][l].rearrange("(kc p) c -> p kc c", p=128), 2, 1024, 0)
            wr = P.ring(s2, 'win', 2, [128, KC, 704], BF16)
            S.xn = P.ring(s2, 'xn', 2, [128, 512], BF16)
            S.t1 = P.ring(s2, 't1', 2, [128, 512], F32)
            S.t2 = P.ring(s2, 't2', 2, [128, 512], F32)
            S.ob = P.ring(s2, 'ob', 3, [128, 512], BF16)
            S.psr = P.ring(s2, 'psr', 2, [128, 512], F32, psum=True)
            proj = P.ring(s2, 'proj', 4, [128, 512], F32, psum=True)
            cqn = P.sb(s2, 'cqn', [128, 3, 512], BF16)
            ckvn = P.sb(s2, 'ckvn', [128, 2, 512], BF16)
            krf = P.sb(s2, 'krf', [64, 512], F32)
            krsq = P.sb(s2, 'krsq', [64, 512], BF16)
            vb = P.ring(s2, 'vb', 2, [128, 512], BF16)
            win_l = Dm['w_in'][l]
            qT, kO, vO = Dm['qT_d'], Dm['k_own'], Dm['v_own']

            def proj_fm(w, col, m, t0, nt, nk=KC, act_=None):
                ps = proj.next()
                for kc in range(nk):
                    a = hT[:, kc, t0:t0 + nt] if act_ is None else act_[:, kc, :nt]
                    mm(P, ps[:m, :nt], w[:, kc, col:col + m], a, start=(kc == 0), stop=(kc == nk - 1))
                return ps

            def proj_tm(w, col, ncols, t0, nt, vcol, nk=KC, act_=None):
                for sub in range((nt + 127) // 128):
                    ntk = min(128, nt - 128 * sub)
                    ps = proj.next()
                    for kc in range(nk):
                        a = hT[:, kc, t0 + 128 * sub:t0 + 128 * sub + ntk] if act_ is None else act_[:, kc, 128 * sub:128 * sub + ntk]
                        mm(P, ps[:ntk, :ncols], a, w[:, kc, col:col + ncols], start=(kc == 0), stop=(kc == nk - 1))
                    v = vb.next()
                    copy(P, v[:ntk, :ncols], ps[:ntk, :ncols], eng='act')
                    store(P, vO[t0 + 128 * sub:t0 + 128 * sub + ntk, vcol:vcol + ncols], v[:ntk, :ncols])

            pipe = Pipe()

            def heads(w, nh, col0, gcol, rope, dst, row0, t0, nt):
                for h in range(nh):
                    pj = (lambda c_=col0 + 128 * h: proj_fm(w, c_, 128, t0, nt)[:, :nt])
                    pipe.push(qk_unit_gen(P, S, C, [dict(proj=pj, dp=128, gain=G[:, gcol:gcol + 1], rope=rope,
                                                         dst=('d', dst[row0 + 128 * h:row0 + 128 * h + 128, t0:t0 + nt]))], 128, nt, t0))

            wts = {}

            def prefetch(gi):
                if gi < len(GROUPS) and gi not in wts:
                    wts[gi] = wr.next()
                    c0_, nc_ = GROUPS[gi][0], GROUPS[gi][1]
                    wcast(P, wts[gi], win_l[:, KC * c0_:KC * (c0_ + nc_)].rearrange("p (k c) -> p k c", k=KC), KC, nc_, 0)

            prefetch(0)
            for gi, (c0, ncol, kind) in enumerate(GROUPS):
                prefetch(gi + 1)
                w = wts.pop(gi)
                if kind in ('aq', 'akv', 'wq', 'wkv', 'nq', 'nk'):
                    for (t0, nt, who) in BLOCKS:
                        if kind == 'aq':
                            heads(w, 4, 0, G_QA, 'r128', qT, 0, t0, nt)
                        elif kind == 'akv':
                            heads(w, 2, 0, G_KA, 'r128', kO, 0, t0, nt)
                        elif kind == 'wq':
                            heads(w, 4, 0, G_QW, 'r128', qT, 1280, t0, nt)
                        elif kind == 'wkv':
                            heads(w, 2, 0, G_KW, 'r128', kO, 1024, t0, nt)
                        elif kind == 'nq':
                            heads(w, 4, 0, G_QN, None, qT, 1792, t0, nt)
                        elif kind == 'nk':
                            heads(w, 4, 0, G_KN, None, kO, 1280, t0, nt)
                    pipe.flush()
                for (t0, nt, who) in BLOCKS:
                    if kind == 'akv':
                        proj_tm(w, 256, 256, t0, nt, 0)
                    elif kind == 'wkv':
                        proj_tm(w, 256, 256, t0, nt, 768)
                    elif kind == 'nv':
                        proj_tm(w, 0, 512, t0, nt, 1024)
                    elif kind == 'mla':
                        pcs = [proj_fm(w, 128 * i, 128, t0, nt) for i in range(3)]
                        qk_unit(P, S, C, [dict(src=pcs[i][:, :nt], dp=128, gain=G[:, G_CQ + i:G_CQ + i + 1],
                                               dst=('s', cqn[:, i, :nt])) for i in range(3)], 384, nt, t0)
                        pcs = [proj_fm(w, 384 + 128 * i, 128, t0, nt) for i in range(2)]
                        qk_unit(P, S, C, [dict(src=pcs[i][:, :nt], dp=128, gain=G[:, G_CKV + i:G_CKV + i + 1],
                                               dst=('s', ckvn[:, i, :nt])) for i in range(2)], 256, nt, t0)
                        pk = proj_fm(w, 640, 64, t0, nt)
                        copy(P, krf[:, :nt], pk[:64, :nt])
                        act(P, krsq[:, :nt], pk[:64, :nt], AF.Square)
                        for h in range(4):
                            pn = proj_fm(wuq, 192 * h, 128, t0, nt, nk=3, act_=cqn)
                            pr = proj_fm(wuq, 192 * h + 128, 64, t0, nt, nk=3, act_=cqn)
                            r0 = 512 + 192 * h
                            qk_unit(P, S, C, [
                                dict(src=pn[:, :nt], dp=128, gain=G[:, G_QM0:G_QM0 + 1], dst=('d', qT[r0:r0 + 128, t0:t0 + nt])),
                                dict(src=pr[:64, :nt], dp=64, gain=G[:64, G_QM1:G_QM1 + 1], rope='r64',
                                     dst=('d', qT[r0 + 128:r0 + 192, t0:t0 + nt]))], 192, nt, t0)
                        for h in range(4):
                            pn = proj_fm(wukv, 128 * h, 128, t0, nt, nk=2, act_=ckvn)
                            r0 = 256 + 192 * h
                            qk_unit(P, S, C, [
                                dict(src=pn[:, :nt], dp=128, gain=G[:, G_KM0:G_KM0 + 1], dst=('d', kO[r0:r0 + 128, t0:t0 + nt])),
                                dict(src=krf[:, :nt], dp=64, gain=G[:64, G_KM1:G_KM1 + 1], rope='r64', sq=krsq[:, :nt],
                                     dst=('d', kO[r0 + 128:r0 + 192, t0:t0 + nt]))], 192, nt, t0)
                        proj_tm(wukv, 512, 512, t0, nt, 256, nk=2, act_=ckvn)


class _RV:
    def __init__(self, tile, ap):
        self.tile, self.ap = tile, ap

    def __getitem__(self, idx):
        return V(self.tile, self.ap[idx])


def attn_block(P, S, C, nt, steps, out_dram, scale, sink=None, LA=3):
    o_ps = S.ops_.next()
    d_ps = S.dps.next()
    n = len(steps)
    sts = {}

    def issue_st(i):
        st = S.st.next()
        kp = steps[i][0]
        for j, (kT, qv) in enumerate(kp):
            mm(P, st[:, :nt], kT, qv, start=(j == 0), stop=(j == len(kp) - 1))
        sts[i] = st

    tbs = {}
    for i, (kp, vch, bias) in enumerate(steps):
        if bias is not None:
            tbs[i] = S.tb.next()
            load(P, tbs[i][:, :nt], bias)
    for i in range(min(LA, n)):
        issue_st(i)
    for i, (kp, vch, bias) in enumerate(steps):
        if i + LA < n:
            issue_st(i + LA)
        st = sts.pop(i)
        pt = S.pt.next()
        if bias is None:
            act(P, pt[:, :nt], st[:, :nt], AF.Exp, scale=scale)
        else:
            tb = tbs[i]
            sf = S.sf.next()
            stt(P, sf[:, :nt], st[:, :nt], scale, tb[:, :nt], ALU.mult, ALU.add)
            act(P, pt[:, :nt], sf[:, :nt], AF.Exp)
        mm(P, o_ps[:, :nt], vch, pt[:, :nt], start=(i == 0), stop=(i == n - 1))
        mm(P, d_ps[:, :nt], C.ones_bf[:, :], pt[:, :nt], start=(i == 0), stop=(i == n - 1))
    rd = S.rd.next()
    if sink is not None:
        ts(P, rd[:, :nt], d_ps[:, :nt], sink, ALU.add)
        recip(P, rd[:, :nt], rd[:, :nt])
    else:
        recip(P, rd[:, :nt], d_ps[:, :nt])
    ob = S.ob.next()
    tt(P, ob[:, :nt], o_ps[:, :nt], rd[:, :nt], ALU.mult)
    store(P, out_dram, ob[:, :nt])


def convert_weights(P, l, Dm):
    cv = Tile(None, 'cvt')
    cv.base = 0
    r = lambda ap: ap.rearrange("p (a b) -> p a b", b=2048)

    jobs = []

    def cvd(dst, src):
        def job(o=r(dst), i=r(src)):
            na = o.shape[1]
            for (a0, a1) in ((0, na // 2), (na // 2, na)):
                if a1 == a0:
                    continue
                rec = P.dma('pool', (lambda oo, ii: (lambda e: e.dma_start(out=oo, in_=ii)))(o[:, a0:a1, :], i[:, a0:a1, :]), owner=cv)
                cv.base += 1
                if cv.base > 1:
                    rec['deps'][('d', cv.semid)] = cv.cnt - 1
        jobs.append(job)

    for jg in range(4):
        for i in range(4):
            cvd(Dm['wb_gate'][i, jg], Dm['w_gate'][l, i, jg])
            cvd(Dm['wb_branch'][i, jg], Dm['w_branch'][l, i, jg])
    for jg in range(4):
        cvd(Dm['wb_out'][jg], Dm['w_out'][l, jg])
    for e in range(16):
        cvd(Dm['wb_w1'][e], Dm['moe_w1'][l, e])
        cvd(Dm['wb_w3'][e], Dm['moe_w3'][l, e])
        cvd(Dm['wb_w2'][e], Dm['moe_w2'][l, e])
    return jobs


def phase2(P, l, Dm, C, need_ctx):
    blocks = BLOCKS if need_ctx else BLOCKS[:4]
    kg, vg, qT, oT = Dm['kg'], Dm['vg'], Dm['qT_d'], Dm['oT_d']
    kown, vown, khalo, vhalo = Dm['k_own'], Dm['v_own'], Dm['khalo'], Dm['vhalo']
    with P.scope() as sc:
        cjobs = convert_weights(P, l, Dm)
        S = Consts()
        S.st = P.ring(sc, 'st', 4, [128, 512], F32, psum=True)
        S.ops_ = P.ring(sc, 'ops', 2, [128, 512], F32, psum=True)
        S.dps = P.ring(sc, 'dps', 2, [128, 512], F32, psum=True)
        S.pt = P.ring(sc, 'pt', 6, [128, 512], BF16)
        S.tb = P.ring(sc, 'tb', 10, [128, 512], F32)
        S.sf = P.ring(sc, 'sf', 3, [128, 512], F32)
        S.rd = P.ring(sc, 'rd', 2, [128, 512], F32)
        S.ob = P.ring(sc, 'ob2', 3, [128, 512], BF16)
        vr = lambda ap: ap.rearrange("(c p) d -> p c d", p=128)
        with P.scope() as s2:
            kn = P.ring(s2, 'kn', 2, [128, 8448], BF16)
            kr = P.ring(s2, 'kr', 2, [64, 8448], BF16)
            vv = P.ring(s2, 'vv', 2, [128, 66, 128], BF16)
            qn = P.ring(s2, 'qn', 2, [128, NT], BF16)
            qr = P.ring(s2, 'qr', 2, [64, NT], BF16)

            def load_k(kt, row0, nr):
                for r in range(4):
                    load(P, kt[:nr, 256 + 2048 * r:256 + 2048 * (r + 1)], kg[r, row0:row0 + nr, 0:2048])
                    load(P, kt[:nr, 64 * r:64 * r + 64], kg[r, row0:row0 + nr, 2048:2112])

            def load_v(vt, col0):
                for r in range(4):
                    load(P, vt[:, 2 + 16 * r:2 + 16 * r + 16, :], vr(vg[r, 0:2048, col0:col0 + 128]))
                    load(P, vt[64 * (r % 2):64 * (r % 2) + 64, r // 2, :], vg[r, 2048:2112, col0:col0 + 128])

            units = [('A', 0), ('A', 1), ('M', 0), ('M', 1), ('M', 2), ('M', 3)]
            for ui, (mx, u) in enumerate(units):
                k1 = kn.next()
                v1 = vv.next()
                k2 = None
                if mx == 'A':
                    load_k(k1, 128 * u, 128)
                    load_v(v1, 128 * u)
                    qheads = [2 * u, 2 * u + 1]
                    scale = 128 ** -0.5
                else:
                    k2 = kr.next()
                    load_k(k1, 256 + 192 * u, 128)
                    load_k(k2, 256 + 192 * u + 128, 64)
                    load_v(v1, 256 + 128 * u)
                    qheads = [u]
                    scale = 192 ** -0.5
                if ui == 0:
                    for job in cjobs:
                        job()
                for h in qheads:
                    q1 = qn.next()
                    q2 = None
                    if mx == 'A':
                        load(P, q1[:, :], qT[128 * h:128 * h + 128, :])
                        oh = h
                    else:
                        q2 = qr.next()
                        load(P, q1[:, :], qT[512 + 192 * h:512 + 192 * h + 128, :])
                        load(P, q2[:, :], qT[512 + 192 * h + 128:512 + 192 * h + 192, :])
                        oh = 4 + h
                    for (t0, nt, who) in blocks:
                        steps = []
                        for c in (range(66) if who == 0 else range(2)):
                            kp = [(k1[:, 128 * c:128 * c + 128], q1[:, t0:t0 + nt])]
                            if k2 is not None:
                                kp.append((k2[:, 128 * c:128 * c + 128], q2[:, t0:t0 + nt]))
                            steps.append((kp, v1[:, c, :], None))
                        attn_block(P, S, C, nt, steps, oT[128 * oh:128 * oh + 128, t0:t0 + nt], scale)
        with P.scope() as s2:
            kw = P.ring(s2, 'kw', 2, [128, 2816], BF16)
            vw = P.ring(s2, 'vw', 2, [128, 22, 128], BF16)
            qw = P.ring(s2, 'qw', 2, [128, NT], BF16)
            esink = P.sb(s2, 'esink', [128, 4], F32)
            load(P, esink[:, :], Dm['sinkT'][l])
            act(P, esink[:, :], esink[:, :], AF.Exp)
            scale = 128 ** -0.5

            def load_band(k1, v1, krow, vcol):
                hr, hc = krow - 1024, vcol - 768
                for r in range(4):
                    load(P, k1[:, 64 * r:64 * r + 64], kg[r, krow:krow + 128, 2048:2112], q='pool')
                    load(P, v1[64 * (r % 2):64 * (r % 2) + 64, r // 2, :], vg[r, 2048:2112, vcol:vcol + 128], q='pool')
                load(P, k1[:, 256:512], khalo[hr:hr + 128, 0:256], q='pool')
                load(P, k1[:, 512:2560], kown[krow:krow + 128, 0:2048], q='pool')
                load(P, k1[:, 2560:2816], khalo[hr:hr + 128, 256:512], q='pool')
                load(P, v1[:, 2:4, :], vr(vhalo[0:256, hc:hc + 128]), q='pool')
                load(P, v1[:, 4:20, :], vr(vown[0:2048, vcol:vcol + 128]), q='pool')
                load(P, v1[:, 20:22, :], vr(vhalo[256:512, hc:hc + 128]), q='pool')

            def band_head(k1, v1, qrow, oh, nrel, koff, tab, sink):
                q1 = qw.next()
                load(P, q1[:, :], qT[qrow:qrow + 128, :], q='pool')
                for (t0, nt, who) in blocks:
                    steps = [([(k1[:, 128 * c:128 * c + 128], q1[:, t0:t0 + nt])], v1[:, c, :], None) for c in range(2)]
                    if who == 0:
                        Q = t0 // 512
                        for rel in range(nrel):
                            c = 2 + (koff + 512 * Q) // 128 + rel
                            steps.append(([(k1[:, 128 * c:128 * c + 128], q1[:, t0:t0 + nt])], v1[:, c, :], tab(Q, rel)))
                    attn_block(P, S, C, nt, steps, oT[128 * oh:128 * oh + 128, t0:t0 + nt], scale, sink=sink)

            for kvh in range(2):
                k1, v1 = kw.next(), vw.next()
                load_band(k1, v1, 1024 + 128 * kvh, 768 + 128 * kvh)
                for gi in range(2):
                    h = 2 * kvh + gi
                    band_head(k1, v1, 1280 + 128 * h, 8 + h, 6, 128, lambda Q, rel: Dm['wtab'][Q, rel], esink[:, h:h + 1])
            for h in range(4):
                k1, v1 = kw.next(), vw.next()
                load_band(k1, v1, 1280 + 128 * h, 1024 + 128 * h)
                band_head(k1, v1, 1792 + 128 * h, 12 + h, 8, 0, (lambda hh: (lambda Q, rel: Dm['ntab'][l, hh, Q, rel]))(h), None)


BIG = 1.0e4


def routing(P, S, C, lg, ntk, brb, comb):
    r = S.rt
    sc_, bi, t3, mb, mb2, e1, e2, w = (r[i] for i in range(8))
    g1, g2, gs, gmask, pen = (S.rs4[i] for i in range(5))
    gm, m1, m2, ws = (S.rs1[i] for i in range(4))
    act(P, sc_[:ntk, :], lg, AF.Sigmoid)
    tt(P, bi[:ntk, :], sc_[:ntk, :], brb[:ntk, :], ALU.add)
    v3 = lambda t: V(t, t.t[:ntk, :].rearrange("p (g e) -> p g e", g=4))
    reduce(P, g1[:ntk, :], v3(bi), ALU.max)
    for g in range(4):
        ts(P, t3[:ntk, 4 * g:4 * g + 4], bi[:ntk, 4 * g:4 * g + 4], g1[:ntk, g:g + 1], ALU.is_equal, -BIG, ALU.mult)
    tt(P, t3[:ntk, :], t3[:ntk, :], bi[:ntk, :], ALU.add)
    reduce(P, g2[:ntk, :], v3(t3), ALU.max)
    tt(P, gs[:ntk, :], g1[:ntk, :], g2[:ntk, :], ALU.add)
    reduce(P, gm[:ntk, :], gs[:ntk, :], ALU.max)
    ts(P, gmask[:ntk, :], gs[:ntk, :], gm[:ntk, 0:1], ALU.is_ge)
    ts(P, pen[:ntk, :], gmask[:ntk, :], 1.0, ALU.subtract, BIG, ALU.mult)
    for g in range(4):
        ts(P, mb[:ntk, 4 * g:4 * g + 4], bi[:ntk, 4 * g:4 * g + 4], gmask[:ntk, g:g + 1], ALU.mult, pen[:ntk, g:g + 1], ALU.add)
    reduce(P, m1[:ntk, :], mb[:ntk, :], ALU.max)
    ts(P, e1[:ntk, :], mb[:ntk, :], m1[:ntk, 0:1], ALU.is_equal)
    stt(P, mb2[:ntk, :], e1[:ntk, :], -BIG, mb[:ntk, :], ALU.mult, ALU.add)
    reduce(P, m2[:ntk, :], mb2[:ntk, :], ALU.max)
    ts(P, e2[:ntk, :], mb2[:ntk, :], m2[:ntk, 0:1], ALU.is_equal)
    tt(P, e1[:ntk, :], e1[:ntk, :], e2[:ntk, :], ALU.add)
    tt(P, w[:ntk, :], e1[:ntk, :], sc_[:ntk, :], ALU.mult)
    reduce(P, ws[:ntk, :], w[:ntk, :], ALU.add)
    recip(P, ws[:ntk, :], ws[:ntk, :])
    ts(P, comb, w[:ntk, :], ws[:ntk, 0:1], ALU.mult)


def phase3(P, l, Dm, C, xin, xout, last):
    blocks = BLOCKS[:4] if last else BLOCKS
    with P.scope() as sc:
        md = P.sb(sc, 'md3', [128, 6, KC, 2], F32)
        load(P, md[:, :, :, :], Dm['mod_d'][l])
        bg = P.sb(sc, 'bg', [128, 4, KC], F32)
        load(P, bg[:, :, :], Dm['bgateT'][l])
        wrt = P.sb(sc, 'wrt', [128, KC, 16], F32)
        load(P, wrt[:, :, :], Dm['wrT'])
        brb = P.sb(sc, 'brb', [128, 16], F32)
        load(P, brb[:, :], Dm['brb'])
        wr2 = [P.sb(sc, f'wr2{n}', [128, KC, 16], F32) for n in range(2)]
        cbr = [P.sb(sc, f'cbr{n}', [128, 16], F32) for n in range(2)]
        S = Consts()
        S.sq = P.ring(sc, 'sq3', 2, [128, 512], BF16)
        S.sd = P.ring(sc, 'sd3', 1, [128, 512], F32)
        S.rs = P.ring(sc, 'rs3', 1, [128, 512], F32)
        S.tmp = P.ring(sc, 'tmp3', 2, [128, 512], F32)
        S.pss = P.ring(sc, 'pss3', 1, [128, 512], F32, psum=True)
        psA = P.ring(sc, 'psA', 2, [128, 512], F32, psum=True)
        psB = P.ring(sc, 'psB', 2, [128, 512], F32, psum=True)
        psC = S.pss
        S.rt = [P.sb(sc, f'rt{i}', [128, 16], F32) for i in range(8)]
        S.rs4 = [P.sb(sc, f'rq{i}', [128, 4], F32) for i in range(5)]
        S.rs1 = [P.sb(sc, f'ro{i}', [128, 1], F32) for i in range(4)]
        shb = S.tmp
        for n in range(2):
            for kc in range(KC):
                ts(P, wr2[n][:, kc, :], wrt[:, kc, :], md[:, MD_A2, kc, n:n + 1], ALU.mult)
            pc = psC.next()
            for kc in range(KC):
                sb_ = shb.next()
                ts(P, sb_[:, 0:128], C.ones_f[:, :], md[:, MD_SH2, kc, n:n + 1], ALU.mult)
                mm(P, pc[:, 0:16], sb_[:, 0:128], wrt[:, kc, :], start=(kc == 0), stop=(kc == KC - 1))
            copy(P, cbr[n][:, :], pc[:, 0:16])
        xb = P.sb(sc, 'xb3', [128, KC, 512], F32)
        hb = P.sb(sc, 'hb3', [128, KC, 512], BF16)
        obr = P.ring(sc, 'ob3', 2, [128, 4, 512], BF16)
        accT = P.sb(sc, 'accT', [128, KC, 512], BF16)
        acc4 = P.sb(sc, 'acc4', [128, 4, 512], F32)
        gtr = P.ring(sc, 'gt', 2, [128, 512], BF16)
        hid = P.ring(sc, 'hid', 2, [128, 4, 512], BF16)
        sar = P.ring(sc, 'sa', 4, [128, 512], BF16)
        cbs = P.ring(sc, 'cbs', 2, [128, 512], F32)
        combT = P.sb(sc, 'combT', [16, 512], F32)
        comb = P.ring(sc, 'comb', 2, [128, 16], F32)
        lgr = P.ring(sc, 'lg', 2, [128, 16], F32)
        rst = P.ring(sc, 'rst', 2, [128, 1], F32)
        wring = P.ring(sc, 'wst', 3, [128, 8192], BF16)
        wringB = P.ring(sc, 'wstb', 2, [128, 8192], BF16)
        psO = P.ring(sc, 'psO', 3, [128, 512], F32, psum=True)
        cme = P.ring(sc, 'cme', 1, [16, 512], F32)
        wbr = P.ring(sc, 'wbr', 2, [128, 4, 512], BF16)
        v3 = lambda ap, k: ap.rearrange("p (k c) -> p k c", k=k)
        wgs = [[v3(Dm['wb_gate'][i, jg], KC) for jg in range(4)] for i in range(4)]
        wbs = [[v3(Dm['wb_branch'][i, jg], 4) for jg in range(4)] for i in range(4)]
        wos = [v3(Dm['wb_out'][jg], KC) for jg in range(4)]
        w1s = [v3(Dm['wb_w1'][e], KC) for e in range(16)]
        w3s = [v3(Dm['wb_w3'][e], KC) for e in range(16)]
        w2s = [v3(Dm['wb_w2'][e], 4) for e in range(16)]

        def wload(dst, src, nk):
            trk = getattr(dst, 'tile', dst)
            h = nk // 2
            for (k0, k1) in ((0, h), (h, nk)):
                P.dma('sync', (lambda o, i: (lambda e: e.dma_start(out=o, in_=i)))(dst.t[:, k0:k1, :], src[:, k0:k1, :]),
                      owner=trk, writes=[trk])

        def wslot(src, nk, ncol, c0, ring=None):
            t = (ring or wring).next()
            v = Tile3(t, nk, ncol)
            wload(v, src, nk)
            return v

        for (t0, nt, who) in blocks:
            xload(P, xb, xin, nt, t0)
            for kh in range(4):
                load(P, hb[:, 4 * kh:4 * kh + 4, :nt], Dm['hT_d'][:, 4 * kh:4 * kh + 4, t0:t0 + nt])
            for jg in range(4):
                for i in range(4):
                    wg = wslot(wgs[i][jg], KC, 512, 0)
                    wb = wbr.next()
                    wload(wb, wbs[i][jg], 4)
                    ob = obr.next()
                    load(P, ob[:, :, :nt], oT_view(Dm['oT_d'])[:, 4 * i:4 * i + 4, t0:t0 + nt])
                    for jj in range(4):
                        j = jg * 4 + jj
                        pg = psA.next()
                        for kc in range(KC):
                            mm(P, pg[:, :nt], wg[:, kc, jj * 128:jj * 128 + 128], hb[:, kc, :nt], start=(kc == 0), stop=(kc == KC - 1))
                        gt = gtr.next()
                        act(P, gt[:, :nt], pg[:, :nt], AF.Sigmoid, bias=bg[:, i, j:j + 1])
                        pb = psB.next()
                        for kc in range(4):
                            mm(P, pb[:, :nt], wb[:, kc, jj * 128:jj * 128 + 128], ob[:, kc, :nt], start=(kc == 0), stop=(kc == 3))
                        if i == 0:
                            tt(P, acc4[:, jj, :nt], gt[:, :nt], pb[:, :nt], ALU.mult)
                        else:
                            tm = S.tmp.next()
                            tt(P, tm[:, :nt], gt[:, :nt], pb[:, :nt], ALU.mult)
                            tt(P, acc4[:, jj, :nt], acc4[:, jj, :nt], tm[:, :nt], ALU.add)
                for jj in range(4):
                    copy(P, accT[:, jg * 4 + jj, :nt], acc4[:, jj, :nt], eng='act')
            for jg in range(4):
                wo = wslot(wos[jg], KC, 512, 0)
                for jj in range(4):
                    j = jg * 4 + jj
                    py = psA.next()
                    for kc in range(KC):
                        mm(P, py[:, :nt], wo[:, kc, jj * 128:jj * 128 + 128], accT[:, kc, :nt], start=(kc == 0), stop=(kc == KC - 1))
                    stt(P, xb[:, j, :nt], py[:, :nt], md[:, MD_G1, j, who:who + 1], xb[:, j, :nt], ALU.mult, ALU.add)
            rs = S.rs.next()
            norm_block(P, S, C, xb, nt, lambda kc, w: md[:, MD_A2, kc, w:w + 1], lambda kc, w: md[:, MD_SH2, kc, w:w + 1],
                       who, lambda kc: hb[:, kc, :nt], rs_out=rs)
            for sub in range((nt + 127) // 128):
                ntk = min(128, nt - 128 * sub)
                a, b = 128 * sub, 128 * sub + ntk
                pl = psB.next()
                for kc in range(KC):
                    mm(P, pl[:ntk, 0:16], xb[:, kc, a:b], wr2[who][:, kc, :], start=(kc == 0), stop=(kc == KC - 1))
                pr = psB.next()
                mm(P, pr[:ntk, 0:1], rs[0:1, a:b], C.ones_f[0:1, 0:1])
                rt_ = rst.next()
                copy(P, rt_[:ntk, :], pr[:ntk, 0:1])
                lg = lgr.next()
                stt(P, lg[:ntk, :], pl[:ntk, 0:16], rt_[:ntk, 0:1], cbr[who][:ntk, :], ALU.mult, ALU.add)
                cm = comb.next()
                routing(P, S, C, lg[:ntk, :], ntk, brb, cm[:ntk, :])
                pT = psB.next()
                transpose(P, pT[:16, :ntk], cm[:ntk, :], C.cf[:ntk, 0:ntk])
                copy(P, combT[:, a:b], pT[:16, :ntk])
            def stage_a1(f, w1, sas):
                pa = psA.next()
                for kc in range(KC):
                    mm(P, pa[:, :nt], w1[:, kc, f * 128:f * 128 + 128], hb[:, kc, :nt], start=(kc == 0), stop=(kc == KC - 1))
                sa = sar.next()
                act(P, sa[:, :nt], pa[:, :nt], AF.Silu)
                sas.append(sa)

            def stage_a2(f, w3, sas, cb_, hd):
                pb = psB.next()
                for kc in range(KC):
                    mm(P, pb[:, :nt], w3[:, kc, f * 128:f * 128 + 128], hb[:, kc, :nt], start=(kc == 0), stop=(kc == KC - 1))
                tm = S.tmp.next()
                tt(P, tm[:, :nt], sas[f][:, :nt], pb[:, :nt], ALU.mult)
                tt(P, hd[:, f, :nt], tm[:, :nt], cb_[:, :nt], ALU.mult)

            def stage_b_chunk(j, w2, hd):
                po = psO.next()
                for f in range(4):
                    mm(P, po[:, :nt], w2[:, f, j * 128:j * 128 + 128], hd[:, f, :nt], start=(f == 0), stop=(f == 3))
                stt(P, xb[:, j, :nt], po[:, :nt], md[:, MD_G2, j, who:who + 1], xb[:, j, :nt], ALU.mult, ALU.add)

            def comb_sel(e):
                ce = cme.next()
                ts(P, ce[:, :nt], combT[:, :nt], C.cf[:16, e:e + 1], ALU.mult)
                return ce

            def comb_bcast(ce):
                pc = psC.next()
                mm(P, pc[:, :nt], C.ones_f[:16, :], ce[:, :nt])
                cb_n = cbs.next()
                copy(P, cb_n[:, :nt], pc[:, :nt], eng='act')
                return cb_n

            prev = None
            cb_next = comb_bcast(comb_sel(0))
            for e in range(17):
                sas = []
                if e < 16:
                    w1 = wslot(w1s[e], KC, 512, 0)
                    w3 = wslot(w3s[e], KC, 512, 0)
                    w2 = wslot(w2s[e], 4, 2048, 0, ring=wringB)
                    cb_ = cb_next
                    ce_n = comb_sel(e + 1) if e < 15 else None
                    hd = hid.next()
                for slot in range(8):
                    if e < 16:
                        if slot < 4:
                            stage_a1(slot, w1, sas)
                        else:
                            stage_a2(slot - 4, w3, sas, cb_, hd)
                    if prev is not None:
                        for j in (2 * slot, 2 * slot + 1):
                            stage_b_chunk(j, prev[0], prev[1])
                if e < 15:
                    cb_next = comb_bcast(ce_n)
                prev = (w2, hd) if e < 16 else None
            if t0 + nt <= xout.shape[2]:
                for kh in range(4):
                    store(P, xout[:, 4 * kh:4 * kh + 4, t0:t0 + nt], xb[:, 4 * kh:4 * kh + 4, :nt])


def oT_view(ap):
    return ap.rearrange("(h p) t -> p h t", p=128)


class Tile3:
    def __init__(self, tile, nk, ncol):
        self.tile = tile
        self.t = tile.t[:, 0:nk * ncol].rearrange("p (k c) -> p k c", k=nk)

    def __getitem__(self, idx):
        return V(self.tile, self.t[idx])


LAYERED = {'bmodT', 'normT', 'w_mod', 'gains', 'mla_w_uq', 'mla_w_ukv_p', 'w_in', 'sinkT', 'ntab', 'bgateT',
           'w_gate', 'w_branch', 'w_out', 'moe_w1', 'moe_w3', 'moe_w2', 'mod_d'}
SHAPES = {
    'xT': ([128, KC, NT], F32), 'csT': ([128, KC, 2], F32), 'bmodT': ([128, 96], F32), 'normT': ([128, 2, KC], F32),
    'w_mod': ([24, 128, KC * 512], F32), 'gains': ([128, NGAIN], F32), 'rope': ([128, 4, NT], F32),
    'mla_w_uq': ([384, 768], F32), 'mla_w_ukv_p': ([256, 1024], F32), 'w_in': ([128, KC * DIN], F32),
    'cf': ([128, 128], F32), 'cb': ([128, 192], BF16), 'sinkT': ([128, 4], F32),
    'wtab': ([4, 6, 128, 512], F32), 'ntab': ([4, 4, 8, 128, 512], F32), 'bgateT': ([128, 4, KC], F32),
    'wrT': ([128, KC, 16], F32), 'brb': ([128, 16], F32), 'w_gate': ([4, 4, 128, KC * 512], F32), 'w_branch': ([4, 4, 128, 4 * 512], F32),
    'w_out': ([4, 128, KC * 512], F32), 'moe_w1': ([16, 128, KC * 512], F32), 'moe_w3': ([16, 128, KC * 512], F32), 'moe_w2': ([16, 128, 4 * D], F32),
    'mod_d': ([128, 6, KC, 2], F32), 'hT_d': ([128, KC, NT], BF16), 'qT_d': ([QROWS, NT], BF16),
    'k_own': ([KROWS, NT], BF16), 'v_own': ([NT, VCOLS], BF16), 'kg': ([4, KROWS, NT], BF16), 'vg': ([4, NT, VCOLS], BF16),
    'khalo': ([768, 512], BF16), 'vhalo': ([512, 768], BF16), 'oT_d': ([2048, NT], BF16),
    'xs1': ([128, KC, NT], F32), 'outT': ([128, KC, NLAT], F32),
    'wb_gate': ([4, 4, 128, KC * 512], BF16), 'wb_branch': ([4, 4, 128, 4 * 512], BF16), 'wb_out': ([4, 128, KC * 512], BF16),
    'wb_w1': ([16, 128, KC * 512], BF16), 'wb_w3': ([16, 128, KC * 512], BF16), 'wb_w2': ([16, 128, 4 * D], BF16),
}
PROD = {'mod_d': 'p0', 'hT_d': 'p1', 'qT_d': 'p1', 'k_own': 'p1', 'v_own': 'p1', 'oT_d': 'p2',
        'kg': 'ag', 'vg': 'ag', 'khalo': 'ag', 'vhalo': 'ag',
        'wb_gate': 'p2', 'wb_branch': 'p2', 'wb_out': 'p2', 'wb_w1': 'p2', 'wb_w3': 'p2', 'wb_w2': 'p2'}
CONS = {'mod_d': ['p1', 'p3'], 'hT_d': ['p3'], 'qT_d': ['p2'], 'k_own': ['p2', 'ag'], 'v_own': ['p2', 'ag'], 'oT_d': ['p3'],
        'kg': ['p2'], 'vg': ['p2'], 'khalo': ['p2'], 'vhalo': ['p2'],
        'wb_gate': ['p3'], 'wb_branch': ['p3'], 'wb_out': ['p3'], 'wb_w1': ['p3'], 'wb_w3': ['p3'], 'wb_w2': ['p3']}


class DramMap:
    def __init__(self, nc, launch, all_launches, li):
        self.nc, self.launch, self.all, self.li = nc, launch, all_launches, li
        self.t = {}
        self.inputs, self.outputs = [], []
        self.cur_layer = 0

    def _kind(self, base, l):
        if base in ('xs1',):
            prod, cons = ('p3', 0), [('p1', 1), ('p3', 1)]
        elif base == 'outT':
            return 'ExternalOutput'
        elif base in PROD:
            prod, cons = (PROD[base], l), [(c, l) for c in CONS[base]]
        else:
            return 'ExternalInput'
        here = prod in self.launch
        later = any(c in L for L in self.all[self.li + 1:] for c in cons)
        if here:
            return 'ExternalOutput' if later else 'Internal'
        return 'ExternalInput'

    def get(self, base, l=None):
        name = base if l is None else f'{base}{l}'
        if name not in self.t:
            shape, dt = SHAPES[base]
            kind = self._kind(base, l)
            self.t[name] = self.nc.dram_tensor(name, shape, dt, kind=kind).ap()
            if kind == 'ExternalInput':
                self.inputs.append(name)
            elif kind == 'ExternalOutput':
                self.outputs.append(name)
        return self.t[name]

    def __getitem__(self, base):
        if base in LAYERED:
            return _Lay(self, base)
        if base in PROD:
            return self.get(base, self.cur_layer)
        return self.get(base)


class _Lay:
    def __init__(self, dm, base):
        self.dm, self.base = dm, base

    def __getitem__(self, idx):
        if isinstance(idx, tuple):
            ap = self.dm.get(self.base, idx[0])
            return ap[idx[1:]] if len(idx) > 2 else ap[idx[1]]
        return self.dm.get(self.base, idx)


def build(launch, all_launches, li):
    nc = bass.Bass("TRN2", target_bir_lowering=False)
    with ExitStack() as es:
        P = Prog(nc, es)
        Dm = DramMap(nc, launch, all_launches, li)
        with P.scope() as sc:
            C = load_consts(P, sc, Dm)
            for (ph, l) in launch:
                Dm.cur_layer = l
                xin = Dm.get('xT') if l == 0 else Dm.get('xs1')
                if ph == 'p0':
                    phase0(P, l, Dm, C)
                elif ph == 'p1':
                    phase1(P, l, Dm, C, xin)
                elif ph == 'p2':
                    phase2(P, l, Dm, C, need_ctx=(l == 0))
                elif ph == 'p3':
                    xout = Dm.get('xs1') if l == 0 else Dm.get('outT')
                    phase3(P, l, Dm, C, xin, xout, last=(l == 1))
    return nc, Dm


def _fm(a):
    T = a.shape[0]
    return np.ascontiguousarray(a.reshape(T, KC, 128).transpose(2, 1, 0))


def _pk(w, nk):
    C_ = w.shape[1]
    return np.ascontiguousarray(w.reshape(nk, 128, C_).transpose(1, 0, 2).reshape(128, nk * C_))


def _pkcols(w, nk, cw):
    return np.stack([_pk(w[:, c:c + cw], nk) for c in range(0, w.shape[1], cw)])


def _rope_tables(s):
    tab = np.zeros((128, 4, NT), np.float32)
    tab[:, 0, :] = 1.0
    tab[:, 2, :] = 1.0
    t = np.arange(NLAT) + s * NLAT
    rows, cols = (t // 64).astype(np.float32), (t % 64).astype(np.float32)
    for (ci, si, half) in ((0, 1, 64), (2, 3, 32)):
        d2 = half // 2
        freqs = (np.float32(10000.0) ** (-np.arange(d2, dtype=np.float32) / np.float32(d2))).astype(np.float32)
        for part, pos in ((0, rows), (1, cols)):
            ang = (pos[None, :] * freqs[:, None]).astype(np.float32)
            c, sn = np.cos(ang).astype(np.float32), np.sin(ang).astype(np.float32)
            base = part * half
            tab[base:base + d2, ci, :NLAT] = c
            tab[base + d2:base + 2 * d2, ci, :NLAT] = c
            tab[base:base + d2, si, :NLAT] = sn
            tab[base + d2:base + 2 * d2, si, :NLAT] = sn
        if half == 32:
            tab[64:, ci, :] = 0.0
    return tab


def _rot_mats():
    import ml_dtypes
    cb = np.zeros((128, 192), np.float32)
    for (off, half) in ((0, 64), (128, 32)):
        d2 = half // 2
        for part in range(2):
            b = part * half
            for i in range(d2):
                m = b + i
                cb[m + d2, off + m] = -1.0
                m2 = b + d2 + i
                cb[m2 - d2, off + m2] = 1.0
    return cb.astype(ml_dtypes.bfloat16)


def _const_f():
    return np.eye(128, dtype=np.float32)


def _wtab(s):
    tab = np.full((4, 6, 128, 512), NEG, np.float32)
    k = np.arange(128)[:, None]
    q = np.arange(512)[None, :]
    for Q in range(4):
        for rel in range(6):
            kpos = -128 + 512 * Q + 128 * rel + k
            qpos = 512 * Q + q
            g = s * NLAT + kpos
            ok = (np.abs(qpos - kpos) <= 128) & (g >= 0) & (g < SEQ)
            tab[Q, rel][np.broadcast_to(ok, (128, 512))] = 0.0
    return tab


def _ntab(s, rpb):
    tab = np.full((4, 4, 8, 128, 512), NEG, np.float32)
    k = np.arange(128)[:, None]
    q = np.arange(512)[None, :]
    yy, cx = k // 64, k % 64
    rr, c = q // 64, q % 64
    cs = np.clip(c - 8, 0, 48)
    for Q in range(4):
        R = 32 * s + 8 * Q + rr
        rs = np.clip(R - 4, 0, 120)
        for rel in range(8):
            Y = 32 * s + 8 * Q - 4 + 2 * rel + yy
            ok = (Y >= rs) & (Y <= rs + 7) & (cx >= cs) & (cx <= cs + 15) & (Y >= 0) & (Y < 128)
            dy = np.clip(Y - R + 7, 0, 14)
            dx = np.clip(cx - c + 15, 0, 30)
            dyb, dxb, okb = np.broadcast_to(dy, (128, 512)), np.broadcast_to(dx, (128, 512)), np.broadcast_to(ok, (128, 512))
            for h in range(4):
                v = rpb[h][dyb, dxb]
                tab[h, Q, rel] = np.where(okb, v, np.float32(NEG))
    return tab


LAUNCHES = [[('p0', 0), ('p1', 0)], [('p2', 0), ('p3', 0), ('p0', 1), ('p1', 1)], [('p2', 1), ('p3', 1)]]


def _static_inputs(inp):
    f32 = lambda a: np.ascontiguousarray(np.asarray(a, dtype=np.float32))
    x, c, ctx, c_ctx = f32(inp['x']), f32(inp['c']), f32(inp['ctx']), f32(inp['c_ctx'])
    shared = {}
    shared['cf'] = _const_f()
    shared['cb'] = _rot_mats()
    shared['wrT'] = np.ascontiguousarray(f32(inp['w_router']).reshape(KC, 128, 16).transpose(1, 0, 2))
    shared['brb'] = np.ascontiguousarray(np.broadcast_to(f32(inp['b_router'])[None, :], (128, 16)))
    for l in range(2):
        shared[f'bmodT{l}'] = np.ascontiguousarray(f32(inp['b_mod'])[l].reshape(96, 128).T)
        nm = np.stack([f32(inp['norm_mix'])[l].reshape(KC, 128).T, f32(inp['norm_ffn'])[l].reshape(KC, 128).T], axis=1)
        shared[f'normT{l}'] = np.ascontiguousarray(nm)
        shared[f'w_mod{l}'] = _pkcols(f32(inp['w_mod'])[l], KC, 512)
        g = np.zeros((128, NGAIN), np.float32)
        for col, key in ((G_QA, 'qn_att'), (G_KA, 'kn_att'), (G_QW, 'qn_win'), (G_KW, 'kn_win'), (G_QN, 'qn_na'), (G_KN, 'kn_na')):
            g[:, col] = f32(inp[key])[l]
        g[:, G_QM0] = f32(inp['qn_mla'])[l][:128]
        g[:64, G_QM1] = f32(inp['qn_mla'])[l][128:]
        g[:, G_KM0] = f32(inp['kn_mla'])[l][:128]
        g[:64, G_KM1] = f32(inp['kn_mla'])[l][128:]
        g[:, G_CQ:G_CQ + 3] = f32(inp['mla_qa_norm'])[l].reshape(3, 128).T
        g[:, G_CKV:G_CKV + 2] = f32(inp['mla_kva_norm'])[l].reshape(2, 128).T
        shared[f'gains{l}'] = g
        shared[f'mla_w_uq{l}'] = f32(inp['mla_w_uq'])[l]
        wk = f32(inp['mla_w_ukv'])[l].reshape(256, 4, 256)
        shared[f'mla_w_ukv_p{l}'] = np.ascontiguousarray(np.concatenate([wk[:, :, :128].reshape(256, 512), wk[:, :, 128:].reshape(256, 512)], axis=1))
        wi = f32(inp['w_in'])[l]
        shared[f'w_in{l}'] = np.ascontiguousarray(np.concatenate([_pk(wi[:, c0:c0 + nc_], KC) for (c0, nc_, _k) in GROUPS], axis=1))
        shared[f'sinkT{l}'] = np.ascontiguousarray(np.broadcast_to(f32(inp['win_sink'])[l][None, :], (128, 4)))
        shared[f'bgateT{l}'] = np.ascontiguousarray(f32(inp['b_gate'])[l].reshape(4, KC, 128).transpose(2, 0, 1))
        shared[f'w_gate{l}'] = np.stack([_pkcols(f32(inp['w_gate'])[l, i], KC, 512) for i in range(4)])
        shared[f'w_branch{l}'] = np.stack([_pkcols(f32(inp['w_branch'])[l, i], 4, 512) for i in range(4)])
        shared[f'w_out{l}'] = _pkcols(f32(inp['w_out'])[l], KC, 512)
        shared[f'moe_w1{l}'] = np.stack([_pk(f32(inp['moe_w1'])[l, e], KC) for e in range(16)])
        shared[f'moe_w3{l}'] = np.stack([_pk(f32(inp['moe_w3'])[l, e], KC) for e in range(16)])
        shared[f'moe_w2{l}'] = np.stack([_pk(f32(inp['moe_w2'])[l, e], 4) for e in range(16)])
    per_core = []
    for core in range(NCORES):
        b, s = core // 4, core % 4
        d = {}
        xt = np.concatenate([x[b, s * NLAT:(s + 1) * NLAT], ctx[b, s * NCTX:(s + 1) * NCTX]], axis=0)
        d['xT'] = _fm(xt)
        d['csT'] = np.ascontiguousarray(np.stack([c[b], c_ctx], axis=0).reshape(2, KC, 128).transpose(2, 1, 0))
        d['rope'] = _rope_tables(s)
        d['wtab'] = _wtab(s)
        for l in range(2):
            d[f'ntab{l}'] = _ntab(s, f32(inp['na_rpb'])[l])
        per_core.append(d)
    return shared, per_core


def _gather(outs, l):
    res = []
    for core in range(NCORES):
        b, s = core // 4, core % 4
        grp = [outs[4 * b + r] for r in range(4)]
        d = {}
        d[f'kg{l}'] = np.stack([g[f'k_own{l}'] for g in grp], axis=0)
        d[f'vg{l}'] = np.stack([g[f'v_own{l}'] for g in grp], axis=0)
        ko, vo = outs[core][f'k_own{l}'], outs[core][f'v_own{l}']
        kh = np.zeros((768, 512), ko.dtype)
        vh = np.zeros((512, 768), vo.dtype)
        if s > 0:
            kh[:, 0:256] = grp[s - 1][f'k_own{l}'][1024:1792, NLAT - 256:NLAT]
            vh[0:256, :] = grp[s - 1][f'v_own{l}'][NLAT - 256:NLAT, 768:1536]
        if s < 3:
            kh[:, 256:512] = grp[s + 1][f'k_own{l}'][1024:1792, 0:256]
            vh[256:512, :] = grp[s + 1][f'v_own{l}'][0:256, 768:1536]
        d[f'khalo{l}'], d[f'vhalo{l}'] = kh, vh
        res.append(d)
    return res


_CACHE = {}


def _launch(nc, in_maps):
    try:
        return run_bass_kernel_spmd(nc, in_maps, core_ids=list(range(NCORES)))
    except Exception as e:
        msg = str(e)
        if 'UNAVAILABLE' not in msg and 'unrecoverable' not in msg:
            raise
        import time
        time.sleep(10.0)
        return run_bass_kernel_spmd(nc, in_maps, core_ids=list(range(NCORES)))


def kernel(**inputs):
    shared, per_core = _static_inputs(inputs)
    avail = [dict(shared, **per_core[c]) for c in range(NCORES)]
    final = None
    for li, launch in enumerate(LAUNCHES):
        if li not in _CACHE:
            _CACHE[li] = build(launch, LAUNCHES, li)
        nc, Dm = _CACHE[li]
        in_maps = [{n: avail[c][n] for n in Dm.inputs} for c in range(NCORES)]
        res = _launch(nc, in_maps)
        outs = res.results
        for c in range(NCORES):
            for n in Dm.outputs:
                avail[c][n] = outs[c][n]
        for (ph, l) in launch:
            if ph == 'p1':
                g = _gather(avail, l)
                for c in range(NCORES):
                    avail[c].update(g[c])
        final = outs
    out = np.zeros((2, SEQ, D), np.float32)
    for core in range(NCORES):
        b, s = core // 4, core % 4
        o = final[core]['outT']
        out[b, s * NLAT:(s + 1) * NLAT] = o.transpose(2, 1, 0).reshape(NLAT, D)
    return out
```
